# Optimizing a Trainium2 kernel written in Bass

```python
import functools
import jax
import jax.numpy as jnp
from jax import lax
import numpy as np

D_MODEL = 2048
BATCH = 4
SEQ = 8192
DEPTH = 1

CTX_LEN = 256
GRID_W = 64
CHUNK = 128
EPS = 1e-6
ADA_CHUNKS = 6

R_HEADS = 8
R_DK = 64
R_DV = 128
R_QK = R_HEADS * R_DK
R_WIDTH = R_HEADS * R_DV
ROPE_BASE = 10000.0

M_HEADS = 4
M_DK = 128
M_DV = 256
M_QK = M_HEADS * M_DK
M_WIDTH = M_HEADS * M_DV
M_CONV = 3
M_N_GATES = 2 * 2 * M_HEADS

P_HEADS = 8
P_NKEYS = 128
P_EXPERTS = P_NKEYS * P_NKEYS
P_DQ = 256
P_TOPK = 16
P_BLOCK = 128

IN_SPLITS = (R_QK, R_QK, R_WIDTH, R_WIDTH, M_QK, M_QK, M_WIDTH, M_WIDTH, M_N_GATES, D_MODEL, D_MODEL)
IN_COLS = 2 * R_QK + 2 * R_WIDTH + 2 * M_QK + 2 * M_WIDTH + M_N_GATES + 2 * D_MODEL

kernel_name = 'hybrid_retention_mlstm_peer_dit'


def _split_in(p):
    out, off = [], 0
    for size in IN_SPLITS:
        out.append(p[..., off:off + size])
        off += size
    return out


def _rmsnorm(x, g):
    xf = x.astype(jnp.float32)
    y = xf * lax.rsqrt(jnp.mean(xf * xf, axis=-1, keepdims=True) + EPS)
    return (y * g.astype(jnp.float32)).astype(x.dtype)


def _modulate(x, g, shift, scale):
    return _rmsnorm(x, g) * (1.0 + scale) + shift


def _head_norm(y):
    yc = y - jnp.mean(y, axis=-1, keepdims=True)
    return yc * lax.rsqrt(jnp.mean(yc * yc, axis=-1, keepdims=True) + EPS)


def _rotary_2d(t, row, col):
    half = t.shape[-1] // 2
    n_ax = half // 2
    freq = ROPE_BASE ** (-jnp.arange(n_ax, dtype=jnp.float32) / n_ax)
    ang = jnp.concatenate([row[:, None] * freq, col[:, None] * freq], axis=-1)
    cos = jnp.cos(ang)[None, :, None, :]
    sin = jnp.sin(ang)[None, :, None, :]
    t1, t2 = t[..., :half], t[..., half:]
    return jnp.concatenate([t1 * cos - t2 * sin, t2 * cos + t1 * sin], axis=-1)


def _to_chunks(t):
    bsz, length = t.shape[:2]
    t = t.reshape((bsz, length // CHUNK, CHUNK) + t.shape[2:])
    return jnp.swapaxes(jnp.moveaxis(t, 1, 0), 2, 3)


def _from_chunks(t):
    t = jnp.moveaxis(jnp.swapaxes(t, 2, 3), 0, 1)
    return t.reshape((t.shape[0], t.shape[1] * t.shape[2]) + t.shape[3:])


def _flip(ts):
    return tuple(jnp.flip(t, axis=1) for t in ts)


def _bidirectional(scan_f, scan_b, lat_f, lat_b, ctx_f, ctx_b, init, need_ctx):
    st_f, yc_f = scan_f(ctx_f, init, need_ctx)
    st_b, yc_b = scan_b(_flip(ctx_b), init, need_ctx)
    _, yl_f = scan_f(lat_f, st_f, True)
    _, yl_b = scan_b(_flip(lat_b), st_b, True)
    y_lat = yl_f + jnp.flip(yl_b, axis=1)
    y_ctx = yc_f + jnp.flip(yc_b, axis=1) if need_ctx else None
    return y_lat, y_ctx


def _retention_scan(seqs, s0, with_out, log_gamma):
    q, k, v = (_to_chunks(t) for t in seqs)
    pos = jnp.arange(CHUNK, dtype=jnp.float32)
    lg = log_gamma[:, None]
    rel = pos[:, None] - pos[None, :]
    intra = jnp.where(rel >= 0, jnp.exp(lg[:, :, None] * jnp.maximum(rel, 0.0)), 0.0)
    q_dec = jnp.exp(lg * (pos + 1.0))[None, :, :, None]
    k_dec = jnp.exp(lg * (CHUNK - 1.0 - pos))[None, :, :, None]
    c_dec = jnp.exp(lg * CHUNK)[None, :, :, None]

    def step(s, inp):
        qc, kc, vc = inp
        s_new = c_dec * s + jnp.einsum('bhcd,bhce->bhde', kc * k_dec, vc)
        if not with_out:
            return s_new, None
        att = jnp.einsum('bhcd,bhsd->bhcs', qc, kc) * intra
        y = jnp.einsum('bhcs,bhse->bhce', att, vc) + jnp.einsum('bhcd,bhde->bhce', qc * q_dec, s)
        return s_new, y

    s_fin, ys = lax.scan(step, s0, (q, k, v))
    return s_fin, (_from_chunks(ys) if with_out else None)


def _mlstm_scan(seqs, state0, with_out):
    q, k, v, ig, lf = (_to_chunks(t) for t in seqs)
    tri = jnp.tril(jnp.ones((CHUNK, CHUNK), dtype=bool))

    def step(carry, inp):
        c_prev, n_prev, m_prev = carry
        qc, kc, vc, ic, fc = inp
        b = jnp.cumsum(fc, axis=-1)
        b_end = b[..., -1]
        w_log = b_end[..., None] - b + ic
        a_end = b_end + m_prev
        m_new = jnp.maximum(a_end, jnp.max(w_log, axis=-1))
        p = jnp.exp(a_end - m_new)
        kw = kc * jnp.exp(w_log - m_new[..., None])[..., None]
        c_new = p[..., None, None] * c_prev + jnp.einsum('bhcd,bhce->bhde', kw, vc)
        n_new = p[..., None] * n_prev + jnp.sum(kw, axis=2)
        new = (c_new, n_new, m_new)
        if not with_out:
            return new, None
        a = b + m_prev[..., None]
        d_log = jnp.where(tri, b[..., :, None] - b[..., None, :] + ic[..., None, :], -jnp.inf)
        m_t = jnp.maximum(a, jnp.max(d_log, axis=-1))
        att = jnp.einsum('bhcd,bhsd->bhcs', qc, kc) * jnp.exp(d_log - m_t[..., None])
        inter = jnp.exp(a - m_t)[..., None]
        num = jnp.einsum('bhcs,bhse->bhce', att, vc) + inter * jnp.einsum('bhcd,bhde->bhce', qc, c_prev)
        den = jnp.sum(att, axis=-1, keepdims=True) + inter * jnp.einsum('bhcd,bhd->bhc', qc, n_prev)[..., None]
        h = num / jnp.maximum(jnp.abs(den), jnp.exp(-m_t)[..., None])
        return new, h

    st, hs = lax.scan(step, state0, (q, k, v, ig, lf))
    return st, (_from_chunks(hs) if with_out else None)


def _retention_inputs(q, k, v, row, col):
    bsz, length = q.shape[:2]
    qh = q.reshape(bsz, length, R_HEADS, R_DK).astype(jnp.float32)
    kh = k.reshape(bsz, length, R_HEADS, R_DK).astype(jnp.float32)
    if row is not None:
        qh = _rotary_2d(qh, row, col)
        kh = _rotary_2d(kh, row, col)
    vh = v.reshape(bsz, length, R_HEADS, R_DV).astype(jnp.float32)
    return (qh, kh * (R_DK ** -0.5), vh)


def _dwconv_centred(x, w):
    ksz, ch = w.shape
    return lax.conv_general_dilated(x, w[:, None, :].astype(x.dtype), window_strides=(1,),
                                    padding=[(ksz // 2, ksz - 1 - ksz // 2)],
                                    dimension_numbers=('NWC', 'WIO', 'NWC'), feature_group_count=ch)


def _mlstm_inputs(q, k, v, gates, conv_w, gate_bias):
    bsz, length = q.shape[:2]
    qk = jax.nn.silu(_dwconv_centred(jnp.concatenate([q, k], axis=-1), conv_w))
    qh = qk[..., :M_QK].reshape(bsz, length, M_HEADS, M_DK).astype(jnp.float32)
    kh = qk[..., M_QK:].reshape(bsz, length, M_HEADS, M_DK).astype(jnp.float32) * (M_DK ** -0.5)
    vh = v.reshape(bsz, length, M_HEADS, M_DV).astype(jnp.float32)
    g = gates.astype(jnp.float32).reshape(bsz, length, 2, 2, M_HEADS) + gate_bias.astype(jnp.float32)
    fwd = (qh, kh, vh, g[:, :, 0, 0], jax.nn.log_sigmoid(g[:, :, 0, 1]))
    bwd = (qh, kh, vh, g[:, :, 1, 0], jax.nn.log_sigmoid(g[:, :, 1, 1]))
    return fwd, bwd


def _retention_out(y, gate):
    bsz, length = y.shape[:2]
    return _head_norm(y).reshape(bsz, length, R_WIDTH).astype(gate.dtype) * jax.nn.silu(gate)


def _mlstm_out(h, o):
    bsz, length = h.shape[:2]
    return _head_norm(h).reshape(bsz, length, M_WIDTH).astype(o.dtype) * jax.nn.sigmoid(o)


def _merge(yr, ym, g_r, g_m, w_ret_out, w_mlstm_out, w_out):
    merged = jax.nn.sigmoid(g_r) * (yr @ w_ret_out) + jax.nn.sigmoid(g_m) * (ym @ w_mlstm_out)
    return merged @ w_out


def _peer(h, w_query, sub_keys, expert_down, expert_up):
    bsz, length, dm = h.shape
    tokens = h.reshape(-1, P_BLOCK, dm)

    def block(tb):
        qh = (tb @ w_query).reshape(P_BLOCK, P_HEADS, 2, P_DQ // 2)
        s = jnp.einsum('thpd,hpkd->thpk', qh, sub_keys).astype(jnp.float32)
        sv, si = lax.top_k(s, P_TOPK)
        cand_s = (sv[:, :, 0, :, None] + sv[:, :, 1, None, :]).reshape(P_BLOCK, P_HEADS, P_TOPK * P_TOPK)
        cand_i = (si[:, :, 0, :, None] * P_NKEYS + si[:, :, 1, None, :]).reshape(P_BLOCK, P_HEADS, P_TOPK * P_TOPK)
        best_s, best_j = lax.top_k(cand_s, P_TOPK)
        eid = jnp.take_along_axis(cand_i, best_j, axis=-1)
        g = jax.nn.softmax(best_s, axis=-1)
        act = jax.nn.gelu(jnp.einsum('thkd,td->thk', expert_down[eid], tb).astype(jnp.float32), approximate=False)
        return jnp.einsum('thk,thkd->td', (g * act).astype(tb.dtype), expert_up[eid])

    return lax.map(block, tokens).reshape(bsz, length, dm)


def _layer(x, ctx, c, c_ctx, row, col, w_ada, b_ada, norm1_g, w_in, ret_decay, m_conv, m_gate_bias,
           w_ret_out, w_mlstm_out, w_out, norm2_g, peer_query, peer_keys, peer_down, peer_up, update_ctx):
    f32 = jnp.float32
    bsz = x.shape[0]
    sh1, sc1, gt1, sh2, sc2, gt2 = jnp.split((jax.nn.silu(c) @ w_ada + b_ada)[:, None, :], ADA_CHUNKS, axis=-1)
    csh1, csc1, cgt1, csh2, csc2, cgt2 = jnp.split(jax.nn.silu(c_ctx) @ w_ada + b_ada, ADA_CHUNKS, axis=-1)
    rq, rk, rv, rg, mq, mk, mv, mo, mg, gr, gm = _split_in(_modulate(x, norm1_g, sh1, sc1) @ w_in)
    crq, crk, crv, crg, cmq, cmk, cmv, cmo, cmg, cgr, cgm = _split_in(_modulate(ctx, norm1_g, csh1, csc1) @ w_in)

    log_gamma = jax.nn.log_sigmoid(ret_decay.astype(f32))
    r_lat = _retention_inputs(rq, rk, rv, row, col)
    r_ctx = _retention_inputs(crq, crk, crv, None, None)
    s0 = jnp.zeros((bsz, R_HEADS, R_DK, R_DV), f32)
    yr, yr_c = _bidirectional(functools.partial(_retention_scan, log_gamma=log_gamma[0]),
                              functools.partial(_retention_scan, log_gamma=log_gamma[1]),
                              r_lat, r_lat, r_ctx, r_ctx, s0, update_ctx)

    m_lat_f, m_lat_b = _mlstm_inputs(mq, mk, mv, mg, m_conv, m_gate_bias)
    m_ctx_f, m_ctx_b = _mlstm_inputs(cmq, cmk, cmv, cmg, m_conv, m_gate_bias)
    m0 = (jnp.zeros((bsz, M_HEADS, M_DK, M_DV), f32), jnp.zeros((bsz, M_HEADS, M_DK), f32),
          jnp.zeros((bsz, M_HEADS), f32))
    ym, ym_c = _bidirectional(_mlstm_scan, _mlstm_scan, m_lat_f, m_lat_b, m_ctx_f, m_ctx_b, m0, update_ctx)

    x = x + gt1 * _merge(_retention_out(yr, rg), _mlstm_out(ym, mo), gr, gm, w_ret_out, w_mlstm_out, w_out)
    x = x + gt2 * _peer(_modulate(x, norm2_g, sh2, sc2), peer_query, peer_keys, peer_down, peer_up)
    if update_ctx:
        ctx = ctx + cgt1 * _merge(_retention_out(yr_c, crg), _mlstm_out(ym_c, cmo), cgr, cgm,
                                  w_ret_out, w_mlstm_out, w_out)
        ctx = ctx + cgt2 * _peer(_modulate(ctx, norm2_g, csh2, csc2), peer_query, peer_keys, peer_down, peer_up)
    return x, ctx


def setup_inputs(seed: int = 0) -> dict:
    key = jax.random.key(seed)
    ks = jax.random.split(key, 24)
    f32 = jnp.float32

    def nrm(k, shape, scale):
        return jax.random.normal(k, shape, f32) * scale

    x = nrm(ks[0], (BATCH, SEQ, D_MODEL), 1.0)
    c = nrm(ks[1], (BATCH, D_MODEL), 1.0)
    ctx = nrm(ks[2], (BATCH, CTX_LEN, D_MODEL), 1.0)
    c_ctx = nrm(ks[3], (D_MODEL,), 1.0)
    w_ada = nrm(ks[4], (DEPTH, D_MODEL, ADA_CHUNKS * D_MODEL), D_MODEL ** -0.5)
    b_ada = nrm(ks[5], (DEPTH, ADA_CHUNKS * D_MODEL), 0.02)
    norm1_g = 1.0 + nrm(ks[6], (DEPTH, D_MODEL), 0.02)
    w_in = nrm(ks[7], (DEPTH, D_MODEL, IN_COLS), D_MODEL ** -0.5)
    decay_logit = jnp.log(2.0 ** (5.0 + jnp.arange(R_HEADS, dtype=f32)) - 1.0)
    ret_decay = decay_logit[None, None, :] + nrm(ks[8], (DEPTH, 2, R_HEADS), 0.05)
    m_conv = nrm(ks[9], (DEPTH, M_CONV, 2 * M_QK), M_CONV ** -0.5)
    gate_base = jnp.stack([jnp.zeros((M_HEADS,), f32), jnp.linspace(3.0, 6.0, M_HEADS, dtype=f32)])
    m_gate_bias = gate_base[None, None] + nrm(ks[10], (DEPTH, 2, 2, M_HEADS), 0.1)
    w_ret_out = nrm(ks[11], (DEPTH, R_WIDTH, D_MODEL), R_WIDTH ** -0.5)
    w_mlstm_out = nrm(ks[12], (DEPTH, M_WIDTH, D_MODEL), M_WIDTH ** -0.5)
    w_out = nrm(ks[13], (DEPTH, D_MODEL, D_MODEL), D_MODEL ** -0.5)
    norm2_g = 1.0 + nrm(ks[14], (DEPTH, D_MODEL), 0.02)
    peer_query = nrm(ks[15], (DEPTH, D_MODEL, P_HEADS * P_DQ), D_MODEL ** -0.5)
    peer_keys = nrm(ks[16], (DEPTH, P_HEADS, 2, P_NKEYS, P_DQ // 2), (P_DQ // 2) ** -0.5)
    peer_down = nrm(ks[17], (DEPTH, P_EXPERTS, D_MODEL), D_MODEL ** -0.5)
    peer_up = nrm(ks[18], (DEPTH, P_EXPERTS, D_MODEL), P_HEADS ** -0.5)
    final_g = 1.0 + nrm(ks[19], (D_MODEL,), 0.02)
    return {'x': x, 'c': c, 'ctx': ctx, 'c_ctx': c_ctx, 'w_ada': w_ada, 'b_ada': b_ada,
            'norm1_g': norm1_g, 'w_in': w_in, 'ret_decay': ret_decay, 'm_conv': m_conv,
            'm_gate_bias': m_gate_bias, 'w_ret_out': w_ret_out, 'w_mlstm_out': w_mlstm_out,
            'w_out': w_out, 'norm2_g': norm2_g, 'peer_query': peer_query, 'peer_keys': peer_keys,
            'peer_down': peer_down, 'peer_up': peer_up, 'final_g': final_g}


def reference(x, c, ctx, c_ctx, w_ada, b_ada, norm1_g, w_in, ret_decay, m_conv, m_gate_bias,
              w_ret_out, w_mlstm_out, w_out, norm2_g, peer_query, peer_keys, peer_down, peer_up, final_g):
    length = x.shape[1]
    ROWS = length // GRID_W
    row = jnp.repeat(jnp.arange(ROWS, dtype=jnp.float32), GRID_W)
    col = jnp.tile(jnp.arange(GRID_W, dtype=jnp.float32), ROWS)
    for layer in range(DEPTH):
        x, ctx = _layer(x, ctx, c, c_ctx, row, col, w_ada[layer], b_ada[layer], norm1_g[layer], w_in[layer],
                        ret_decay[layer], m_conv[layer], m_gate_bias[layer], w_ret_out[layer],
                        w_mlstm_out[layer], w_out[layer], norm2_g[layer], peer_query[layer],
                        peer_keys[layer], peer_down[layer], peer_up[layer], update_ctx=layer < DEPTH - 1)
    return _rmsnorm(x, final_g)
```

```python
import math
from contextlib import ExitStack
import numpy as np
import concourse.bass as bass
import concourse.mybir as mybir
from concourse.bass_utils import run_bass_kernel_spmd

F32 = mybir.dt.float32
BF16 = mybir.dt.bfloat16
I32 = mybir.dt.int32
U32 = mybir.dt.uint32
ALU = mybir.AluOpType
AF = mybir.ActivationFunctionType
AX = mybir.AxisListType

D = 2048
KC = 16
EPS = 1e-6
NEG = -1.0e30


class Buf:
    __slots__ = ("name", "w", "r", "multi", "sem", "base")

    def __init__(self, name, multi=False):
        self.name = name
        self.w = {}
        self.r = {}
        self.multi = multi
        self.sem = None
        self.base = {}

    def fresh(self):
        base = dict(self.w)
        for k, v in self.r.items():
            if base.get(k, 0) < v:
                base[k] = v
        self.base = base
        self.w = {}
        self.r = {}


class Prog:
    def __init__(self, nc, es):
        self.nc = nc
        self.es = es
        self.eng = {"pe": nc.tensor, "dve": nc.vector, "act": nc.scalar, "pool": nc.gpsimd, "sp": nc.sync}
        self.sems = {}
        self.cnt = {}
        self.waited = {e: {} for e in self.eng}
        for e in self.eng:
            self.sems[e] = es.enter_context(nc.semaphore("s_" + e))
            self.cnt[e] = 0
        self.ndsem = 0
        self.ninst = 0
        self.rec = None

    class _RecEng:
        def __init__(self):
            self.call = None

        def __getattr__(self, name):
            def f(*a, **k):
                self.call = (name, a, k)
                return None
            return f

    def zip_emit(self, lists):
        saved, self.rec = self.rec, None
        lists = [l for l in lists if l]
        idx = [0] * len(lists)
        total = sum(len(l) for l in lists)
        for _ in range(total):
            best = min((idx[i] / len(lists[i]), i) for i in range(len(lists)) if idx[i] < len(lists[i]))[1]
            it = lists[best][idx[best]]
            idx[best] += 1
            if it[0] == "op":
                _, e, name, a_, k_, r, w = it
                self.op(e, lambda eng, name=name, a_=a_, k_=k_: getattr(eng, name)(*a_, **k_), r, w)
            else:
                _, out, in_, r, w, sb, e, kw = it
                self.dma(out, in_, r=r, w=w, sb=sb, e=e, **kw)
        self.rec = saved

    def _deps(self, r, w):
        deps = {}

        def add(k, v):
            if deps.get(k, 0) < v:
                deps[k] = v
        for b in r:
            for k, v in b.w.items():
                add(k, v)
        for b in w:
            if not b.multi:
                for k, v in b.w.items():
                    add(k, v)
                for k, v in b.r.items():
                    add(k, v)
            else:
                for k, v in b.base.items():
                    add(k, v)
        return deps

    def _wait(self, e, deps):
        wd = self.waited[e]
        for k, v in deps.items():
            if e == "pe" and k == "pe":
                continue
            if wd.get(k, 0) >= v:
                continue
            self.eng[e].wait_ge(self.sems[k], v)
            wd[k] = v
            self.ninst += 1

    def _mark(self, key, val, r, w):
        for b in r:
            if b.r.get(key, 0) < val:
                b.r[key] = val
        for b in w:
            if b.multi:
                if b.w.get(key, 0) < val:
                    b.w[key] = val
            else:
                b.w = {key: val}
                b.r = {}

    def op(self, e, fn, r=(), w=()):
        if self.rec is not None:
            pe = Prog._RecEng()
            fn(pe)
            name, a_, k_ = pe.call
            self.rec.append(("op", e, name, a_, k_, tuple(r), tuple(w)))
            return None
        self._wait(e, self._deps(r, w))
        ins = fn(self.eng[e])
        self.cnt[e] += 1
        ins.then_inc(self.sems[e], 1)
        self.ninst += 1
        self._mark(e, self.cnt[e], r, w)
        return ins

    def dma(self, out, in_, r=(), w=(), sb=None, e="sp", **kw):
        if self.rec is not None:
            self.rec.append(("dma", out, in_, tuple(r), tuple(w), sb, e, kw))
            return None
        if sb.sem is None:
            key = "d%d" % self.ndsem
            self.ndsem += 1
            self.sems[key] = self.es.enter_context(self.nc.semaphore("s_" + key))
            self.cnt[key] = 0
            sb.sem = key
        key = sb.sem
        self._wait(e, self._deps(r, w))
        ins = self.eng[e].dma_start(out=out, in_=in_, **kw)
        self.cnt[key] += 16
        ins.then_inc(self.sems[key], 16)
        self.ninst += 1
        self._mark(key, self.cnt[key], r, w)
        return ins

    def gather(self, out, table, idx_ap, r=(), w=(), sb=None):
        if sb.sem is None:
            key = "d%d" % self.ndsem
            self.ndsem += 1
            self.sems[key] = self.es.enter_context(self.nc.semaphore("s_" + key))
            self.cnt[key] = 0
            sb.sem = key
        key = sb.sem
        e = "pool"
        self._wait(e, self._deps(r, w))
        ins = self.nc.gpsimd.indirect_dma_start(
            out=out, out_offset=None, in_=table,
            in_offset=bass.IndirectOffsetOnAxis(ap=idx_ap, axis=0))
        self.cnt[key] += 16
        ins.then_inc(self.sems[key], 16)
        self.ninst += 1
        self._mark(key, self.cnt[key], r, w)
        return ins

    def finish(self):
        for k, v in self.cnt.items():
            if v > 0 and k != "sp":
                self.nc.sync.wait_ge(self.sems[k], v)


R_HEADS, R_DK, R_DV = 8, 64, 128
M_HEADS, M_DK, M_DV = 4, 128, 256
IN_SPLITS = (512, 512, 1024, 1024, 512, 512, 1024, 1024, 16, 2048, 2048)
IN_OFF = [0]
for _s in IN_SPLITS:
    IN_OFF.append(IN_OFF[-1] + _s)
(O_RQ, O_RK, O_RV, O_RG, O_MQ, O_MK, O_MV, O_MO, O_MG, O_GR, O_GM, IN_COLS) = IN_OFF
P_HEADS, P_NK, P_TOPK = 8, 128, 16
NEXP = 16384
CL = 256


class Ring:
    def __init__(self, nc, es, name, shape, dt, n, psum=False, multi=False):
        self.items = []
        self.multi = multi
        for i in range(n):
            if psum:
                t = es.enter_context(nc.psum_tensor("r_%s%d" % (name, i), shape, dt))
            else:
                t = es.enter_context(nc.sbuf_tensor("r_%s%d" % (name, i), shape, dt))
            self.items.append((t, Buf("%s%d" % (name, i), multi)))
        self.i = 0

    def next(self):
        it = self.items[self.i % len(self.items)]
        self.i += 1
        if self.multi:
            it[1].fresh()
        return it


class RingView:
    def __init__(self, items):
        self.items = list(items)
        self.i = 0

    def next(self):
        it = self.items[self.i % len(self.items)]
        self.i += 1
        return it


def build_program(L, dbg=False):
    H = L // 2
    NST = H // 512
    nc = bass.Bass("TRN2", target_bir_lowering=False)

    def din(name, shape, dt=F32):
        return nc.dram_tensor(name, list(shape), dt, kind="ExternalInput").ap()

    def dscr(name, shape, dt):
        return nc.dram_tensor(name, list(shape), dt, kind=("ExternalOutput" if dbg else "Internal")).ap()

    x_d = din("x", [L, D])
    ctx_d = din("ctx", [CL, D])
    cvec_d = din("cvec", [2, D])
    wada_d = din("w_ada", [D, 6 * D])
    bada_d = din("b_ada", [1, 6 * D])
    g1_d = din("norm1_g", [1, D])
    win_d = din("w_in", [D, IN_COLS])
    wmg_d = din("w_mg", [D, 16])
    gbias_d = din("gbias", [1, 16])
    rdec_d = din("ret_decay", [1, 16])
    mconv_d = din("m_conv", [3, 1024])
    wro_d = din("w_ret_out", [1024, D])
    wmo_d = din("w_mlstm_out", [1024, D])
    wout_d = din("w_out", [D, D])
    g2_d = din("norm2_g", [1, D])
    pq_d = din("peer_query", [D, D])
    pk_d = din("peer_keys", [16 * 128, 128])
    pdn_d = din("peer_down", [NEXP, D])
    pup_d = din("peer_up", [NEXP, D])
    fg_d = din("final_g", [1, D])
    rot_d = din("rot", [L, 64])
    cst_d = din("cst", [128, 6, 128])
    pos_d = din("pos", [128, 4])
    out_d = nc.dram_tensor("out", [H, D], F32, kind="ExternalOutput").ap()

    xmT_d = dscr("xmT", [D, L + 2], BF16)
    cmT_d = dscr("cmT", [D, CL + 2], BF16)
    ada_d = dscr("ada", [2, 6 * D], F32)
    winb_d = dscr("winb", [D, IN_COLS], BF16)
    wrob_d = dscr("wrob", [1024, D], BF16)
    wmob_d = dscr("wmob", [1024, D], BF16)
    woutb_d = dscr("woutb", [D, D], BF16)
    pqb_d = dscr("pqb", [D, D], BF16)
    pcomb_d = dscr("pcomb", [NEXP, 2 * D], BF16)
    yA_d = dscr("yA", [H, D], F32)
    yoT_d = dscr("yoT", [D, H], BF16)
    x1_d = dscr("x1", [H, D], F32)
    B_xmT, B_cmT, B_ada = Buf("xmT_d", True), Buf("cmT_d", True), Buf("ada_d")
    B_winb, B_wrob, B_wmob, B_woutb, B_pqb = (Buf(n, True) for n in ("winb", "wrob", "wmob", "woutb", "pqb"))
    B_pdnb, B_pupb = Buf("pdnb", True), Buf("pupb", True)
    B_yA, B_yoT, B_x1, B_out = Buf("yA", True), Buf("yoT", True), Buf("x1", True), Buf("out", True)

    with ExitStack() as es:
        P = Prog(nc, es)

        def barrier():
            for e in ("pe", "dve", "act", "pool", "sp"):
                P._wait(e, {k: v for k, v in P.cnt.items() if v > 0 and k != e})

        def sbt(st, name, shape, dt):
            return st.enter_context(nc.sbuf_tensor("t_" + name, shape, dt)), Buf(name)

        cst, b_cst = sbt(es, "cst", [128, 6, 128], F32)
        pos, b_pos = sbt(es, "pos", [128, 4], F32)
        identb, b_identb = sbt(es, "identb", [128, 128], BF16)
        P.dma(cst[:], cst_d, w=[b_cst], sb=b_cst)
        P.dma(pos[:], pos_d, w=[b_pos], sb=b_pos)
        P.op("dve", lambda e: e.tensor_copy(identb[:], cst[:, 0, :]), r=[b_cst], w=[b_identb])
        identf = cst[:, 0, :]
        mhalf, b_mhalf = sbt(es, "mhalf", [128, 16], F32)
        P.op("pool", lambda e: e.memset(mhalf[:], -0.5), w=[b_mhalf])
        tri = [cst[:, 1, :], cst[:, 2, :]]
        negm = [cst[:, 3, :], cst[:, 4, :]]
        onesf = cst[:, 5, :]
        modF, b_modF = sbt(es, "modF", [128, 6, KC], F32)
        psr = Ring(nc, es, "ps", [128, 512], F32, 8, psum=True)

        with ExitStack() as ph:
            cT, b_cT = sbt(ph, "cT", [128, 2, KC], F32)
            cTs, b_cTs = sbt(ph, "cTs", [128, 2, KC], F32)
            for r_ in range(2):
                P.dma(cT[:, r_, :], cvec_d[r_:r_ + 1, :].rearrange("r (k p) -> p (r k)", p=128), w=[b_cT], sb=b_cT,
                      allow_slow_non_contiguous=True)
            P.op("act", lambda e: e.activation(cTs[:], cT[:], AF.Silu), r=[b_cT], w=[b_cTs])
            adasb, b_adasb = sbt(ph, "adasb", [2, 6 * D], F32)
            badasb, b_badasb = sbt(ph, "badasb", [2, 6 * D], F32)
            P.dma(badasb[:], bada_d.partition_broadcast(2), w=[b_badasb], sb=b_badasb)
            wring = Ring(nc, ph, "wada", [128, KC, 512], F32, 2)
            for nb in range(24):
                wt, bw = wring.next()
                P.dma(wt[:], wada_d[:, nb * 512:(nb + 1) * 512].rearrange("(k p) n -> p k n", p=128),
                      w=[bw], sb=bw)
                pt, bp = psr.next()
                for k in range(KC):
                    P.op("pe", lambda e: e.matmul(pt[0:2, :], lhsT=cTs[:, :, k], rhs=wt[:, k, :],
                                                  start=(k == 0), stop=(k == KC - 1)),
                         r=[b_cTs, bw], w=[bp])
                P.op("dve", lambda e: e.tensor_tensor(adasb[:, nb * 512:(nb + 1) * 512], pt[0:2, :],
                                                      badasb[:, nb * 512:(nb + 1) * 512], ALU.add),
                     r=[bp, b_badasb], w=[b_adasb])
            P.dma(ada_d, adasb[:], r=[b_adasb], w=[B_ada], sb=b_adasb)
            raw, b_raw = sbt(ph, "rawmod", [128, 8, KC], F32)
            srcs = [ada_d[0:1, 0:D], ada_d[0:1, D:2 * D], ada_d[0:1, 3 * D:4 * D], ada_d[0:1, 4 * D:5 * D],
                    ada_d[1:2, 0:D], ada_d[1:2, D:2 * D], g1_d, g2_d]
            for i, s in enumerate(srcs):
                P.dma(raw[:, i, :], s.rearrange("r (k p) -> p (r k)", p=128), r=[B_ada], w=[b_raw], sb=b_raw,
                      allow_slow_non_contiguous=True)
            for (dst, sc, g, sh) in ((0, 1, 6, 0), (2, 5, 6, 4), (4, 3, 7, 2)):
                P.op("dve", lambda e: e.scalar_tensor_tensor(out=modF[:, dst, :], in0=raw[:, sc, :], scalar=1.0,
                                                             in1=raw[:, g, :], op0=ALU.add, op1=ALU.mult),
                     r=[b_raw], w=[b_modF])
                P.op("dve", lambda e: e.tensor_copy(modF[:, dst + 1, :], raw[:, sh, :]), r=[b_raw], w=[b_modF])
        barrier()

        with ExitStack() as ph:
            FMAX = 4096
            cin = Ring(nc, ph, "cvin", [128, FMAX], F32, 6)
            cout = Ring(nc, ph, "cvout", [128, FMAX], BF16, 6)
            cnt = [0]

            def convert(pairs, bdst):
                for (sa, da) in pairs:
                    fsz = sa.shape[1]
                    ti, bi = cin.next()
                    to, bo = cout.next()
                    P.dma(ti[:, 0:fsz], sa, w=[bi], sb=bi)
                    eng = ("dve", "act")[cnt[0] % 2]
                    cnt[0] += 1
                    if eng == "act":
                        P.op("act", lambda e: e.copy(to[:, 0:fsz], ti[:, 0:fsz]), r=[bi], w=[bo])
                    else:
                        P.op(eng, lambda e: e.tensor_copy(to[:, 0:fsz], ti[:, 0:fsz]), r=[bi], w=[bo])
                    if len(da.shape) == 3:
                        P.dma(da, to[:, 0:fsz].rearrange("p (r c) -> p r c", r=da.shape[1]), r=[bo], w=[bdst], sb=bo)
                    else:
                        P.dma(da, to[:, 0:fsz], r=[bo], w=[bdst], sb=bo)

            convert([(win_d[rb * 128:(rb + 1) * 128, cb * 2564:(cb + 1) * 2564], winb_d[rb * 128:(rb + 1) * 128, cb * 2564:(cb + 1) * 2564])
                     for rb in range(16) for cb in range(4)], B_winb)
            for s_, d_, b_ in ((wro_d, wrob_d, B_wrob), (wmo_d, wmob_d, B_wmob), (wout_d, woutb_d, B_woutb), (pq_d, pqb_d, B_pqb)):
                sv_ = s_.rearrange("(rb p r) c -> rb p (r c)", p=128, r=2)
                dv_ = d_.rearrange("(rb p r) c -> rb p (r c)", p=128, r=2)
                convert([(sv_[i], dv_[i]) for i in range(sv_.shape[0])], b_)
            for s_, c0_, b_ in ((pdn_d, 0, B_pdnb), (pup_d, D, B_pupb)):
                sv_ = s_.rearrange("(rb p r) c -> rb p (r c)", p=128, r=2)
                dv_ = pcomb_d.rearrange("(rb p r) c -> rb p r c", p=128, r=2)
                convert([(sv_[i], dv_[i][:, :, c0_:c0_ + D]) for i in range(sv_.shape[0])], b_)
        barrier()

        with ExitStack() as ph:
            xin = Ring(nc, ph, "xin", [128, D], F32, 3)
            xnr = Ring(nc, ph, "xn", [128, D], BF16, 2)
            jk, b_jk = sbt(ph, "jk", [128, D], BF16)
            stat = Ring(nc, ph, "stat", [128, 4], F32, 3)
            xts = Ring(nc, ph, "xts", [128, KC, 512], BF16, 2, multi=True)
            zt, b_zt = sbt(ph, "zt", [128, KC, 1], BF16)
            P.op("pool", lambda e: e.memset(zt[:], 0.0), w=[b_zt])
            for dst, bd, n in ((xmT_d, B_xmT, L), (cmT_d, B_cmT, CL)):
                v = dst.rearrange("(k p) n -> p k n", p=128)
                P.dma(v[:, :, 0:1], zt[:], r=[b_zt], w=[bd], sb=b_zt, allow_slow_non_contiguous=True)
                P.dma(v[:, :, n + 1:n + 2], zt[:], r=[b_zt], w=[bd], sb=b_zt, allow_slow_non_contiguous=True)
            units = [(ctx_d, cmT_d, B_cmT, 0, CL, 2)] + [(x_d, xmT_d, B_xmT, s * 512, 512, 0) for s in range(L // 512)]
            ecnt = 0
            for (src, dst, bd, base, NT, mi) in units:
                XT, bXT = xts.next()
                for j in range(NT // 128):
                    xt, bx = xin.next()
                    P.dma(xt[:], src[base + j * 128: base + (j + 1) * 128, :], w=[bx], sb=bx)
                    st, bs = stat.next()
                    P.op("dve", lambda e: e.scalar_tensor_tensor(out=jk[:], in0=xt[:], scalar=1.0, in1=xt[:],
                                                                 op0=ALU.mult, op1=ALU.mult, accum_out=st[:, 0:1]),
                         r=[bx], w=[b_jk, bs])
                    P.op("dve", lambda e: e.tensor_scalar(st[:, 1:2], st[:, 0:1], 1.0 / D, EPS, op0=ALU.mult, op1=ALU.add),
                         r=[bs], w=[bs])
                    P.op("act", lambda e: e.activation(st[:, 2:3], st[:, 1:2], AF.Sqrt), r=[bs], w=[bs])
                    P.op("dve", lambda e: e.reciprocal(st[:, 3:4], st[:, 2:3]), r=[bs], w=[bs])
                    xn, bxn = xnr.next()
                    P.op("act", lambda e: e.activation(xn[:], xt[:], AF.Identity, scale=st[:, 3:4]), r=[bx, bs], w=[bxn])
                    for half in range(2):
                        pt, bp = psr.next()
                        ptb = pt[:].bitcast(BF16)
                        for kk in range(8):
                            k = half * 8 + kk
                            P.op("pe", lambda e: e.transpose(ptb[:, kk * 128:(kk + 1) * 128], xn[:, k * 128:(k + 1) * 128], identb[:]),
                                 r=[bxn, b_identb], w=[bp])
                        for kk in range(8):
                            k = half * 8 + kk
                            o = XT[:, k, j * 128:(j + 1) * 128]
                            i_ = ptb[:, kk * 128:(kk + 1) * 128]
                            if half == 0:
                                P.op("act", lambda e: e.activation(o, i_, AF.Identity, bias=modF[:, mi + 1, k:k + 1], scale=modF[:, mi, k:k + 1]),
                                     r=[b_modF], w=[bXT, bp])
                            else:
                                P.op("dve", lambda e: e.tensor_scalar(o, i_, modF[:, mi, k:k + 1], modF[:, mi + 1, k:k + 1], op0=ALU.mult, op1=ALU.add),
                                     r=[b_modF], w=[bXT, bp])
                P.dma(dst.rearrange("(k p) n -> p k n", p=128)[:, :, 1 + base:1 + base + NT], XT[:, :, 0:NT], r=[bXT], w=[bd], sb=bXT)
        barrier()

        with ExitStack() as ph:
            rd, b_rd = sbt(ph, "rd", [128, 16], F32)
            lgn, b_lgn = sbt(ph, "lgn", [128, 16], F32)
            tmp16, b_tmp16 = sbt(ph, "tmp16", [128, 16], F32)
            dec, b_dec = sbt(ph, "dec", [128, 2, 3, 8], F32)
            P.dma(rd[:], rdec_d.partition_broadcast(128), w=[b_rd], sb=b_rd)
            P.op("act", lambda e: e.activation(rd[:], rd[:], AF.Exp, scale=-1.0), r=[b_rd], w=[b_rd])
            P.op("dve", lambda e: e.tensor_scalar(lgn[:], rd[:], -1.0 / 8, 1.0 / 7, op0=ALU.mult, op1=ALU.add), r=[b_rd], w=[b_lgn])
            for cf in (6, 5, 4, 3, 2, 1):
                P.op("dve", lambda e: e.tensor_tensor(tmp16[:], lgn[:], rd[:], ALU.mult), r=[b_lgn, b_rd], w=[b_tmp16])
                P.op("dve", lambda e: e.tensor_scalar(lgn[:], tmp16[:], -1.0, 1.0 / cf, op0=ALU.mult, op1=ALU.add), r=[b_tmp16], w=[b_lgn])
            P.op("dve", lambda e: e.tensor_tensor(lgn[:], lgn[:], rd[:], ALU.mult), r=[b_lgn, b_rd], w=[b_lgn])
            for dr in range(2):
                P.op("dve", lambda e: e.tensor_scalar(tmp16[:, 0:8], lgn[:, dr * 8:(dr + 1) * 8], pos[:, dr:dr + 1], None, op0=ALU.mult),
                     r=[b_lgn, b_pos], w=[b_tmp16])
                P.op("act", lambda e: e.activation(dec[:, dr, 0, :], tmp16[:, 0:8], AF.Exp, scale=-1.0), r=[b_tmp16], w=[b_dec])
                P.op("act", lambda e: e.activation(dec[:, dr, 1, :], tmp16[:, 0:8], AF.Exp, bias=-math.log(8.0)), r=[b_tmp16], w=[b_dec])
                P.op("act", lambda e: e.activation(dec[:, dr, 2, :], lgn[:, dr * 8:(dr + 1) * 8], AF.Exp, scale=-128.0), r=[b_lgn], w=[b_dec])
            cw, b_cw = sbt(ph, "cw", [128, 3, 8], F32)
            for j_ in range(3):
                P.dma(cw[:, j_, :], mconv_d[j_:j_ + 1, :].rearrange("r (b p) -> p (r b)", p=128), w=[b_cw], sb=b_cw, allow_slow_non_contiguous=True)
            gb, b_gb = sbt(ph, "gb", [128, 16], F32)
            P.dma(gb[:], gbias_d.partition_broadcast(128), w=[b_gb], sb=b_gb)
            wmgf, b_wmgf = sbt(ph, "wmgf", [128, KC, 16], F32)
            wmg, b_wmg = sbt(ph, "wmg", [128, KC, 16], BF16)
            P.dma(wmgf[:], wmg_d.rearrange("(k p) n -> p k n", p=128), w=[b_wmgf], sb=b_wmgf)
            P.op("dve", lambda e: e.tensor_copy(wmg[:], wmgf[:]), r=[b_wmgf], w=[b_wmg])
            S32, b_S32 = sbt(ph, "S32", [64, 8, 128], F32)
            Sbf, b_Sbf = sbt(ph, "Sbf", [64, 8, 128], BF16)
            C32, b_C32 = sbt(ph, "C32", [128, 4, 257], F32)
            Cbf, b_Cbf = sbt(ph, "Cbf", [128, 4, 257], BF16)
            mprev, b_mprev = sbt(ph, "mprev", [128, 4], F32)
            XTr = Ring(nc, ph, "XT", [128, KC, 514], BF16, 1)
            Wbr = Ring(nc, ph, "Wb", [128, KC, 512], BF16, 2)
            RKr = Ring(nc, ph, "RK", [128, 4, 512], BF16, 1, multi=True)
            RQr = Ring(nc, ph, "RQ", [128, 4, 512], BF16, 1, multi=True)
            RVr = Ring(nc, ph, "RVs", [128, 4, 8, 128], BF16, 1, multi=True)
            MQKr = Ring(nc, ph, "MQK", [128, 8, 512], BF16, 1, multi=True)
            MVr = Ring(nc, ph, "MV", [128, 4, 1024], BF16, 1, multi=True)
            RGr = Ring(nc, ph, "RG", [128, 4, 1024], BF16, 1, multi=True)
            MOr = Ring(nc, ph, "MO", [128, 4, 1024], BF16, 1, multi=True)
            Gr = Ring(nc, ph, "G", [128, 4, 16], F32, 1, multi=True)
            rotr = Ring(nc, ph, "rot", [128, 4, 64], F32, 2)
            rawr = Ring(nc, ph, "raw", [128, 514], F32, 2)
            accr = Ring(nc, ph, "cacc", [128, 512], F32, 2)
            rtr = Ring(nc, ph, "rtmp", [128, 2, 8, 2, 32], F32, 1)
            QTr = Ring(nc, ph, "QT", [64, 8, 128], BF16, 2)
            KTr = Ring(nc, ph, "KT", [64, 8, 128], BF16, 2)
            ATr = Ring(nc, ph, "AT", [128, 8, 128], BF16, 1)
            ATmr = Ring(nc, ph, "ATm", [128, 4, 128], BF16, 2)
            MKtr = Ring(nc, ph, "MKt", [128, 4, 128], BF16, 2)
            Vmr = Ring(nc, ph, "Vm", [128, 4, 257], BF16, 2)
            gsr = Ring(nc, ph, "gs", [128, 16, 4], F32, 2)
            spur = Ring(nc, ph, "spu", [128, 2, 4, 4], F32, 2)
            Dr = Ring(nc, ph, "Dg", [128, 4, 128], F32, 1)
            Umr = Ring(nc, ph, "Um", [128, 4, 128], F32, 1)
            Yr = Ring(nc, ph, "Y", [128, D], F32, 2)
            Ym_bufs = [Buf("Ym0"), Buf("Ym1")]
            YAr = Ring(nc, ph, "YA", [128, D], F32, 1)
            sqr = Ring(nc, ph, "sq", [128, D], F32, 1)
            yor = Ring(nc, ph, "yo", [128, D], BF16, 1)
            nsr = Ring(nc, ph, "ns", [128, 4, 12], F32, 2)
            YOTr = Ring(nc, ph, "YOT", [128, KC, 512], BF16, 1, multi=True)
            dnr = Ring(nc, ph, "dn", [128, 4, 4], F32, 2)

            def proj_tok(XT, bXT, j, Wb, bW, width=512):
                pt, bp = psr.next()
                for k in range(KC):
                    P.op("pe", lambda e: e.matmul(pt[:, 0:width], lhsT=XT[:, k, 1 + j * 128:1 + (j + 1) * 128], rhs=Wb[:, k, 0:width],
                                                  start=(k == 0), stop=(k == KC - 1)), r=[bXT, bW], w=[bp])
                return pt, bp

            def load_w(off):
                Wb, bW = Wbr.next()
                P.dma(Wb[:], winb_d[:, off:off + 512].rearrange("(k p) n -> p k n", p=128), r=[B_winb], w=[bW], sb=bW)
                return Wb, bW

            def scan_pass(dr):
                maskT = tri[dr]
                qdec = dec[:, dr, 0, :]
                kdec = dec[:, dr, 1, :]
                cdec = dec[:, dr, 2, :]
                P.op("pool", lambda e: e.memset(S32[:], 0.0), w=[b_S32])
                P.op("pool", lambda e: e.memset(Sbf[:], 0.0), w=[b_Sbf])
                P.op("pool", lambda e: e.memset(C32[:], 0.0), w=[b_C32])
                P.op("pool", lambda e: e.memset(Cbf[:], 0.0), w=[b_Cbf])
                P.op("pool", lambda e: e.memset(mprev[:], 0.0), w=[b_mprev])
                if dr == 0:
                    units = [(True, 0, CL, False)] + [(False, s * 512, 512, True) for s in range(NST)]
                else:
                    units = [(True, 0, CL, False)] + [(False, s * 512, 512, False) for s in range(2 * NST - 1, NST - 1, -1)] \
                        + [(False, s * 512, 512, True) for s in range(NST - 1, -1, -1)]
                for (is_ctx, base, NT, with_out) in units:
                    nch = NT // 128
                    srcT, bsrc = (cmT_d, B_cmT) if is_ctx else (xmT_d, B_xmT)
                    XT, bXT = XTr.next()
                    P.dma(XT[:, :, 0:NT + 2], srcT.rearrange("(k p) n -> p k n", p=128)[:, :, base:base + NT + 2], r=[bsrc], w=[bXT], sb=bXT)
                    if not is_ctx:
                        rot, brot = rotr.next()
                        P.dma(rot[:, 0:nch, :], rot_d[base:base + NT, :].rearrange("(j p) c -> p j c", p=128), w=[brot], sb=brot)
                    RK, bRK = RKr.next()
                    RQ, bRQ = RQr.next()
                    RVs, bRV = RVr.next()
                    MQK, bMQK = MQKr.next()
                    MV, bMV = MVr.next()
                    G, bG = Gr.next()
                    RG, bRG = RGr.next()
                    MO, bMO = MOr.next()

                    def rotary(pt, bp, dst, bdst, j):
                        if is_ctx:
                            P.op("act", lambda e: e.copy(dst[:, j, :], pt[:, 0:512]), w=[bdst, bp])
                            return
                        tm, btm = rtr.next()
                        pv = pt[:, 0:512].rearrange("p (h t i) -> p h t i", h=8, t=2)
                        cosb = rot[:, j, 0:32].unsqueeze(1).unsqueeze(1).to_broadcast([128, 8, 2, 32])
                        sinb = rot[:, j, 32:64].unsqueeze(1).unsqueeze(1).to_broadcast([128, 8, 2, 32])
                        P.op("dve", lambda e: e.tensor_tensor(tm[:, 0], pv, cosb, ALU.mult), r=[brot], w=[btm, bp])
                        P.op("dve", lambda e: e.tensor_tensor(tm[:, 1], pv, sinb, ALU.mult), r=[brot], w=[btm, bp])
                        dv = dst[:, j, :].rearrange("p (h t i) -> p h t i", h=8, t=2)
                        P.op("pool", lambda e: e.tensor_tensor(dv[:, :, 0, :], tm[:, 0, :, 0, :], tm[:, 1, :, 1, :], ALU.subtract), r=[btm], w=[bdst])
                        P.op("pool", lambda e: e.tensor_tensor(dv[:, :, 1, :], tm[:, 0, :, 1, :], tm[:, 1, :, 0, :], ALU.add), r=[btm], w=[bdst])

                    Wb, bW = load_w(O_RK)
                    for j in range(nch):
                        pt, bp = proj_tok(XT, bXT, j, Wb, bW)
                        rotary(pt, bp, RK, bRK, j)
                    for hb in range(2):
                        Wb, bW = load_w(O_RV + hb * 512)
                        for j in range(nch):
                            pt, bp = proj_tok(XT, bXT, j, Wb, bW)
                            P.op("dve", lambda e: e.tensor_tensor(RVs[:, j, hb * 4:(hb + 1) * 4, :], pt[:, 0:512].rearrange("p (h e) -> p h e", h=4),
                                                                  kdec[:, hb * 4:(hb + 1) * 4].unsqueeze(2).to_broadcast([128, 4, 128]), ALU.mult),
                                 r=[b_dec], w=[bRV, bp])
                    for hb in range(2):
                        Wb, bW = load_w(O_MV + hb * 512)
                        for j in range(nch):
                            pt, bp = proj_tok(XT, bXT, j, Wb, bW)
                            P.op("act", lambda e: e.copy(MV[:, j, hb * 512:(hb + 1) * 512], pt[:, 0:512]), w=[bMV, bp])
                    for j in range(nch):
                        pt, bp = proj_tok(XT, bXT, j, wmg, b_wmg, width=16)
                        P.op("dve", lambda e: e.tensor_tensor(G[:, j, :], pt[:, 0:16], gb[:], ALU.add), r=[b_gb], w=[bG, bp])
                    spu, bspu = spur.next()
                    P.op("act", lambda e: e.activation(spu[:, 1, 0:nch, :], G[:, 0:nch, dr * 8 + 4:dr * 8 + 8], AF.Exp, scale=-1.0), r=[bG], w=[bspu])
                    P.op("act", lambda e: e.activation(spu[:, 0, 0:nch, :], spu[:, 1, 0:nch, :], AF.Ln, bias=1.0), w=[bspu])
                    fm_blocks = [(O_MK, 4)] + ([(O_MQ, 0)] if with_out else [])
                    nh = (NT + 2) // 2
                    for (off, b0) in fm_blocks:
                        Wb, bW = load_w(off)
                        for hb in range(4):
                            raw, braw = rawr.next()
                            for half in range(2):
                                pt, bp = psr.next()
                                for k in range(KC):
                                    P.op("pe", lambda e: e.matmul(pt[:, 0:nh], lhsT=Wb[:, k, hb * 128:(hb + 1) * 128], rhs=XT[:, k, half * nh:(half + 1) * nh],
                                                                  start=(k == 0), stop=(k == KC - 1)), r=[bXT, bW], w=[bp])
                                if half == 0:
                                    P.op("act", lambda e: e.copy(raw[:, 0:nh], pt[:, 0:nh]), w=[braw, bp])
                                else:
                                    P.op("dve", lambda e: e.tensor_copy(raw[:, nh:2 * nh], pt[:, 0:nh]), w=[braw, bp])
                            b = b0 + hb
                            ac, bac = accr.next()
                            P.op("act", lambda e: e.activation(ac[:, 0:NT], raw[:, 1:NT + 1], AF.Identity, scale=cw[:, 1, b:b + 1]), r=[braw, b_cw], w=[bac])
                            P.op("dve", lambda e: e.scalar_tensor_tensor(out=ac[:, 0:NT], in0=raw[:, 0:NT], scalar=cw[:, 0, b:b + 1], in1=ac[:, 0:NT],
                                                                         op0=ALU.mult, op1=ALU.add), r=[braw, b_cw], w=[bac])
                            P.op("dve", lambda e: e.scalar_tensor_tensor(out=ac[:, 0:NT], in0=raw[:, 2:NT + 2], scalar=cw[:, 2, b:b + 1], in1=ac[:, 0:NT],
                                                                         op0=ALU.mult, op1=ALU.add), r=[braw, b_cw], w=[bac])
                            P.op("act", lambda e: e.activation(MQK[:, b, 0:NT], ac[:, 0:NT], AF.Silu), r=[bac], w=[bMQK])
                    if with_out:
                        Wb, bW = load_w(O_RQ)
                        for j in range(nch):
                            pt, bp = proj_tok(XT, bXT, j, Wb, bW)
                            rotary(pt, bp, RQ, bRQ, j)
                        if dr == 1:
                            for (off, dstt, bdd, fn) in ((O_RG, RG, bRG, AF.Silu), (O_MO, MO, bMO, AF.Sigmoid)):
                                for hb in range(2):
                                    Wb, bW = load_w(off + hb * 512)
                                    for j in range(nch):
                                        pt, bp = proj_tok(XT, bXT, j, Wb, bW)
                                        P.op("act", lambda e: e.activation(dstt[:, j, hb * 512:(hb + 1) * 512], pt[:, 0:512], fn), w=[bdd, bp])
                        YOT, bYOT = YOTr.next()
                    order = range(nch) if dr == 0 else range(nch - 1, -1, -1)
                    pend_out = []
                    psM, psR, psO = RingView(psr.items[0:4]), RingView(psr.items[4:7]), RingView(psr.items[7:8])
                    psx = [psM]
                    for j in order:
                        tok0 = base + j * 128
                        if with_out:
                            Y, bY = Yr.next()
                            bYm = Ym_bufs[(Yr.i - 1) % len(Yr.items)]
                        lst_R, lst_M, lst_O = [], [], []
                        P.rec = lst_R
                        psx[0] = psR
                        if with_out:
                            QT, bQT = QTr.next()
                            KT, bKT = KTr.next()
                            for (srcq, bsq, dT, bdT, eng) in ((RQ, bRQ, QT, bQT, "act"), (RK, bRK, KT, bKT, "dve")):
                                pt, bp = psx[0].next()
                                ptb = pt[:].bitcast(BF16)
                                for h in range(8):
                                    P.op("pe", lambda e: e.transpose(ptb[0:64, h * 128:(h + 1) * 128], srcq[:, j, h * 64:(h + 1) * 64], identb[:]),
                                         r=[bsq, b_identb], w=[bp])
                                if eng == "act":
                                    P.op("act", lambda e: e.copy(dT[:].rearrange("p h c -> p (h c)"), ptb[0:64, :]), w=[bdT, bp])
                                else:
                                    P.op("dve", lambda e: e.tensor_copy(dT[:].rearrange("p h c -> p (h c)"), ptb[0:64, :]), w=[bdT, bp])
                            AT, bAT = ATr.next()
                            for hb in range(2):
                                pa, bpa = psx[0].next()
                                for hh in range(4):
                                    h = hb * 4 + hh
                                    P.op("pe", lambda e: e.matmul(pa[:, hh * 128:(hh + 1) * 128], lhsT=KT[:, h, :], rhs=QT[:, h, :], start=True, stop=True),
                                         r=[bKT, bQT], w=[bpa])
                                P.op("dve", lambda e: e.tensor_tensor(AT[:, hb * 4:(hb + 1) * 4, :], pa[:, 0:512].rearrange("p (h c) -> p h c", h=4),
                                                                      maskT.unsqueeze(1).to_broadcast([128, 4, 128]), ALU.mult), r=[b_cst], w=[bAT, bpa])
                            for hb in range(2):
                                py, bpy = psx[0].next()
                                for hh in range(4):
                                    h = hb * 4 + hh
                                    P.op("pe", lambda e: e.matmul(py[:, hh * 128:(hh + 1) * 128], lhsT=AT[:, h, :], rhs=RVs[:, j, h, :], start=True, stop=False),
                                         r=[bAT, bRV], w=[bpy])
                                    P.op("pe", lambda e: e.matmul(py[:, hh * 128:(hh + 1) * 128], lhsT=QT[:, h, :], rhs=Sbf[:, h, :], start=False, stop=True),
                                         r=[bQT, b_Sbf], w=[bpy])
                                P.op("dve", lambda e: e.tensor_tensor(Y[:, hb * 512:(hb + 1) * 512].rearrange("p (h e) -> p h e", h=4),
                                                                      py[:, 0:512].rearrange("p (h e) -> p h e", h=4),
                                                                      qdec[:, hb * 4:(hb + 1) * 4].unsqueeze(2).to_broadcast([128, 4, 128]), ALU.mult),
                                     r=[b_dec], w=[bY, bpy])
                        for hb in range(2):
                            pS, bpS = psx[0].next()
                            for hh in range(4):
                                h = hb * 4 + hh
                                P.op("pe", lambda e: e.matmul(pS[0:64, hh * 128:(hh + 1) * 128], lhsT=RK[:, j, h * 64:(h + 1) * 64], rhs=RVs[:, j, h, :],
                                                              start=True, stop=True), r=[bRK, bRV], w=[bpS])
                            sv = S32[:, hb * 4:(hb + 1) * 4, :]
                            P.op("dve", lambda e: e.tensor_tensor(sv, sv, pS[0:64, 0:512].rearrange("p (h e) -> p h e", h=4), ALU.add), w=[b_S32, bpS])
                            P.op("pool", lambda e: e.tensor_tensor(sv, sv, cdec[0:64, hb * 4:(hb + 1) * 4].unsqueeze(2).to_broadcast([64, 4, 128]), ALU.mult),
                                 r=[b_dec], w=[b_S32])
                            P.op("act", lambda e: e.copy(Sbf[:, hb * 4:(hb + 1) * 4, :], sv), r=[b_S32], w=[b_Sbf])
                        P.rec = lst_M
                        psx[0] = psM
                        gs, bgs = gsr.next()
                        zi = G[:, j, dr * 8:dr * 8 + 4]
                        zf = G[:, j, dr * 8 + 4:dr * 8 + 8]
                        pg, bpg = psx[0].next()
                        P.op("pe", lambda e: e.matmul(pg[:, 0:4], lhsT=tri[dr], rhs=spu[:, 0, j, :], start=True, stop=True), r=[bspu, b_cst], w=[bpg])
                        P.op("pe", lambda e: e.matmul(pg[:, 4:8], lhsT=onesf, rhs=spu[:, 0, j, :], start=True, stop=True), r=[bspu, b_cst], w=[bpg])
                        cs = pg[:, 0:4]
                        tot = pg[:, 4:8]
                        P.op("dve", lambda e: e.tensor_tensor(gs[:, 2, :], zi, cs, ALU.add), r=[bG], w=[bgs, bpg])
                        Dg, bDg = Dr.next()
                        P.op("dve", lambda e: e.tensor_tensor(Dg[:], identf.unsqueeze(1).to_broadcast([128, 4, 128]),
                                                              gs[:, 2, :].unsqueeze(2).to_broadcast([128, 4, 128]), ALU.mult), r=[b_cst, bgs], w=[bDg])
                        pu, bpu = psx[0].next()
                        P.op("pe", lambda e: e.matmul(pu[:, 0:512], lhsT=onesf, rhs=Dg[:].rearrange("p h s -> p (h s)"), start=True, stop=True),
                             r=[bDg, b_cst], w=[bpu])
                        puv = pu[:, 0:512].rearrange("p (h s) -> p h s", h=4)
                        P.op("dve", lambda e: e.tensor_reduce(out=gs[:, 5, :], in_=puv, axis=AX.X, op=ALU.max), w=[bgs, bpu])
                        Um, bUm = Umr.next()
                        P.op("dve", lambda e: e.tensor_tensor(Um[:], puv, negm[dr].unsqueeze(1).to_broadcast([128, 4, 128]), ALU.add), r=[b_cst], w=[bUm, bpu])
                        P.op("dve", lambda e: e.tensor_reduce(out=gs[:, 6, :], in_=Um[:], axis=AX.X, op=ALU.max), r=[bUm], w=[bgs])
                        P.op("dve", lambda e: e.tensor_tensor(gs[:, 3, :], gs[:, 6, :], mprev[:], ALU.max), r=[b_mprev], w=[bgs])
                        P.op("dve", lambda e: e.tensor_tensor(gs[:, 4, :], gs[:, 5, :], mprev[:], ALU.max), r=[b_mprev], w=[bgs])
                        P.op("dve", lambda e: e.tensor_tensor(gs[:, 11, :], gs[:, 2, :], gs[:, 4, :], ALU.subtract), w=[bgs])
                        P.op("act", lambda e: e.activation(gs[:, 7, :], gs[:, 11, :], AF.Exp, bias=-0.5 * math.log(128.0)), w=[bgs])
                        P.op("dve", lambda e: e.tensor_tensor(gs[:, 12, :], gs[:, 4, :], gs[:, 3, :], ALU.subtract), w=[bgs])
                        P.op("act", lambda e: e.activation(gs[:, 8, :], gs[:, 12, :], AF.Exp), w=[bgs])
                        P.op("dve", lambda e: e.tensor_tensor(gs[:, 13, :], mprev[:], gs[:, 4, :], ALU.subtract), r=[b_mprev], w=[bgs])
                        P.op("act", lambda e: e.activation(gs[:, 9, :], gs[:, 13, :], AF.Exp), w=[bgs])
                        P.op("dve", lambda e: e.tensor_tensor(gs[:, 14, :], cs, gs[:, 3, :], ALU.subtract), w=[bgs, bpg])
                        P.op("act", lambda e: e.activation(gs[:, 10, :], gs[:, 14, :], AF.Exp), w=[bgs])
                        P.op("dve", lambda e: e.tensor_tensor(mprev[:], gs[:, 4, :], tot, ALU.subtract), r=[bgs], w=[b_mprev, bpg])
                        Vm, bVm = Vmr.next()
                        P.op("dve", lambda e: e.tensor_tensor(Vm[:, :, 0:256], MV[:, j, :].rearrange("p (h e) -> p h e", h=4),
                                                              gs[:, 7, :].unsqueeze(2).to_broadcast([128, 4, 256]), ALU.mult), r=[bMV, bgs], w=[bVm])
                        P.op("pool", lambda e: e.tensor_copy(Vm[:, :, 256:257], gs[:, 7, :].unsqueeze(2)), r=[bgs], w=[bVm])
                        P.op("pool", lambda e: e.tensor_tensor(C32[:], C32[:], gs[:, 9, :].unsqueeze(2).to_broadcast([128, 4, 257]), ALU.mult), r=[bgs], w=[b_C32])
                        P.op("act", lambda e: e.copy(Cbf[:], C32[:]), r=[b_C32], w=[b_Cbf])
                        MKt, bMKt = MKtr.next()
                        pt, bp = psx[0].next()
                        ptb = pt[:].bitcast(BF16)
                        for h in range(4):
                            P.op("pe", lambda e: e.transpose(ptb[:, h * 128:(h + 1) * 128], MQK[:, 4 + h, j * 128:(j + 1) * 128], identb[:]),
                                 r=[bMQK, b_identb], w=[bp])
                        P.op("act", lambda e: e.copy(MKt[:].rearrange("p h d -> p (h d)"), ptb[:, 0:512]), w=[bMKt, bp])
                        if with_out:
                            ATm, bATm = ATmr.next()
                            pa, bpa = psx[0].next()
                            for h in range(4):
                                P.op("pe", lambda e: e.matmul(pa[:, h * 128:(h + 1) * 128], lhsT=MQK[:, 4 + h, j * 128:(j + 1) * 128],
                                                              rhs=MQK[:, h, j * 128:(j + 1) * 128], start=True, stop=True), r=[bMQK], w=[bpa])
                            P.op("dve", lambda e: e.tensor_tensor(ATm[:], pa[:, 0:512].rearrange("p (h c) -> p h c", h=4),
                                                                  maskT.unsqueeze(1).to_broadcast([128, 4, 128]), ALU.mult), r=[b_cst], w=[bATm, bpa])
                            dn, bdn = dnr.next()
                            for h in range(4):
                                ph_, bph = psx[0].next()
                                P.op("pe", lambda e: e.matmul(ph_[:, 0:257], lhsT=ATm[:, h, :], rhs=Vm[:, h, :], start=True, stop=False), r=[bATm, bVm], w=[bph])
                                P.op("pe", lambda e: e.matmul(ph_[:, 0:257], lhsT=MQK[:, h, j * 128:(j + 1) * 128], rhs=Cbf[:, h, :], start=False, stop=True),
                                     r=[bMQK, b_Cbf], w=[bph])
                                P.op("act", lambda e: e.activation(dn[:, h, 0:1], ph_[:, 256:257], AF.Abs), w=[bdn, bph])
                                P.op("dve", lambda e: e.tensor_scalar(dn[:, h, 1:2], dn[:, h, 0:1], gs[:, 8, h:h + 1], gs[:, 10, h:h + 1], op0=ALU.mult, op1=ALU.max),
                                     r=[bgs], w=[bdn])
                                P.op("dve", lambda e: e.reciprocal(dn[:, h, 2:3], dn[:, h, 1:2]), w=[bdn])
                                P.op("dve", lambda e: e.tensor_tensor(dn[:, h, 3:4], dn[:, h, 2:3], gs[:, 8, h:h + 1], ALU.mult), r=[bgs], w=[bdn])
                                P.op("act", lambda e: e.activation(Y[:, 1024 + h * 256:1024 + (h + 1) * 256], ph_[:, 0:256], AF.Identity, scale=dn[:, h, 3:4]),
                                     r=[bdn], w=[bYm, bph])
                        for h in range(4):
                            pc, bpc = psx[0].next()
                            P.op("pe", lambda e: e.matmul(pc[:, 0:257], lhsT=MKt[:, h, :], rhs=Vm[:, h, :], start=True, stop=True), r=[bMKt, bVm], w=[bpc])
                            P.op("dve", lambda e: e.tensor_tensor(C32[:, h, :], C32[:, h, :], pc[:, 0:257], ALU.add), w=[b_C32, bpc])
                        P.rec = lst_O
                        psx[0] = psO
                        if with_out and dr == 0:
                            P.dma(yA_d[tok0:tok0 + 128, :], Y[:], r=[bY, bYm], w=[B_yA], sb=bY)
                        if with_out and dr == 1:
                            YA, bYA = YAr.next()
                            P.dma(YA[:], yA_d[tok0:tok0 + 128, :], r=[B_yA], w=[bYA], sb=bYA)
                            P.op("dve", lambda e: e.tensor_tensor(Y[:], Y[:], YA[:], ALU.add), r=[bYA], w=[bY, bYm])
                            ns, bns = nsr.next()
                            hn, bhn = Y, bY
                            sq, bsq = sqr.next()
                            yo, byo = yor.next()
                            for (gi, c0, ng, gw) in ((0, 0, 8, 128), (1, 1024, 4, 256)):
                                yv = Y[:, c0:c0 + 1024].rearrange("p (g e) -> p g e", g=ng)
                                hv = hn[:, c0:c0 + 1024].rearrange("p (g e) -> p g e", g=ng)
                                sv2 = sq[:, c0:c0 + 1024].rearrange("p (g e) -> p g e", g=ng)
                                st_ = ns[:, gi * 2:gi * 2 + 2, :]
                                P.op("dve", lambda e: e.tensor_reduce(out=st_[:, 0, 0:ng], in_=yv, axis=AX.X, op=ALU.add), r=[bY], w=[bns])
                                P.op("dve", lambda e: e.tensor_scalar(st_[:, 0, 0:ng], st_[:, 0, 0:ng], 1.0 / gw, None, op0=ALU.mult), w=[bns])
                                P.op("dve", lambda e: e.tensor_tensor(hv, yv, st_[:, 0, 0:ng].unsqueeze(2).to_broadcast([128, ng, gw]), ALU.subtract),
                                     r=[bY, bns], w=[bhn])
                                P.op("pool", lambda e: e.tensor_tensor(sv2, hv, hv, ALU.mult), r=[bhn], w=[bsq])
                                P.op("dve", lambda e: e.tensor_reduce(out=st_[:, 1, 0:ng], in_=sv2, axis=AX.X, op=ALU.add), r=[bsq], w=[bns])
                                P.op("dve", lambda e: e.tensor_scalar(st_[:, 1, 0:ng], st_[:, 1, 0:ng], 1.0 / gw, EPS, op0=ALU.mult, op1=ALU.add), w=[bns])
                                P.op("pool", lambda e: e.tensor_tensor(st_[:, 1, 0:ng], st_[:, 1, 0:ng], mhalf[:, 0:ng], ALU.pow), r=[b_mhalf], w=[bns])
                                P.op("dve", lambda e: e.tensor_tensor(hv, hv, st_[:, 1, 0:ng].unsqueeze(2).to_broadcast([128, ng, gw]), ALU.mult),
                                     r=[bns], w=[bhn])
                                gate, bgate = (RG, bRG) if gi == 0 else (MO, bMO)
                                P.op("pool", lambda e: e.tensor_tensor(yo[:, c0:c0 + 1024], hn[:, c0:c0 + 1024], gate[:, j, :], ALU.mult),
                                     r=[bhn, bgate, bYm], w=[byo])
                            for half in range(2):
                                pt, bp = psx[0].next()
                                ptb = pt[:].bitcast(BF16)
                                for kk in range(8):
                                    k = half * 8 + kk
                                    P.op("pe", lambda e: e.transpose(ptb[:, kk * 128:(kk + 1) * 128], yo[:, k * 128:(k + 1) * 128], identb[:]),
                                         r=[byo, b_identb], w=[bp])
                                ov = YOT[:, half * 8:(half + 1) * 8, j * 128:(j + 1) * 128]
                                iv = ptb[:, 0:1024].rearrange("p (k t) -> p k t", k=8)
                                if half == 0:
                                    P.op("act", lambda e: e.copy(ov, iv), w=[bYOT, bp])
                                else:
                                    P.op("dve", lambda e: e.tensor_copy(ov, iv), w=[bYOT, bp])
                        P.rec = None
                        psx[0] = psM
                        P.zip_emit([lst_M, lst_R, pend_out])
                        pend_out = lst_O
                    P.zip_emit([pend_out])
                    if with_out and dr == 1:
                        P.dma(yoT_d.rearrange("(k p) n -> p k n", p=128)[:, :, base:base + NT], YOT[:, :, 0:NT], r=[bYOT], w=[B_yoT], sb=bYOT)

            scan_pass(0)
            scan_pass(1)
        barrier()

        with ExitStack() as ph:
            gt1b, b_gt1b = sbt(ph, "gt1b", [128, D], F32)
            P.dma(gt1b[:], ada_d[0:1, 2 * D:3 * D].partition_broadcast(128), r=[B_ada], w=[b_gt1b], sb=b_gt1b)
            XTr = Ring(nc, ph, "cXT", [128, KC, 512], BF16, 1)
            YTr = Ring(nc, ph, "cYT", [128, KC, 512], BF16, 1)
            MTr = Ring(nc, ph, "cMT", [128, KC, 512], BF16, 1, multi=True)
            Wr = Ring(nc, ph, "cW", [128, KC, 512], BF16, 3)
            sgr = Ring(nc, ph, "sg", [128, 512], F32, 2)
            t1r = Ring(nc, ph, "t1", [128, 512], F32, 2)
            t2r = Ring(nc, ph, "t2", [128, 512], F32, 2)
            xr = Ring(nc, ph, "cx", [128, D], F32, 4, multi=True)
            for s in range(NST):
                base = s * 512
                XT, bXT = XTr.next()
                YT, bYT = YTr.next()
                MT, bMT = MTr.next()
                P.dma(XT[:], xmT_d.rearrange("(k p) n -> p k n", p=128)[:, :, 1 + base:1 + base + 512], r=[B_xmT], w=[bXT], sb=bXT)
                P.dma(YT[:], yoT_d.rearrange("(k p) n -> p k n", p=128)[:, :, base:base + 512], r=[B_yoT], w=[bYT], sb=bYT)
                xs = []
                for j in range(4):
                    xt, bx = xr.next()
                    P.dma(xt[:], x_d[base + j * 128:base + (j + 1) * 128, :], w=[bx], sb=bx)
                    xs.append((xt, bx))
                for cb in range(4):
                    Wgr, bWgr = Wr.next()
                    P.dma(Wgr[:], winb_d[:, O_GR + cb * 512:O_GR + (cb + 1) * 512].rearrange("(k p) n -> p k n", p=128), r=[B_winb], w=[bWgr], sb=bWgr)
                    Wgm, bWgm = Wr.next()
                    P.dma(Wgm[:], winb_d[:, O_GM + cb * 512:O_GM + (cb + 1) * 512].rearrange("(k p) n -> p k n", p=128), r=[B_winb], w=[bWgm], sb=bWgm)
                    Wo, bWo = Wr.next()
                    P.dma(Wo[:, 0:8, :], wrob_d[:, cb * 512:(cb + 1) * 512].rearrange("(k p) n -> p k n", p=128), r=[B_wrob], w=[bWo], sb=bWo)
                    P.dma(Wo[:, 8:16, :], wmob_d[:, cb * 512:(cb + 1) * 512].rearrange("(k p) n -> p k n", p=128), r=[B_wmob], w=[bWo], sb=bWo)
                    for m in range(4):
                        db = cb * 4 + m
                        res = []
                        for (Wg, bWg, k0, tr_) in ((Wgr, bWgr, 0, t1r), (Wgm, bWgm, 8, t2r)):
                            pg_, bpg_ = psr.next()
                            for k in range(KC):
                                P.op("pe", lambda e: e.matmul(pg_[:, 0:512], lhsT=Wg[:, k, m * 128:(m + 1) * 128], rhs=XT[:, k, :],
                                                              start=(k == 0), stop=(k == KC - 1)), r=[bWg, bXT], w=[bpg_])
                            sg, bsg = sgr.next()
                            P.op("act", lambda e: e.activation(sg[:], pg_[:, 0:512], AF.Sigmoid), w=[bsg, bpg_])
                            pp, bpp = psr.next()
                            for k in range(8):
                                P.op("pe", lambda e: e.matmul(pp[:, 0:512], lhsT=Wo[:, k0 + k, m * 128:(m + 1) * 128], rhs=YT[:, k0 + k, :],
                                                              start=(k == 0), stop=(k == 7)), r=[bWo, bYT], w=[bpp])
                            tt, btt = tr_.next()
                            P.op("dve", lambda e: e.tensor_tensor(tt[:], pp[:, 0:512], sg[:], ALU.mult), r=[bsg], w=[btt, bpp])
                            res.append((tt, btt))
                        P.op("pool", lambda e: e.tensor_tensor(MT[:, db, :], res[0][0][:], res[1][0][:], ALU.add), r=[res[0][1], res[1][1]], w=[bMT])
                for cb in range(4):
                    Wo, bWo = Wr.next()
                    P.dma(Wo[:], woutb_d[:, cb * 512:(cb + 1) * 512].rearrange("(k p) n -> p k n", p=128), r=[B_woutb], w=[bWo], sb=bWo)
                    for j in range(4):
                        po, bpo = psr.next()
                        for k in range(KC):
                            P.op("pe", lambda e: e.matmul(po[:, 0:512], lhsT=MT[:, k, j * 128:(j + 1) * 128], rhs=Wo[:, k, :],
                                                          start=(k == 0), stop=(k == KC - 1)), r=[bMT, bWo], w=[bpo])
                        tt, btt = t1r.next()
                        P.op("dve", lambda e: e.tensor_tensor(tt[:], po[:, 0:512], gt1b[:, cb * 512:(cb + 1) * 512], ALU.mult), r=[b_gt1b], w=[btt, bpo])
                        xt, bx = xs[j]
                        P.op("pool", lambda e: e.tensor_tensor(xt[:, cb * 512:(cb + 1) * 512], xt[:, cb * 512:(cb + 1) * 512], tt[:], ALU.add), r=[btt], w=[bx])
                for j in range(4):
                    xt, bx = xs[j]
                    P.dma(x1_d[base + j * 128:base + (j + 1) * 128, :], xt[:], r=[bx], w=[B_x1], sb=bx)
        barrier()

        with ExitStack() as ph:
            Wq, b_Wq = sbt(ph, "Wq", [128, KC, D], BF16)
            for cb in range(4):
                P.dma(Wq[:, :, cb * 512:(cb + 1) * 512], pqb_d[:, cb * 512:(cb + 1) * 512].rearrange("(k p) n -> p k n", p=128), r=[B_pqb], w=[b_Wq], sb=b_Wq)
            keysT, b_keysT = sbt(ph, "keysT", [128, 16, 128], BF16)
            with ExitStack() as ph2:
                kf, b_kf = sbt(ph2, "kf", [128, 16, 128], F32)
                P.dma(kf[:], pk_d.rearrange("(g k) d -> k g d", k=128), w=[b_kf], sb=b_kf)
                for g4 in range(4):
                    pt, bp = psr.next()
                    for gg in range(4):
                        g = g4 * 4 + gg
                        P.op("pe", lambda e: e.transpose(pt[:, gg * 128:(gg + 1) * 128], kf[:, g, :], identf), r=[b_kf, b_cst], w=[bp])
                    P.op("dve", lambda e: e.tensor_copy(keysT[:, g4 * 4:(g4 + 1) * 4, :], pt[:, 0:512].rearrange("p (g k) -> p g k", g=4)), w=[b_keysT, bp])
            barrier()
            gt2b, b_gt2b = sbt(ph, "gt2b", [128, D], F32)
            fgb, b_fgb = sbt(ph, "fgb", [128, D], F32)
            P.dma(gt2b[:], ada_d[0:1, 5 * D:6 * D].partition_broadcast(128), r=[B_ada], w=[b_gt2b], sb=b_gt2b)
            P.dma(fgb[:], fg_d.partition_broadcast(128), w=[b_fgb], sb=b_fgb)
            iota16, b_iota = sbt(ph, "iota16", [128, 16], F32)
            for i in range(16):
                P.op("dve", lambda e: e.memset(iota16[:, i:i + 1], float(i)), w=[b_iota])
            x1r = Ring(nc, ph, "px1", [128, D], F32, 1)
            hnbr = Ring(nc, ph, "phn", [128, D], BF16, 1)
            hbr = Ring(nc, ph, "phb", [128, D], BF16, 2, multi=True)
            hTr = Ring(nc, ph, "phT", [128, KC, 128], BF16, 1, multi=True)
            qTr = Ring(nc, ph, "pqT", [128, 16, 128], BF16, 1, multi=True)
            Scr = Ring(nc, ph, "pSc", [128, 16, 128], F32, 1, multi=True)
            wkr = Ring(nc, ph, "pwk", [128, 16, 128], F32, 1)
            mxr = Ring(nc, ph, "pmx", [128, 16, 16], F32, 1)
            mir = Ring(nc, ph, "pmi", [128, 16, 16], U32, 1)
            mifr = Ring(nc, ph, "pmif", [128, 16, 16], F32, 1)
            bsr = Ring(nc, ph, "pbs", [128, 8, 16], F32, 1)
            bjr = Ring(nc, ph, "pbj", [128, 8, 16], U32, 1)
            ijr = Ring(nc, ph, "pij", [128, 4, 8, 16], F32, 1)
            iju = Ring(nc, ph, "piju", [128, 2, 8, 16], U32, 1)
            eqr = Ring(nc, ph, "peq", [128, 4, 16, 16], F32, 1)
            eidr = Ring(nc, ph, "peid", [128, 128], I32, 2)
            gwr = Ring(nc, ph, "pgw", [128, 2, 128], F32, 2)
            avr = Ring(nc, ph, "pav", [128, 3, 128], F32, 2, multi=True)
            Gr_ = Ring(nc, ph, "pG", [128, 2 * D], BF16, 6)
            jkr = Ring(nc, ph, "pjk", [128, D], BF16, 1)
            dgr = Ring(nc, ph, "pdg", [128, 128], BF16, 4)
            str_ = Ring(nc, ph, "pst", [128, 4], F32, 4)
            pacc = [psr.items[4 + i] for i in range(4)]
            psr.items = psr.items[0:4]
            psr.i = 0

            def rms_rstd(src, bsrc):
                st, bs = str_.next()
                jk, bjk = jkr.next()
                P.op("dve", lambda e: e.scalar_tensor_tensor(out=jk[:], in0=src[:], scalar=1.0, in1=src[:], op0=ALU.mult, op1=ALU.mult, accum_out=st[:, 0:1]),
                     r=[bsrc], w=[bjk, bs])
                P.op("dve", lambda e: e.tensor_scalar(st[:, 1:2], st[:, 0:1], 1.0 / D, EPS, op0=ALU.mult, op1=ALU.add), w=[bs])
                P.op("pool", lambda e: e.tensor_tensor(st[:, 3:4], st[:, 1:2], mhalf[:, 0:1], ALU.pow), r=[b_mhalf], w=[bs])
                return st, bs

            def stage1(blk, res):
                x1, bx1 = x1r.next()
                P.dma(x1[:], x1_d[blk * 128:(blk + 1) * 128, :], r=[B_x1], w=[bx1], sb=bx1)
                st, bs = rms_rstd(x1, bx1)
                yield
                hnb, bhnb = hnbr.next()
                P.op("act", lambda e: e.activation(hnb[:], x1[:], AF.Identity, scale=st[:, 3:4]), r=[bx1, bs], w=[bhnb])
                yield
                hT, bhT = hTr.next()
                for half in range(2):
                    pt, bp = psr.next()
                    ptb = pt[:].bitcast(BF16)
                    for kk in range(8):
                        k = half * 8 + kk
                        P.op("pe", lambda e: e.transpose(ptb[:, kk * 128:(kk + 1) * 128], hnb[:, k * 128:(k + 1) * 128], identb[:]), r=[bhnb, b_identb], w=[bp])
                    for kk in range(8):
                        k = half * 8 + kk
                        if half == 0:
                            P.op("act", lambda e: e.activation(hT[:, k, :], ptb[:, kk * 128:(kk + 1) * 128], AF.Identity, bias=modF[:, 5, k:k + 1], scale=modF[:, 4, k:k + 1]),
                                 r=[b_modF], w=[bhT, bp])
                        else:
                            P.op("dve", lambda e: e.tensor_scalar(hT[:, k, :], ptb[:, kk * 128:(kk + 1) * 128], modF[:, 4, k:k + 1], modF[:, 5, k:k + 1], op0=ALU.mult, op1=ALU.add),
                                 r=[b_modF], w=[bhT, bp])
                    yield
                hb, bhb = hbr.next()
                for half in range(2):
                    pt, bp = psr.next()
                    ptb = pt[:].bitcast(BF16)
                    for kk in range(8):
                        k = half * 8 + kk
                        P.op("pe", lambda e: e.transpose(ptb[:, kk * 128:(kk + 1) * 128], hT[:, k, :], identb[:]), r=[bhT, b_identb], w=[bp])
                    P.op("act", lambda e: e.copy(hb[:, half * 1024:(half + 1) * 1024], ptb[:, 0:1024]), w=[bhb, bp])
                    yield
                qT, bqT = qTr.next()
                for g4 in range(4):
                    pt, bp = psr.next()
                    for gg in range(4):
                        g = g4 * 4 + gg
                        for k in range(KC):
                            P.op("pe", lambda e: e.matmul(pt[:, gg * 128:(gg + 1) * 128], lhsT=Wq[:, k, g * 128:(g + 1) * 128], rhs=hT[:, k, :],
                                                          start=(k == 0), stop=(k == KC - 1)), r=[b_Wq, bhT], w=[bp])
                    P.op("act", lambda e: e.copy(qT[:, g4 * 4:(g4 + 1) * 4, :], pt[:, 0:512].rearrange("p (g t) -> p g t", g=4)), w=[bqT, bp])
                    yield
                Sc, bSc = Scr.next()
                for g4 in range(4):
                    pt, bp = psr.next()
                    for gg in range(4):
                        g = g4 * 4 + gg
                        P.op("pe", lambda e: e.matmul(pt[:, gg * 128:(gg + 1) * 128], lhsT=qT[:, g, :], rhs=keysT[:, g, :], start=True, stop=True),
                             r=[bqT, b_keysT], w=[bp])
                    P.op("act", lambda e: e.copy(Sc[:, g4 * 4:(g4 + 1) * 4, :], pt[:, 0:512].rearrange("p (g t) -> p g t", g=4)), w=[bSc, bp])
                    yield
                mx, bmx = mxr.next()
                mi, bmi = mir.next()
                wk, bwk = wkr.next()
                for g in range(16):
                    P.op("dve", lambda e: e.max(out=mx[:, g, 0:8], in_=Sc[:, g, :]), r=[bSc], w=[bmx])
                    yield
                    P.op("dve", lambda e: e.max_index(out=mi[:, g, 0:8], in_max=mx[:, g, 0:8], in_values=Sc[:, g, :]), r=[bSc, bmx], w=[bmi])
                    yield
                    P.op("dve", lambda e: e.match_replace(out=wk[:, g, :], in_to_replace=mx[:, g, 0:8], in_values=Sc[:, g, :], imm_value=NEG), r=[bSc, bmx], w=[bwk])
                    yield
                    P.op("dve", lambda e: e.max(out=mx[:, g, 8:16], in_=wk[:, g, :]), r=[bwk], w=[bmx])
                    yield
                    P.op("dve", lambda e: e.max_index(out=mi[:, g, 8:16], in_max=mx[:, g, 8:16], in_values=wk[:, g, :]), r=[bwk, bmx], w=[bmi])
                    yield
                    yield
                mif, bmif = mifr.next()
                P.op("dve", lambda e: e.tensor_copy(mif[:], mi[:]), r=[bmi], w=[bmif])
                cand_t, bcand = wkr.next()
                cand = cand_t[:].rearrange("p (h a) k -> p h (a k)", a=2)
                mxv = mx[:].rearrange("p (h t) i -> p h t i", t=2)
                P.op("pool", lambda e: e.tensor_tensor(cand.rearrange("p h (i j) -> p h i j", i=16),
                                                      mxv[:, :, 0, :].unsqueeze(3).to_broadcast([128, 8, 16, 16]),
                                                      mxv[:, :, 1, :].unsqueeze(2).to_broadcast([128, 8, 16, 16]), ALU.add), r=[bmx], w=[bcand])
                yield
                bs_, bbs = bsr.next()
                bj, bbj = bjr.next()
                cw2_t, bcw2 = Scr.next()
                cw2 = cw2_t[:].rearrange("p (h a) k -> p h (a k)", a=2)
                for h in range(8):
                    P.op("dve", lambda e: e.max(out=bs_[:, h, 0:8], in_=cand[:, h, :]), r=[bcand], w=[bbs])
                    yield
                    P.op("dve", lambda e: e.max_index(out=bj[:, h, 0:8], in_max=bs_[:, h, 0:8], in_values=cand[:, h, :]), r=[bcand, bbs], w=[bbj])
                    yield
                    P.op("dve", lambda e: e.match_replace(out=cw2[:, h, :], in_to_replace=bs_[:, h, 0:8], in_values=cand[:, h, :], imm_value=NEG), r=[bcand, bbs], w=[bcw2])
                    yield
                    P.op("dve", lambda e: e.max(out=bs_[:, h, 8:16], in_=cw2[:, h, :]), r=[bcw2], w=[bbs])
                    yield
                    P.op("dve", lambda e: e.max_index(out=bj[:, h, 8:16], in_max=bs_[:, h, 8:16], in_values=cw2[:, h, :]), r=[bcw2, bbs], w=[bbj])
                    yield
                    yield
                ju, bju = iju.next()
                ij, bij = ijr.next()
                P.op("dve", lambda e: e.tensor_single_scalar(ju[:, 0], bj[:], 4, op=ALU.logical_shift_right), r=[bbj], w=[bju])
                P.op("dve", lambda e: e.tensor_single_scalar(ju[:, 1], bj[:], 15, op=ALU.bitwise_and), r=[bbj], w=[bju])
                P.op("dve", lambda e: e.tensor_copy(ij[:, 0:2], ju[:]), r=[bju], w=[bij])
                mifv = mif[:].rearrange("p (h t) i -> p h t i", t=2)
                for t in range(2):
                    for hh in range(2):
                        hs = slice(hh * 4, (hh + 1) * 4)
                        eq, beq = eqr.next()
                        P.op("dve", lambda e: e.tensor_tensor(eq[:], ij[:, t, hs].unsqueeze(3).to_broadcast([128, 4, 16, 16]),
                                                               iota16[:].unsqueeze(1).unsqueeze(1).to_broadcast([128, 4, 16, 16]), ALU.is_equal), r=[bij, b_iota], w=[beq])
                        P.op("pool", lambda e: e.tensor_tensor(eq[:], eq[:], mifv[:, hs, t, :].unsqueeze(2).to_broadcast([128, 4, 16, 16]), ALU.mult), r=[bmif], w=[beq])
                        P.op("dve", lambda e: e.tensor_reduce(out=ij[:, 2 + t, hs], in_=eq[:], axis=AX.X, op=ALU.add), r=[beq], w=[bij])
                        yield
                P.op("dve", lambda e: e.scalar_tensor_tensor(out=ij[:, 0], in0=ij[:, 2], scalar=128.0, in1=ij[:, 3], op0=ALU.mult, op1=ALU.add), w=[bij])
                eid, beid = eidr.next()
                P.op("dve", lambda e: e.tensor_copy(eid[:], ij[:, 0].rearrange("p h k -> p (h k)")), r=[bij], w=[beid])
                yield
                gw, bgw = gwr.next()
                gwv = gw[:, 0, :].rearrange("p (h k) -> p h k", h=8)
                P.op("dve", lambda e: e.tensor_tensor(gwv, bs_[:], bs_[:, :, 0:1].to_broadcast([128, 8, 16]), ALU.subtract), r=[bbs], w=[bgw])
                P.op("act", lambda e: e.activation(gw[:, 0, :], gw[:, 0, :], AF.Exp), w=[bgw])
                P.op("dve", lambda e: e.tensor_reduce(out=gw[:, 1, 0:8], in_=gwv, axis=AX.X, op=ALU.add), w=[bgw])
                P.op("dve", lambda e: e.reciprocal(gw[:, 1, 0:8], gw[:, 1, 0:8]), w=[bgw])
                P.op("dve", lambda e: e.tensor_tensor(gwv, gwv, gw[:, 1, 0:8].unsqueeze(2).to_broadcast([128, 8, 16]), ALU.mult), w=[bgw])
                res.update(hb=hb, bhb=bhb, eid=eid, beid=beid, gw=gw, bgw=bgw)

            def stage2(blk, s_, bg=None):
                hb, bhb, eid, beid, gw, bgw = (s_[k_] for k_ in ("hb", "bhb", "eid", "beid", "gw", "bgw"))
                av, bav = avr.next()
                jk, bjk = jkr.next()
                pend = None
                bsl = [Buf("sl%d" % i_) for i_ in range(128)]
                for b__ in bsl:
                    b__.w = dict(bav.base)

                def tail(slot, Gt, bGt):
                    P.op("dve", lambda e: e.tensor_tensor(av[:, 2, slot:slot + 1], av[:, 1, slot:slot + 1], gw[:, 0, slot:slot + 1], ALU.mult), r=[bsl[slot], bgw], w=[bsl[slot]])
                    dg, bdg = dgr.next()
                    P.op("act", lambda e: e.activation(dg[:], identb[:], AF.Identity, scale=av[:, 2, slot:slot + 1]), r=[b_identb, bsl[slot]], w=[bdg])
                    for cb in range(4):
                        po, bpo = pacc[cb]
                        P.op("pe", lambda e: e.matmul(po[:, 0:512], lhsT=dg[:], rhs=Gt[:, D + cb * 512:D + (cb + 1) * 512], start=(slot == 0), stop=(slot == 127)),
                             r=[bdg, bGt], w=[bpo])

                for slot in range(128):
                    Gt, bGt = Gr_.next()
                    P.gather(Gt[:], pcomb_d, eid[:, slot:slot + 1].bitcast(U32), r=[beid, B_pdnb, B_pupb], w=[bGt], sb=bGt)
                    P.op("dve", lambda e: e.scalar_tensor_tensor(out=jk[:], in0=Gt[:, 0:D], scalar=1.0, in1=hb[:], op0=ALU.mult, op1=ALU.mult,
                                                                 accum_out=av[:, 0, slot:slot + 1]), r=[bGt, bhb], w=[bsl[slot]])
                    P.op("act", lambda e: e.activation(av[:, 1, slot:slot + 1], av[:, 0, slot:slot + 1], AF.Gelu), r=[bsl[slot]], w=[bsl[slot]])
                    if pend is not None:
                        tail(*pend)
                    pend = (slot, Gt, bGt)
                    if bg is not None and slot >= 4:
                        next(bg, None)
                        next(bg, None)
                tail(*pend)
                if bg is not None:
                    for _ in bg:
                        pass
                for b__ in bsl:
                    for k__, v__ in list(b__.w.items()) + list(b__.r.items()):
                        if bav.w.get(k__, 0) < v__:
                            bav.w[k__] = v__
                tmG, btm = Gr_.next()
                tm = tmG[:].bitcast(F32)
                x1G, bx1 = Gr_.next()
                x1 = x1G[:].bitcast(F32)
                P.dma(x1, x1_d[blk * 128:(blk + 1) * 128, :], r=[B_x1], w=[bx1], sb=bx1)
                for cb in range(4):
                    po, bpo = pacc[cb]
                    P.op("dve", lambda e: e.tensor_tensor(tm[:, cb * 512:(cb + 1) * 512], po[:, 0:512], gt2b[:, cb * 512:(cb + 1) * 512], ALU.mult), r=[b_gt2b], w=[btm, bpo])
                P.op("pool", lambda e: e.tensor_tensor(x1, x1, tm, ALU.add), r=[btm], w=[bx1])
                st2, bs2 = str_.next()
                jk, bjk = jkr.next()
                P.op("dve", lambda e: e.scalar_tensor_tensor(out=jk[:], in0=x1, scalar=1.0, in1=x1, op0=ALU.mult, op1=ALU.mult, accum_out=st2[:, 0:1]),
                     r=[bx1], w=[bjk, bs2])
                P.op("dve", lambda e: e.tensor_scalar(st2[:, 1:2], st2[:, 0:1], 1.0 / D, EPS, op0=ALU.mult, op1=ALU.add), w=[bs2])
                P.op("pool", lambda e: e.tensor_tensor(st2[:, 3:4], st2[:, 1:2], mhalf[:, 0:1], ALU.pow), r=[b_mhalf], w=[bs2])
                P.op("pool", lambda e: e.scalar_tensor_tensor(out=tm, in0=x1, scalar=st2[:, 3:4], in1=fgb[:], op0=ALU.mult, op1=ALU.mult), r=[bx1, bs2, b_fgb], w=[btm]) if False else \
                    P.op("dve", lambda e: e.scalar_tensor_tensor(out=tm, in0=x1, scalar=st2[:, 3:4], in1=fgb[:], op0=ALU.mult, op1=ALU.mult), r=[bx1, bs2, b_fgb], w=[btm])
                P.dma(out_d[blk * 128:(blk + 1) * 128, :], tm, r=[btm], w=[B_out], sb=btm)

            NBLK = H // 128
            cur = {}
            for _ in stage1(0, cur):
                pass
            for blk in range(NBLK):
                nxt = {}
                bg = stage1(blk + 1, nxt) if blk + 1 < NBLK else None
                stage2(blk, cur, bg)
                cur = nxt
        P.finish()
    return nc


_PROG_CACHE = {}


def _host_inputs(inp, L, cores):
    f32 = np.float32
    x = np.asarray(inp["x"], f32)
    ctx = np.asarray(inp["ctx"], f32)
    c = np.asarray(inp["c"], f32)
    c_ctx = np.asarray(inp["c_ctx"], f32)
    w_in = np.ascontiguousarray(np.asarray(inp["w_in"], f32)[0])
    shared = dict(
        w_ada=np.ascontiguousarray(np.asarray(inp["w_ada"], f32)[0]),
        b_ada=np.asarray(inp["b_ada"], f32).reshape(1, -1),
        norm1_g=np.asarray(inp["norm1_g"], f32).reshape(1, -1),
        w_in=w_in,
        w_ret_out=np.ascontiguousarray(np.asarray(inp["w_ret_out"], f32)[0]),
        w_mlstm_out=np.ascontiguousarray(np.asarray(inp["w_mlstm_out"], f32)[0]),
        w_out=np.ascontiguousarray(np.asarray(inp["w_out"], f32)[0]),
        norm2_g=np.asarray(inp["norm2_g"], f32).reshape(1, -1),
        peer_query=np.ascontiguousarray(np.asarray(inp["peer_query"], f32)[0]),
        peer_keys=np.ascontiguousarray(np.asarray(inp["peer_keys"], f32)[0].reshape(16 * 128, 128)),
        peer_down=np.ascontiguousarray(np.asarray(inp["peer_down"], f32)[0]),
        peer_up=np.ascontiguousarray(np.asarray(inp["peer_up"], f32)[0]),
        final_g=np.asarray(inp["final_g"], f32).reshape(1, -1),
    )
    p = np.arange(128)
    ident = (p[:, None] == p[None, :]).astype(f32)
    triA = (p[:, None] <= p[None, :]).astype(f32)
    triB = (p[:, None] >= p[None, :]).astype(f32)
    negA = (triB - 1.0) * 1.0e30
    negB = (triA - 1.0) * 1.0e30
    ones = np.ones((128, 128), f32)
    cst = np.ascontiguousarray(np.stack([ident, triA, triB, negA, negB, ones], axis=1).astype(f32))
    pos = np.zeros((128, 4), f32)
    pos[:, 0] = p + 1.0
    pos[:, 1] = 128.0 - p
    shared["cst"] = cst
    shared["pos"] = pos
    w_mg = w_in[:, O_MG:O_MG + 16]
    gbias = np.asarray(inp["m_gate_bias"], f32)[0].reshape(16)
    rdec = np.asarray(inp["ret_decay"], f32)[0]
    mconv = np.asarray(inp["m_conv"], f32)[0]
    n_ax = 16
    freq = (10000.0 ** (-np.arange(n_ax, dtype=np.float64) / n_ax))
    maps = []
    for (b, half) in cores:
        t = np.arange(L) if half == 0 else np.arange(L - 1, -1, -1)
        row = (t // 64).astype(np.float64)
        col = (t % 64).astype(np.float64)
        ang = np.concatenate([row[:, None] * freq, col[:, None] * freq], axis=-1)
        rot = np.concatenate([np.cos(ang), np.sin(ang)], axis=-1).astype(f32)
        m = dict(shared)
        if half == 0:
            m["x"] = np.ascontiguousarray(x[b])
            m["ctx"] = np.ascontiguousarray(ctx[b])
            m["w_mg"] = np.ascontiguousarray(w_mg)
            m["gbias"] = gbias.reshape(1, 16).copy()
            m["ret_decay"] = rdec.reshape(1, 16).copy()
            m["m_conv"] = np.ascontiguousarray(mconv)
        else:
            m["x"] = np.ascontiguousarray(x[b, ::-1])
            m["ctx"] = np.ascontiguousarray(ctx[b, ::-1])
            m["w_mg"] = np.ascontiguousarray(np.concatenate([w_mg[:, 8:16], w_mg[:, 0:8]], axis=1))
            m["gbias"] = np.concatenate([gbias[8:16], gbias[0:8]]).reshape(1, 16).copy()
            m["ret_decay"] = np.concatenate([rdec[1], rdec[0]]).reshape(1, 16).copy()
            m["m_conv"] = np.ascontiguousarray(mconv[::-1])
        m["cvec"] = np.ascontiguousarray(np.stack([c[b], c_ctx], axis=0))
        m["rot"] = rot
        maps.append(m)
    return maps


def kernel(**inputs):
    x = np.asarray(inputs["x"])
    B, L, _ = x.shape
    cores = [(b, h) for b in range(B) for h in range(2)]
    if L not in _PROG_CACHE:
        _PROG_CACHE[L] = build_program(L)
    nc = _PROG_CACHE[L]
    maps = _host_inputs(inputs, L, cores)
    res = run_bass_kernel_spmd(nc, maps, core_ids=list(range(len(cores))))
    out = np.empty((B, L, D), np.float32)
    Hh = L // 2
    for i, (b, h) in enumerate(cores):
        o = np.asarray(res.results[i]["out"], np.float32)
        if h == 0:
            out[b, 0:Hh] = o
        else:
            out[b, Hh:L] = o[::-1]
    return out
```

```python
import math
from contextlib import ExitStack
import numpy as np
import concourse.bass as bass
import concourse.mybir as mybir
from concourse.bass_utils import run_bass_kernel_spmd

F32 = mybir.dt.float32
BF16 = mybir.dt.bfloat16
I32 = mybir.dt.int32
U32 = mybir.dt.uint32
ALU = mybir.AluOpType
AF = mybir.ActivationFunctionType
AX = mybir.AxisListType

D = 2048
KC = 16
EPS = 1e-6
NEG = -1.0e30


class Buf:
    __slots__ = ("name", "w", "r", "multi", "sem", "base")

    def __init__(self, name, multi=False):
        self.name = name
        self.w = {}
        self.r = {}
        self.multi = multi
        self.sem = None
        self.base = {}

    def fresh(self):
        base = dict(self.w)
        for k, v in self.r.items():
            if base.get(k, 0) < v:
                base[k] = v
        self.base = base
        self.w = {}
        self.r = {}


class Prog:
    def __init__(self, nc, es):
        self.nc = nc
        self.es = es
        self.eng = {"pe": nc.tensor, "dve": nc.vector, "act": nc.scalar, "pool": nc.gpsimd, "sp": nc.sync}
        self.sems = {}
        self.cnt = {}
        self.waited = {e: {} for e in self.eng}
        for e in self.eng:
            self.sems[e] = es.enter_context(nc.semaphore("s_" + e))
            self.cnt[e] = 0
        self.ndsem = 0
        self.ninst = 0
        self.rec = None

    class _RecEng:
        def __init__(self):
            self.call = None

        def __getattr__(self, name):
            def f(*a, **k):
                self.call = (name, a, k)
                return None
            return f

    def zip_emit(self, lists):
        saved, self.rec = self.rec, None
        lists = [l for l in lists if l]
        idx = [0] * len(lists)
        total = sum(len(l) for l in lists)
        for _ in range(total):
            best = min((idx[i] / len(lists[i]), i) for i in range(len(lists)) if idx[i] < len(lists[i]))[1]
            it = lists[best][idx[best]]
            idx[best] += 1
            if it[0] == "fresh":
                it[1].fresh()
            elif it[0] == "op":
                _, e, name, a_, k_, r, w = it
                self.op(e, lambda eng, name=name, a_=a_, k_=k_: getattr(eng, name)(*a_, **k_), r, w)
            else:
                _, out, in_, r, w, sb, e, kw = it
                self.dma(out, in_, r=r, w=w, sb=sb, e=e, **kw)
        self.rec = saved

    def _deps(self, r, w):
        deps = {}

        def add(k, v):
            if deps.get(k, 0) < v:
                deps[k] = v
        for b in r:
            for k, v in b.w.items():
                add(k, v)
        for b in w:
            if not b.multi:
                for k, v in b.w.items():
                    add(k, v)
                for k, v in b.r.items():
                    add(k, v)
            else:
                for k, v in b.base.items():
                    add(k, v)
        return deps

    def _wait(self, e, deps):
        wd = self.waited[e]
        for k, v in deps.items():
            if e == "pe" and k == "pe":
                continue
            if wd.get(k, 0) >= v:
                continue
            self.eng[e].wait_ge(self.sems[k], v)
            wd[k] = v
            self.ninst += 1

    def _mark(self, key, val, r, w):
        for b in r:
            if b.r.get(key, 0) < val:
                b.r[key] = val
        for b in w:
            if b.multi:
                if b.w.get(key, 0) < val:
                    b.w[key] = val
            else:
                b.w = {key: val}
                b.r = {}

    def op(self, e, fn, r=(), w=()):
        if self.rec is not None:
            pe = Prog._RecEng()
            fn(pe)
            name, a_, k_ = pe.call
            self.rec.append(("op", e, name, a_, k_, tuple(r), tuple(w)))
            return None
        self._wait(e, self._deps(r, w))
        ins = fn(self.eng[e])
        self.cnt[e] += 1
        ins.then_inc(self.sems[e], 1)
        self.ninst += 1
        self._mark(e, self.cnt[e], r, w)
        return ins

    def dma(self, out, in_, r=(), w=(), sb=None, e="sp", **kw):
        if self.rec is not None:
            self.rec.append(("dma", out, in_, tuple(r), tuple(w), sb, e, kw))
            return None
        if sb.sem is None:
            key = "d%d" % self.ndsem
            self.ndsem += 1
            self.sems[key] = self.es.enter_context(self.nc.semaphore("s_" + key))
            self.cnt[key] = 0
            sb.sem = key
        key = sb.sem
        self._wait(e, self._deps(r, w))
        ins = self.eng[e].dma_start(out=out, in_=in_, **kw)
        self.cnt[key] += 16
        ins.then_inc(self.sems[key], 16)
        self.ninst += 1
        self._mark(key, self.cnt[key], r, w)
        return ins

    def gather(self, out, table, idx_ap, r=(), w=(), sb=None):
        if sb.sem is None:
            key = "d%d" % self.ndsem
            self.ndsem += 1
            self.sems[key] = self.es.enter_context(self.nc.semaphore("s_" + key))
            self.cnt[key] = 0
            sb.sem = key
        key = sb.sem
        e = "pool"
        self._wait(e, self._deps(r, w))
        ins = self.nc.gpsimd.indirect_dma_start(
            out=out, out_offset=None, in_=table,
            in_offset=bass.IndirectOffsetOnAxis(ap=idx_ap, axis=0))
        self.cnt[key] += 16
        ins.then_inc(self.sems[key], 16)
        self.ninst += 1
        self._mark(key, self.cnt[key], r, w)
        return ins

    def finish(self):
        for k, v in self.cnt.items():
            if v > 0 and k != "sp":
                self.nc.sync.wait_ge(self.sems[k], v)


R_HEADS, R_DK, R_DV = 8, 64, 128
M_HEADS, M_DK, M_DV = 4, 128, 256
IN_SPLITS = (512, 512, 1024, 1024, 512, 512, 1024, 1024, 16, 2048, 2048)
IN_OFF = [0]
for _s in IN_SPLITS:
    IN_OFF.append(IN_OFF[-1] + _s)
(O_RQ, O_RK, O_RV, O_RG, O_MQ, O_MK, O_MV, O_MO, O_MG, O_GR, O_GM, IN_COLS) = IN_OFF
P_HEADS, P_NK, P_TOPK = 8, 128, 16
NEXP = 16384
CL = 256


class Ring:
    prog = None

    def __init__(self, nc, es, name, shape, dt, n, psum=False, multi=False):
        self.items = []
        self.multi = multi
        for i in range(n):
            if psum:
                t = es.enter_context(nc.psum_tensor("r_%s%d" % (name, i), shape, dt))
            else:
                t = es.enter_context(nc.sbuf_tensor("r_%s%d" % (name, i), shape, dt))
            self.items.append((t, Buf("%s%d" % (name, i), multi)))
        self.i = 0

    def next(self):
        it = self.items[self.i % len(self.items)]
        self.i += 1
        if self.multi:
            if Ring.prog is not None and Ring.prog.rec is not None:
                Ring.prog.rec.append(("fresh", it[1]))
            else:
                it[1].fresh()
        return it


class RingView:
    def __init__(self, items):
        self.items = list(items)
        self.i = 0

    def next(self):
        it = self.items[self.i % len(self.items)]
        self.i += 1
        return it


def build_program(L, dbg=False):
    H = L // 2
    NST = H // 512
    nc = bass.Bass("TRN2", target_bir_lowering=False)

    def din(name, shape, dt=F32):
        return nc.dram_tensor(name, list(shape), dt, kind="ExternalInput").ap()

    def dscr(name, shape, dt):
        return nc.dram_tensor(name, list(shape), dt, kind=("ExternalOutput" if dbg else "Internal")).ap()

    x_d = din("x", [L, D])
    ctx_d = din("ctx", [CL, D])
    cvec_d = din("cvec", [2, D])
    wada_d = din("w_ada", [D, 6 * D])
    bada_d = din("b_ada", [1, 6 * D])
    g1_d = din("norm1_g", [1, D])
    win_d = din("w_in", [D, IN_COLS])
    wmg_d = din("w_mg", [D, 16])
    gbias_d = din("gbias", [1, 16])
    rdec_d = din("ret_decay", [1, 16])
    mconv_d = din("m_conv", [3, 1024])
    wro_d = din("w_ret_out", [1024, D])
    wmo_d = din("w_mlstm_out", [1024, D])
    wout_d = din("w_out", [D, D])
    g2_d = din("norm2_g", [1, D])
    pq_d = din("peer_query", [D, D])
    pk_d = din("peer_keys", [16 * 128, 128])
    pdn_d = din("peer_down", [NEXP, D])
    pup_d = din("peer_up", [NEXP, D])
    fg_d = din("final_g", [1, D])
    rot_d = din("rot", [L, 64])
    cst_d = din("cst", [128, 6, 128])
    pos_d = din("pos", [128, 4])
    out_d = nc.dram_tensor("out", [H, D], F32, kind="ExternalOutput").ap()

    xmT_d = dscr("xmT", [D, L + 2], BF16)
    cmT_d = dscr("cmT", [D, CL + 2], BF16)
    ada_d = dscr("ada", [2, 6 * D], F32)
    winb_d = dscr("winb", [D, IN_COLS], BF16)
    wrob_d = dscr("wrob", [1024, D], BF16)
    wmob_d = dscr("wmob", [1024, D], BF16)
    woutb_d = dscr("woutb", [D, D], BF16)
    pqb_d = dscr("pqb", [D, D], BF16)
    pcomb_d = dscr("pcomb", [NEXP, 2 * D], BF16)
    yA_d = dscr("yA", [H, D], F32)
    yoT_d = dscr("yoT", [D, H], BF16)
    x1_d = dscr("x1", [H, D], F32)
    B_xmT, B_cmT, B_ada = Buf("xmT_d", True), Buf("cmT_d", True), Buf("ada_d")
    B_winb, B_wrob, B_wmob, B_woutb, B_pqb = (Buf(n, True) for n in ("winb", "wrob", "wmob", "woutb", "pqb"))
    B_pdnb, B_pupb = Buf("pdnb", True), Buf("pupb", True)
    B_yA, B_yoT, B_x1, B_out = Buf("yA", True), Buf("yoT", True), Buf("x1", True), Buf("out", True)

    with ExitStack() as es:
        P = Prog(nc, es)
        Ring.prog = P

        def barrier():
            for e in ("pe", "dve", "act", "pool", "sp"):
                P._wait(e, {k: v for k, v in P.cnt.items() if v > 0 and k != e})

        def sbt(st, name, shape, dt):
            return st.enter_context(nc.sbuf_tensor("t_" + name, shape, dt)), Buf(name)

        cst, b_cst = sbt(es, "cst", [128, 6, 128], F32)
        pos, b_pos = sbt(es, "pos", [128, 4], F32)
        identb, b_identb = sbt(es, "identb", [128, 128], BF16)
        P.dma(cst[:], cst_d, w=[b_cst], sb=b_cst)
        P.dma(pos[:], pos_d, w=[b_pos], sb=b_pos)
        P.op("dve", lambda e: e.tensor_copy(identb[:], cst[:, 0, :]), r=[b_cst], w=[b_identb])
        identf = cst[:, 0, :]
        mhalf, b_mhalf = sbt(es, "mhalf", [128, 16], F32)
        P.op("pool", lambda e: e.memset(mhalf[:], -0.5), w=[b_mhalf])
        tri = [cst[:, 1, :], cst[:, 2, :]]
        negm = [cst[:, 3, :], cst[:, 4, :]]
        onesf = cst[:, 5, :]
        modF, b_modF = sbt(es, "modF", [128, 6, KC], F32)
        psr = Ring(nc, es, "ps", [128, 512], F32, 8, psum=True)

        with ExitStack() as ph:
            cT, b_cT = sbt(ph, "cT", [128, 2, KC], F32)
            cTs, b_cTs = sbt(ph, "cTs", [128, 2, KC], F32)
            for r_ in range(2):
                P.dma(cT[:, r_, :], cvec_d[r_:r_ + 1, :].rearrange("r (k p) -> p (r k)", p=128), w=[b_cT], sb=b_cT,
                      allow_slow_non_contiguous=True)
            P.op("act", lambda e: e.activation(cTs[:], cT[:], AF.Silu), r=[b_cT], w=[b_cTs])
            adasb, b_adasb = sbt(ph, "adasb", [2, 6 * D], F32)
            badasb, b_badasb = sbt(ph, "badasb", [2, 6 * D], F32)
            P.dma(badasb[:], bada_d.partition_broadcast(2), w=[b_badasb], sb=b_badasb)
            wring = Ring(nc, ph, "wada", [128, KC, 512], F32, 2)
            for nb in range(24):
                wt, bw = wring.next()
                P.dma(wt[:], wada_d[:, nb * 512:(nb + 1) * 512].rearrange("(k p) n -> p k n", p=128),
                      w=[bw], sb=bw)
                pt, bp = psr.next()
                for k in range(KC):
                    P.op("pe", lambda e: e.matmul(pt[0:2, :], lhsT=cTs[:, :, k], rhs=wt[:, k, :],
                                                  start=(k == 0), stop=(k == KC - 1)),
                         r=[b_cTs, bw], w=[bp])
                P.op("dve", lambda e: e.tensor_tensor(adasb[:, nb * 512:(nb + 1) * 512], pt[0:2, :],
                                                      badasb[:, nb * 512:(nb + 1) * 512], ALU.add),
                     r=[bp, b_badasb], w=[b_adasb])
            P.dma(ada_d, adasb[:], r=[b_adasb], w=[B_ada], sb=b_adasb)
            raw, b_raw = sbt(ph, "rawmod", [128, 8, KC], F32)
            srcs = [ada_d[0:1, 0:D], ada_d[0:1, D:2 * D], ada_d[0:1, 3 * D:4 * D], ada_d[0:1, 4 * D:5 * D],
                    ada_d[1:2, 0:D], ada_d[1:2, D:2 * D], g1_d, g2_d]
            for i, s in enumerate(srcs):
                P.dma(raw[:, i, :], s.rearrange("r (k p) -> p (r k)", p=128), r=[B_ada], w=[b_raw], sb=b_raw,
                      allow_slow_non_contiguous=True)
            for (dst, sc, g, sh) in ((0, 1, 6, 0), (2, 5, 6, 4), (4, 3, 7, 2)):
                P.op("dve", lambda e: e.scalar_tensor_tensor(out=modF[:, dst, :], in0=raw[:, sc, :], scalar=1.0,
                                                             in1=raw[:, g, :], op0=ALU.add, op1=ALU.mult),
                     r=[b_raw], w=[b_modF])
                P.op("dve", lambda e: e.tensor_copy(modF[:, dst + 1, :], raw[:, sh, :]), r=[b_raw], w=[b_modF])
        barrier()

        with ExitStack() as ph:
            lst_b, lst_c = [], []
            P.rec = lst_b
            FMAX = 4096
            cin = Ring(nc, ph, "cvin", [128, FMAX], F32, 5)
            cout = Ring(nc, ph, "cvout", [128, FMAX], BF16, 5)
            cnt = [0]

            def convert(pairs, bdst):
                for (sa, da) in pairs:
                    fsz = sa.shape[1]
                    ti, bi = cin.next()
                    to, bo = cout.next()
                    P.dma(ti[:, 0:fsz], sa, w=[bi], sb=bi)
                    eng = ("dve", "act")[cnt[0] % 2]
                    cnt[0] += 1
                    if eng == "act":
                        P.op("act", lambda e: e.copy(to[:, 0:fsz], ti[:, 0:fsz]), r=[bi], w=[bo])
                    else:
                        P.op(eng, lambda e: e.tensor_copy(to[:, 0:fsz], ti[:, 0:fsz]), r=[bi], w=[bo])
                    if len(da.shape) == 3:
                        P.dma(da, to[:, 0:fsz].rearrange("p (r c) -> p r c", r=da.shape[1]), r=[bo], w=[bdst], sb=bo)
                    else:
                        P.dma(da, to[:, 0:fsz], r=[bo], w=[bdst], sb=bo)

            convert([(win_d[rb * 128:(rb + 1) * 128, cb * 2564:(cb + 1) * 2564], winb_d[rb * 128:(rb + 1) * 128, cb * 2564:(cb + 1) * 2564])
                     for rb in range(16) for cb in range(4)], B_winb)
            for s_, d_, b_ in ((wro_d, wrob_d, B_wrob), (wmo_d, wmob_d, B_wmob), (wout_d, woutb_d, B_woutb), (pq_d, pqb_d, B_pqb)):
                sv_ = s_.rearrange("(rb p r) c -> rb p (r c)", p=128, r=2)
                dv_ = d_.rearrange("(rb p r) c -> rb p (r c)", p=128, r=2)
                convert([(sv_[i], dv_[i]) for i in range(sv_.shape[0])], b_)
            for s_, c0_, b_ in ((pdn_d, 0, B_pdnb), (pup_d, D, B_pupb)):
                sv_ = s_.rearrange("(rb p r) c -> rb p (r c)", p=128, r=2)
                dv_ = pcomb_d.rearrange("(rb p r) c -> rb p r c", p=128, r=2)
                convert([(sv_[i], dv_[i][:, :, c0_:c0_ + D]) for i in range(sv_.shape[0])], b_)
            P.rec = lst_c
            xin = Ring(nc, ph, "xin", [128, D], F32, 3)
            xnr = Ring(nc, ph, "xn", [128, D], BF16, 2)
            jk, b_jk = sbt(ph, "jk", [128, D], BF16)
            stat = Ring(nc, ph, "stat", [128, 4], F32, 3)
            xts = Ring(nc, ph, "xts", [128, KC, 512], BF16, 2, multi=True)
            zt, b_zt = sbt(ph, "zt", [128, KC, 1], BF16)
            P.op("pool", lambda e: e.memset(zt[:], 0.0), w=[b_zt])
            for dst, bd, n in ((xmT_d, B_xmT, L), (cmT_d, B_cmT, CL)):
                v = dst.rearrange("(k p) n -> p k n", p=128)
                P.dma(v[:, :, 0:1], zt[:], r=[b_zt], w=[bd], sb=b_zt, allow_slow_non_contiguous=True)
                P.dma(v[:, :, n + 1:n + 2], zt[:], r=[b_zt], w=[bd], sb=b_zt, allow_slow_non_contiguous=True)
            units = [(ctx_d, cmT_d, B_cmT, 0, CL, 2)] + [(x_d, xmT_d, B_xmT, s * 512, 512, 0) for s in range(L // 512)]
            ecnt = 0
            for (src, dst, bd, base, NT, mi) in units:
                XT, bXT = xts.next()
                for j in range(NT // 128):
                    xt, bx = xin.next()
                    P.dma(xt[:], src[base + j * 128: base + (j + 1) * 128, :], w=[bx], sb=bx)
                    st, bs = stat.next()
                    P.op("dve", lambda e: e.scalar_tensor_tensor(out=jk[:], in0=xt[:], scalar=1.0, in1=xt[:],
                                                                 op0=ALU.mult, op1=ALU.mult, accum_out=st[:, 0:1]),
                         r=[bx], w=[b_jk, bs])
                    P.op("dve", lambda e: e.tensor_scalar(st[:, 1:2], st[:, 0:1], 1.0 / D, EPS, op0=ALU.mult, op1=ALU.add),
                         r=[bs], w=[bs])
                    P.op("act", lambda e: e.activation(st[:, 2:3], st[:, 1:2], AF.Sqrt), r=[bs], w=[bs])
                    P.op("dve", lambda e: e.reciprocal(st[:, 3:4], st[:, 2:3]), r=[bs], w=[bs])
                    xn, bxn = xnr.next()
                    P.op("act", lambda e: e.activation(xn[:], xt[:], AF.Identity, scale=st[:, 3:4]), r=[bx, bs], w=[bxn])
                    for half in range(2):
                        pt, bp = psr.next()
                        ptb = pt[:].bitcast(BF16)
                        for kk in range(8):
                            k = half * 8 + kk
                            P.op("pe", lambda e: e.transpose(ptb[:, kk * 128:(kk + 1) * 128], xn[:, k * 128:(k + 1) * 128], identb[:]),
                                 r=[bxn, b_identb], w=[bp])
                        for kk in range(8):
                            k = half * 8 + kk
                            o = XT[:, k, j * 128:(j + 1) * 128]
                            i_ = ptb[:, kk * 128:(kk + 1) * 128]
                            if half == 0:
                                P.op("act", lambda e: e.activation(o, i_, AF.Identity, bias=modF[:, mi + 1, k:k + 1], scale=modF[:, mi, k:k + 1]),
                                     r=[b_modF], w=[bXT, bp])
                            else:
                                P.op("dve", lambda e: e.tensor_scalar(o, i_, modF[:, mi, k:k + 1], modF[:, mi + 1, k:k + 1], op0=ALU.mult, op1=ALU.add),
                                     r=[b_modF], w=[bXT, bp])
                P.dma(dst.rearrange("(k p) n -> p k n", p=128)[:, :, 1 + base:1 + base + NT], XT[:, :, 0:NT], r=[bXT], w=[bd], sb=bXT)
            P.rec = None
            P.zip_emit([lst_b, lst_c])
        barrier()

        with ExitStack() as ph:
            rd, b_rd = sbt(ph, "rd", [128, 16], F32)
            lgn, b_lgn = sbt(ph, "lgn", [128, 16], F32)
            tmp16, b_tmp16 = sbt(ph, "tmp16", [128, 16], F32)
            dec, b_dec = sbt(ph, "dec", [128, 2, 3, 8], F32)
            P.dma(rd[:], rdec_d.partition_broadcast(128), w=[b_rd], sb=b_rd)
            P.op("act", lambda e: e.activation(rd[:], rd[:], AF.Exp, scale=-1.0), r=[b_rd], w=[b_rd])
            P.op("dve", lambda e: e.tensor_scalar(lgn[:], rd[:], -1.0 / 8, 1.0 / 7, op0=ALU.mult, op1=ALU.add), r=[b_rd], w=[b_lgn])
            for cf in (6, 5, 4, 3, 2, 1):
                P.op("dve", lambda e: e.tensor_tensor(tmp16[:], lgn[:], rd[:], ALU.mult), r=[b_lgn, b_rd], w=[b_tmp16])
                P.op("dve", lambda e: e.tensor_scalar(lgn[:], tmp16[:], -1.0, 1.0 / cf, op0=ALU.mult, op1=ALU.add), r=[b_tmp16], w=[b_lgn])
            P.op("dve", lambda e: e.tensor_tensor(lgn[:], lgn[:], rd[:], ALU.mult), r=[b_lgn, b_rd], w=[b_lgn])
            for dr in range(2):
                P.op("dve", lambda e: e.tensor_scalar(tmp16[:, 0:8], lgn[:, dr * 8:(dr + 1) * 8], pos[:, dr:dr + 1], None, op0=ALU.mult),
                     r=[b_lgn, b_pos], w=[b_tmp16])
                P.op("act", lambda e: e.activation(dec[:, dr, 0, :], tmp16[:, 0:8], AF.Exp, scale=-1.0), r=[b_tmp16], w=[b_dec])
                P.op("act", lambda e: e.activation(dec[:, dr, 1, :], tmp16[:, 0:8], AF.Exp, bias=-math.log(8.0)), r=[b_tmp16], w=[b_dec])
                P.op("act", lambda e: e.activation(dec[:, dr, 2, :], lgn[:, dr * 8:(dr + 1) * 8], AF.Exp, scale=-128.0), r=[b_lgn], w=[b_dec])
            cw, b_cw = sbt(ph, "cw", [128, 3, 8], F32)
            for j_ in range(3):
                P.dma(cw[:, j_, :], mconv_d[j_:j_ + 1, :].rearrange("r (b p) -> p (r b)", p=128), w=[b_cw], sb=b_cw, allow_slow_non_contiguous=True)
            gb, b_gb = sbt(ph, "gb", [128, 16], F32)
            P.dma(gb[:], gbias_d.partition_broadcast(128), w=[b_gb], sb=b_gb)
            wmgf, b_wmgf = sbt(ph, "wmgf", [128, KC, 16], F32)
            wmg, b_wmg = sbt(ph, "wmg", [128, KC, 16], BF16)
            P.dma(wmgf[:], wmg_d.rearrange("(k p) n -> p k n", p=128), w=[b_wmgf], sb=b_wmgf)
            P.op("dve", lambda e: e.tensor_copy(wmg[:], wmgf[:]), r=[b_wmgf], w=[b_wmg])
            S32, b_S32 = sbt(ph, "S32", [64, 8, 128], F32)
            Sbf, b_Sbf = sbt(ph, "Sbf", [64, 8, 128], BF16)
            C32, b_C32 = sbt(ph, "C32", [128, 4, 257], F32)
            Cbf, b_Cbf = sbt(ph, "Cbf", [128, 4, 257], BF16)
            mprev, b_mprev = sbt(ph, "mprev", [128, 4], F32)
            XTr = Ring(nc, ph, "XT", [128, KC, 514], BF16, 1)
            Wbr = Ring(nc, ph, "Wb", [128, KC, 512], BF16, 2)
            RKr = Ring(nc, ph, "RK", [128, 4, 512], BF16, 1, multi=True)
            RQr = Ring(nc, ph, "RQ", [128, 4, 512], BF16, 1, multi=True)
            RVr = Ring(nc, ph, "RVs", [128, 4, 8, 128], BF16, 1, multi=True)
            MQKr = Ring(nc, ph, "MQK", [128, 8, 512], BF16, 1, multi=True)
            MVr = Ring(nc, ph, "MV", [128, 4, 1024], BF16, 1, multi=True)
            RGr = Ring(nc, ph, "RG", [128, 4, 1024], BF16, 1, multi=True)
            MOr = Ring(nc, ph, "MO", [128, 4, 1024], BF16, 1, multi=True)
            Gr = Ring(nc, ph, "G", [128, 4, 16], F32, 1, multi=True)
            rotr = Ring(nc, ph, "rot", [128, 4, 64], F32, 2)
            rawr = Ring(nc, ph, "raw", [128, 514], F32, 2)
            accr = Ring(nc, ph, "cacc", [128, 512], F32, 2)
            rtr = Ring(nc, ph, "rtmp", [128, 2, 8, 2, 32], F32, 1)
            QTr = Ring(nc, ph, "QT", [64, 8, 128], BF16, 2)
            KTr = Ring(nc, ph, "KT", [64, 8, 128], BF16, 2)
            ATr = Ring(nc, ph, "AT", [128, 8, 128], BF16, 1)
            ATmr = Ring(nc, ph, "ATm", [128, 4, 128], BF16, 2)
            MKtr = Ring(nc, ph, "MKt", [128, 4, 128], BF16, 2)
            Vmr = Ring(nc, ph, "Vm", [128, 4, 257], BF16, 2)
            gsr = Ring(nc, ph, "gs", [128, 16, 4], F32, 2)
            spur = Ring(nc, ph, "spu", [128, 2, 4, 4], F32, 2)
            Dr = Ring(nc, ph, "Dg", [128, 4, 128], F32, 1)
            Umr = Ring(nc, ph, "Um", [128, 4, 128], F32, 1)
            Yr = Ring(nc, ph, "Y", [128, D], F32, 2)
            Ym_bufs = [Buf("Ym0"), Buf("Ym1")]
            YAr = Ring(nc, ph, "YA", [128, D], F32, 1)
            sqr = Ring(nc, ph, "sq", [128, D], F32, 1)
            yor = Ring(nc, ph, "yo", [128, D], BF16, 1)
            nsr = Ring(nc, ph, "ns", [128, 4, 12], F32, 2)
            YOTr = Ring(nc, ph, "YOT", [128, KC, 512], BF16, 1, multi=True)
            dnr = Ring(nc, ph, "dn", [128, 4, 4], F32, 2)

            def proj_tok(XT, bXT, j, Wb, bW, width=512):
                pt, bp = psr.next()
                for k in range(KC):
                    P.op("pe", lambda e: e.matmul(pt[:, 0:width], lhsT=XT[:, k, 1 + j * 128:1 + (j + 1) * 128], rhs=Wb[:, k, 0:width],
                                                  start=(k == 0), stop=(k == KC - 1)), r=[bXT, bW], w=[bp])
                return pt, bp

            def load_w(off):
                Wb, bW = Wbr.next()
                P.dma(Wb[:], winb_d[:, off:off + 512].rearrange("(k p) n -> p k n", p=128), r=[B_winb], w=[bW], sb=bW)
                return Wb, bW

            def scan_pass(dr):
                maskT = tri[dr]
                qdec = dec[:, dr, 0, :]
                kdec = dec[:, dr, 1, :]
                cdec = dec[:, dr, 2, :]
                P.op("pool", lambda e: e.memset(S32[:], 0.0), w=[b_S32])
                P.op("pool", lambda e: e.memset(Sbf[:], 0.0), w=[b_Sbf])
                P.op("pool", lambda e: e.memset(C32[:], 0.0), w=[b_C32])
                P.op("pool", lambda e: e.memset(Cbf[:], 0.0), w=[b_Cbf])
                P.op("pool", lambda e: e.memset(mprev[:], 0.0), w=[b_mprev])
                if dr == 0:
                    units = [(True, 0, CL, False)] + [(False, s * 512, 512, True) for s in range(NST)]
                else:
                    units = [(True, 0, CL, False)] + [(False, s * 512, 512, False) for s in range(2 * NST - 1, NST - 1, -1)] \
                        + [(False, s * 512, 512, True) for s in range(NST - 1, -1, -1)]
                for (is_ctx, base, NT, with_out) in units:
                    nch = NT // 128
                    srcT, bsrc = (cmT_d, B_cmT) if is_ctx else (xmT_d, B_xmT)
                    XT, bXT = XTr.next()
                    P.dma(XT[:, :, 0:NT + 2], srcT.rearrange("(k p) n -> p k n", p=128)[:, :, base:base + NT + 2], r=[bsrc], w=[bXT], sb=bXT)
                    if not is_ctx:
                        rot, brot = rotr.next()
                        P.dma(rot[:, 0:nch, :], rot_d[base:base + NT, :].rearrange("(j p) c -> p j c", p=128), w=[brot], sb=brot)
                    RK, bRK = RKr.next()
                    RQ, bRQ = RQr.next()
                    RVs, bRV = RVr.next()
                    MQK, bMQK = MQKr.next()
                    MV, bMV = MVr.next()
                    G, bG = Gr.next()
                    RG, bRG = RGr.next()
                    MO, bMO = MOr.next()

                    def rotary(pt, bp, dst, bdst, j):
                        if is_ctx:
                            P.op("act", lambda e: e.copy(dst[:, j, :], pt[:, 0:512]), w=[bdst, bp])
                            return
                        tm, btm = rtr.next()
                        pv = pt[:, 0:512].rearrange("p (h t i) -> p h t i", h=8, t=2)
                        cosb = rot[:, j, 0:32].unsqueeze(1).unsqueeze(1).to_broadcast([128, 8, 2, 32])
                        sinb = rot[:, j, 32:64].unsqueeze(1).unsqueeze(1).to_broadcast([128, 8, 2, 32])
                        P.op("dve", lambda e: e.tensor_tensor(tm[:, 0], pv, cosb, ALU.mult), r=[brot], w=[btm, bp])
                        P.op("dve", lambda e: e.tensor_tensor(tm[:, 1], pv, sinb, ALU.mult), r=[brot], w=[btm, bp])
                        dv = dst[:, j, :].rearrange("p (h t i) -> p h t i", h=8, t=2)
                        P.op("pool", lambda e: e.tensor_tensor(dv[:, :, 0, :], tm[:, 0, :, 0, :], tm[:, 1, :, 1, :], ALU.subtract), r=[btm], w=[bdst])
                        P.op("pool", lambda e: e.tensor_tensor(dv[:, :, 1, :], tm[:, 0, :, 1, :], tm[:, 1, :, 0, :], ALU.add), r=[btm], w=[bdst])

                    Wb, bW = load_w(O_RK)
                    for j in range(nch):
                        pt, bp = proj_tok(XT, bXT, j, Wb, bW)
                        rotary(pt, bp, RK, bRK, j)
                    for hb in range(2):
                        Wb, bW = load_w(O_RV + hb * 512)
                        for j in range(nch):
                            pt, bp = proj_tok(XT, bXT, j, Wb, bW)
                            P.op("dve", lambda e: e.tensor_tensor(RVs[:, j, hb * 4:(hb + 1) * 4, :], pt[:, 0:512].rearrange("p (h e) -> p h e", h=4),
                                                                  kdec[:, hb * 4:(hb + 1) * 4].unsqueeze(2).to_broadcast([128, 4, 128]), ALU.mult),
                                 r=[b_dec], w=[bRV, bp])
                    for hb in range(2):
                        Wb, bW = load_w(O_MV + hb * 512)
                        for j in range(nch):
                            pt, bp = proj_tok(XT, bXT, j, Wb, bW)
                            P.op("act", lambda e: e.copy(MV[:, j, hb * 512:(hb + 1) * 512], pt[:, 0:512]), w=[bMV, bp])
                    for j in range(nch):
                        pt, bp = proj_tok(XT, bXT, j, wmg, b_wmg, width=16)
                        P.op("dve", lambda e: e.tensor_tensor(G[:, j, :], pt[:, 0:16], gb[:], ALU.add), r=[b_gb], w=[bG, bp])
                    spu, bspu = spur.next()
                    P.op("act", lambda e: e.activation(spu[:, 1, 0:nch, :], G[:, 0:nch, dr * 8 + 4:dr * 8 + 8], AF.Exp, scale=-1.0), r=[bG], w=[bspu])
                    P.op("act", lambda e: e.activation(spu[:, 0, 0:nch, :], spu[:, 1, 0:nch, :], AF.Ln, bias=1.0), w=[bspu])
                    fm_blocks = [(O_MK, 4)] + ([(O_MQ, 0)] if with_out else [])
                    nh = (NT + 2) // 2
                    for (off, b0) in fm_blocks:
                        Wb, bW = load_w(off)
                        for hb in range(4):
                            raw, braw = rawr.next()
                            for half in range(2):
                                pt, bp = psr.next()
                                for k in range(KC):
                                    P.op("pe", lambda e: e.matmul(pt[:, 0:nh], lhsT=Wb[:, k, hb * 128:(hb + 1) * 128], rhs=XT[:, k, half * nh:(half + 1) * nh],
                                                                  start=(k == 0), stop=(k == KC - 1)), r=[bXT, bW], w=[bp])
                                if half == 0:
                                    P.op("act", lambda e: e.copy(raw[:, 0:nh], pt[:, 0:nh]), w=[braw, bp])
                                else:
                                    P.op("dve", lambda e: e.tensor_copy(raw[:, nh:2 * nh], pt[:, 0:nh]), w=[braw, bp])
                            b = b0 + hb
                            ac, bac = accr.next()
                            P.op("act", lambda e: e.activation(ac[:, 0:NT], raw[:, 1:NT + 1], AF.Identity, scale=cw[:, 1, b:b + 1]), r=[braw, b_cw], w=[bac])
                            P.op("dve", lambda e: e.scalar_tensor_tensor(out=ac[:, 0:NT], in0=raw[:, 0:NT], scalar=cw[:, 0, b:b + 1], in1=ac[:, 0:NT],
                                                                         op0=ALU.mult, op1=ALU.add), r=[braw, b_cw], w=[bac])
                            P.op("dve", lambda e: e.scalar_tensor_tensor(out=ac[:, 0:NT], in0=raw[:, 2:NT + 2], scalar=cw[:, 2, b:b + 1], in1=ac[:, 0:NT],
                                                                         op0=ALU.mult, op1=ALU.add), r=[braw, b_cw], w=[bac])
                            P.op("act", lambda e: e.activation(MQK[:, b, 0:NT], ac[:, 0:NT], AF.Silu), r=[bac], w=[bMQK])
                    if with_out:
                        Wb, bW = load_w(O_RQ)
                        for j in range(nch):
                            pt, bp = proj_tok(XT, bXT, j, Wb, bW)
                            rotary(pt, bp, RQ, bRQ, j)
                        if dr == 1:
                            for (off, dstt, bdd, fn) in ((O_RG, RG, bRG, AF.Silu), (O_MO, MO, bMO, AF.Sigmoid)):
                                for hb in range(2):
                                    Wb, bW = load_w(off + hb * 512)
                                    for j in range(nch):
                                        pt, bp = proj_tok(XT, bXT, j, Wb, bW)
                                        P.op("act", lambda e: e.activation(dstt[:, j, hb * 512:(hb + 1) * 512], pt[:, 0:512], fn), w=[bdd, bp])
                        YOT, bYOT = YOTr.next()
                    order = range(nch) if dr == 0 else range(nch - 1, -1, -1)
                    pend_out = []
                    psM, psR, psO = RingView(psr.items[0:4]), RingView(psr.items[4:7]), RingView(psr.items[7:8])
                    psx = [psM]
                    for j in order:
                        tok0 = base + j * 128
                        if with_out:
                            Y, bY = Yr.next()
                            bYm = Ym_bufs[(Yr.i - 1) % len(Yr.items)]
                        lst_R, lst_M, lst_O = [], [], []
                        P.rec = lst_R
                        psx[0] = psR
                        if with_out:
                            QT, bQT = QTr.next()
                            KT, bKT = KTr.next()
                            for (srcq, bsq, dT, bdT, eng) in ((RQ, bRQ, QT, bQT, "act"), (RK, bRK, KT, bKT, "dve")):
                                pt, bp = psx[0].next()
                                ptb = pt[:].bitcast(BF16)
                                for h in range(8):
                                    P.op("pe", lambda e: e.transpose(ptb[0:64, h * 128:(h + 1) * 128], srcq[:, j, h * 64:(h + 1) * 64], identb[:]),
                                         r=[bsq, b_identb], w=[bp])
                                if eng == "act":
                                    P.op("act", lambda e: e.copy(dT[:].rearrange("p h c -> p (h c)"), ptb[0:64, :]), w=[bdT, bp])
                                else:
                                    P.op("dve", lambda e: e.tensor_copy(dT[:].rearrange("p h c -> p (h c)"), ptb[0:64, :]), w=[bdT, bp])
                            AT, bAT = ATr.next()
                            for hb in range(2):
                                pa, bpa = psx[0].next()
                                for hh in range(4):
                                    h = hb * 4 + hh
                                    P.op("pe", lambda e: e.matmul(pa[:, hh * 128:(hh + 1) * 128], lhsT=KT[:, h, :], rhs=QT[:, h, :], start=True, stop=True),
                                         r=[bKT, bQT], w=[bpa])
                                P.op("dve", lambda e: e.tensor_tensor(AT[:, hb * 4:(hb + 1) * 4, :], pa[:, 0:512].rearrange("p (h c) -> p h c", h=4),
                                                                      maskT.unsqueeze(1).to_broadcast([128, 4, 128]), ALU.mult), r=[b_cst], w=[bAT, bpa])
                            for hb in range(2):
                                py, bpy = psx[0].next()
                                for hh in range(4):
                                    h = hb * 4 + hh
                                    P.op("pe", lambda e: e.matmul(py[:, hh * 128:(hh + 1) * 128], lhsT=AT[:, h, :], rhs=RVs[:, j, h, :], start=True, stop=False),
                                         r=[bAT, bRV], w=[bpy])
                                    P.op("pe", lambda e: e.matmul(py[:, hh * 128:(hh + 1) * 128], lhsT=QT[:, h, :], rhs=Sbf[:, h, :], start=False, stop=True),
                                         r=[bQT, b_Sbf], w=[bpy])
                                P.op("dve", lambda e: e.tensor_tensor(Y[:, hb * 512:(hb + 1) * 512].rearrange("p (h e) -> p h e", h=4),
                                                                      py[:, 0:512].rearrange("p (h e) -> p h e", h=4),
                                                                      qdec[:, hb * 4:(hb + 1) * 4].unsqueeze(2).to_broadcast([128, 4, 128]), ALU.mult),
                                     r=[b_dec], w=[bY, bpy])
                        for hb in range(2):
                            pS, bpS = psx[0].next()
                            for hh in range(4):
                                h = hb * 4 + hh
                                P.op("pe", lambda e: e.matmul(pS[0:64, hh * 128:(hh + 1) * 128], lhsT=RK[:, j, h * 64:(h + 1) * 64], rhs=RVs[:, j, h, :],
                                                              start=True, stop=True), r=[bRK, bRV], w=[bpS])
                            sv = S32[:, hb * 4:(hb + 1) * 4, :]
                            P.op("dve", lambda e: e.tensor_tensor(sv, sv, pS[0:64, 0:512].rearrange("p (h e) -> p h e", h=4), ALU.add), w=[b_S32, bpS])
                            P.op("pool", lambda e: e.tensor_tensor(sv, sv, cdec[0:64, hb * 4:(hb + 1) * 4].unsqueeze(2).to_broadcast([64, 4, 128]), ALU.mult),
                                 r=[b_dec], w=[b_S32])
                            P.op("act", lambda e: e.copy(Sbf[:, hb * 4:(hb + 1) * 4, :], sv), r=[b_S32], w=[b_Sbf])
                        P.rec = lst_M
                        psx[0] = psM
                        gs, bgs = gsr.next()
                        zi = G[:, j, dr * 8:dr * 8 + 4]
                        zf = G[:, j, dr * 8 + 4:dr * 8 + 8]
                        pg, bpg = psx[0].next()
                        P.op("pe", lambda e: e.matmul(pg[:, 0:4], lhsT=tri[dr], rhs=spu[:, 0, j, :], start=True, stop=True), r=[bspu, b_cst], w=[bpg])
                        P.op("pe", lambda e: e.matmul(pg[:, 4:8], lhsT=onesf, rhs=spu[:, 0, j, :], start=True, stop=True), r=[bspu, b_cst], w=[bpg])
                        cs = pg[:, 0:4]
                        tot = pg[:, 4:8]
                        P.op("dve", lambda e: e.tensor_tensor(gs[:, 2, :], zi, cs, ALU.add), r=[bG], w=[bgs, bpg])
                        Dg, bDg = Dr.next()
                        P.op("dve", lambda e: e.tensor_tensor(Dg[:], identf.unsqueeze(1).to_broadcast([128, 4, 128]),
                                                              gs[:, 2, :].unsqueeze(2).to_broadcast([128, 4, 128]), ALU.mult), r=[b_cst, bgs], w=[bDg])
                        pu, bpu = psx[0].next()
                        P.op("pe", lambda e: e.matmul(pu[:, 0:512], lhsT=onesf, rhs=Dg[:].rearrange("p h s -> p (h s)"), start=True, stop=True),
                             r=[bDg, b_cst], w=[bpu])
                        puv = pu[:, 0:512].rearrange("p (h s) -> p h s", h=4)
                        P.op("dve", lambda e: e.tensor_reduce(out=gs[:, 5, :], in_=puv, axis=AX.X, op=ALU.max), w=[bgs, bpu])
                        Um, bUm = Umr.next()
                        P.op("dve", lambda e: e.tensor_tensor(Um[:], puv, negm[dr].unsqueeze(1).to_broadcast([128, 4, 128]), ALU.add), r=[b_cst], w=[bUm, bpu])
                        P.op("dve", lambda e: e.tensor_reduce(out=gs[:, 6, :], in_=Um[:], axis=AX.X, op=ALU.max), r=[bUm], w=[bgs])
                        P.op("dve", lambda e: e.tensor_tensor(gs[:, 3, :], gs[:, 6, :], mprev[:], ALU.max), r=[b_mprev], w=[bgs])
                        P.op("dve", lambda e: e.tensor_tensor(gs[:, 4, :], gs[:, 5, :], mprev[:], ALU.max), r=[b_mprev], w=[bgs])
                        P.op("dve", lambda e: e.tensor_tensor(gs[:, 11, :], gs[:, 2, :], gs[:, 4, :], ALU.subtract), w=[bgs])
                        P.op("act", lambda e: e.activation(gs[:, 7, :], gs[:, 11, :], AF.Exp, bias=-0.5 * math.log(128.0)), w=[bgs])
                        P.op("dve", lambda e: e.tensor_tensor(gs[:, 12, :], gs[:, 4, :], gs[:, 3, :], ALU.subtract), w=[bgs])
                        P.op("act", lambda e: e.activation(gs[:, 8, :], gs[:, 12, :], AF.Exp), w=[bgs])
                        P.op("dve", lambda e: e.tensor_tensor(gs[:, 13, :], mprev[:], gs[:, 4, :], ALU.subtract), r=[b_mprev], w=[bgs])
                        P.op("act", lambda e: e.activation(gs[:, 9, :], gs[:, 13, :], AF.Exp), w=[bgs])
                        P.op("dve", lambda e: e.tensor_tensor(gs[:, 14, :], cs, gs[:, 3, :], ALU.subtract), w=[bgs, bpg])
                        P.op("act", lambda e: e.activation(gs[:, 10, :], gs[:, 14, :], AF.Exp), w=[bgs])
                        P.op("dve", lambda e: e.tensor_tensor(mprev[:], gs[:, 4, :], tot, ALU.subtract), r=[bgs], w=[b_mprev, bpg])
                        Vm, bVm = Vmr.next()
                        P.op("dve", lambda e: e.tensor_tensor(Vm[:, :, 0:256], MV[:, j, :].rearrange("p (h e) -> p h e", h=4),
                                                              gs[:, 7, :].unsqueeze(2).to_broadcast([128, 4, 256]), ALU.mult), r=[bMV, bgs], w=[bVm])
                        P.op("pool", lambda e: e.tensor_copy(Vm[:, :, 256:257], gs[:, 7, :].unsqueeze(2)), r=[bgs], w=[bVm])
                        P.op("pool", lambda e: e.tensor_tensor(C32[:], C32[:], gs[:, 9, :].unsqueeze(2).to_broadcast([128, 4, 257]), ALU.mult), r=[bgs], w=[b_C32])
                        P.op("act", lambda e: e.copy(Cbf[:], C32[:]), r=[b_C32], w=[b_Cbf])
                        MKt, bMKt = MKtr.next()
                        pt, bp = psx[0].next()
                        ptb = pt[:].bitcast(BF16)
                        for h in range(4):
                            P.op("pe", lambda e: e.transpose(ptb[:, h * 128:(h + 1) * 128], MQK[:, 4 + h, j * 128:(j + 1) * 128], identb[:]),
                                 r=[bMQK, b_identb], w=[bp])
                        P.op("act", lambda e: e.copy(MKt[:].rearrange("p h d -> p (h d)"), ptb[:, 0:512]), w=[bMKt, bp])
                        if with_out:
                            ATm, bATm = ATmr.next()
                            pa, bpa = psx[0].next()
                            for h in range(4):
                                P.op("pe", lambda e: e.matmul(pa[:, h * 128:(h + 1) * 128], lhsT=MQK[:, 4 + h, j * 128:(j + 1) * 128],
                                                              rhs=MQK[:, h, j * 128:(j + 1) * 128], start=True, stop=True), r=[bMQK], w=[bpa])
                            P.op("dve", lambda e: e.tensor_tensor(ATm[:], pa[:, 0:512].rearrange("p (h c) -> p h c", h=4),
                                                                  maskT.unsqueeze(1).to_broadcast([128, 4, 128]), ALU.mult), r=[b_cst], w=[bATm, bpa])
                            dn, bdn = dnr.next()
                            for h in range(4):
                                ph_, bph = psx[0].next()
                                P.op("pe", lambda e: e.matmul(ph_[:, 0:257], lhsT=ATm[:, h, :], rhs=Vm[:, h, :], start=True, stop=False), r=[bATm, bVm], w=[bph])
                                P.op("pe", lambda e: e.matmul(ph_[:, 0:257], lhsT=MQK[:, h, j * 128:(j + 1) * 128], rhs=Cbf[:, h, :], start=False, stop=True),
                                     r=[bMQK, b_Cbf], w=[bph])
                                P.op("act", lambda e: e.activation(dn[:, h, 0:1], ph_[:, 256:257], AF.Abs), w=[bdn, bph])
                                P.op("dve", lambda e: e.tensor_scalar(dn[:, h, 1:2], dn[:, h, 0:1], gs[:, 8, h:h + 1], gs[:, 10, h:h + 1], op0=ALU.mult, op1=ALU.max),
                                     r=[bgs], w=[bdn])
                                P.op("dve", lambda e: e.reciprocal(dn[:, h, 2:3], dn[:, h, 1:2]), w=[bdn])
                                P.op("dve", lambda e: e.tensor_tensor(dn[:, h, 3:4], dn[:, h, 2:3], gs[:, 8, h:h + 1], ALU.mult), r=[bgs], w=[bdn])
                                P.op("act", lambda e: e.activation(Y[:, 1024 + h * 256:1024 + (h + 1) * 256], ph_[:, 0:256], AF.Identity, scale=dn[:, h, 3:4]),
                                     r=[bdn], w=[bYm, bph])
                        for h in range(4):
                            pc, bpc = psx[0].next()
                            P.op("pe", lambda e: e.matmul(pc[:, 0:257], lhsT=MKt[:, h, :], rhs=Vm[:, h, :], start=True, stop=True), r=[bMKt, bVm], w=[bpc])
                            P.op("dve", lambda e: e.tensor_tensor(C32[:, h, :], C32[:, h, :], pc[:, 0:257], ALU.add), w=[b_C32, bpc])
                        P.rec = lst_O
                        psx[0] = psO
                        if with_out and dr == 0:
                            P.dma(yA_d[tok0:tok0 + 128, :], Y[:], r=[bY, bYm], w=[B_yA], sb=bY)
                        if with_out and dr == 1:
                            YA, bYA = YAr.next()
                            P.dma(YA[:], yA_d[tok0:tok0 + 128, :], r=[B_yA], w=[bYA], sb=bYA)
                            P.op("dve", lambda e: e.tensor_tensor(Y[:], Y[:], YA[:], ALU.add), r=[bYA], w=[bY, bYm])
                            ns, bns = nsr.next()
                            hn, bhn = Y, bY
                            sq, bsq = sqr.next()
                            yo, byo = yor.next()
                            for (gi, c0, ng, gw) in ((0, 0, 8, 128), (1, 1024, 4, 256)):
                                yv = Y[:, c0:c0 + 1024].rearrange("p (g e) -> p g e", g=ng)
                                hv = hn[:, c0:c0 + 1024].rearrange("p (g e) -> p g e", g=ng)
                                sv2 = sq[:, c0:c0 + 1024].rearrange("p (g e) -> p g e", g=ng)
                                st_ = ns[:, gi * 2:gi * 2 + 2, :]
                                P.op("dve", lambda e: e.tensor_reduce(out=st_[:, 0, 0:ng], in_=yv, axis=AX.X, op=ALU.add), r=[bY], w=[bns])
                                P.op("dve", lambda e: e.tensor_scalar(st_[:, 0, 0:ng], st_[:, 0, 0:ng], 1.0 / gw, None, op0=ALU.mult), w=[bns])
                                P.op("dve", lambda e: e.tensor_tensor(hv, yv, st_[:, 0, 0:ng].unsqueeze(2).to_broadcast([128, ng, gw]), ALU.subtract),
                                     r=[bY, bns], w=[bhn])
                                P.op("pool", lambda e: e.tensor_tensor(sv2, hv, hv, ALU.mult), r=[bhn], w=[bsq])
                                P.op("dve", lambda e: e.tensor_reduce(out=st_[:, 1, 0:ng], in_=sv2, axis=AX.X, op=ALU.add), r=[bsq], w=[bns])
                                P.op("dve", lambda e: e.tensor_scalar(st_[:, 1, 0:ng], st_[:, 1, 0:ng], 1.0 / gw, EPS, op0=ALU.mult, op1=ALU.add), w=[bns])
                                P.op("pool", lambda e: e.tensor_tensor(st_[:, 1, 0:ng], st_[:, 1, 0:ng], mhalf[:, 0:ng], ALU.pow), r=[b_mhalf], w=[bns])
                                P.op("dve", lambda e: e.tensor_tensor(hv, hv, st_[:, 1, 0:ng].unsqueeze(2).to_broadcast([128, ng, gw]), ALU.mult),
                                     r=[bns], w=[bhn])
                                gate, bgate = (RG, bRG) if gi == 0 else (MO, bMO)
                                P.op("pool", lambda e: e.tensor_tensor(yo[:, c0:c0 + 1024], hn[:, c0:c0 + 1024], gate[:, j, :], ALU.mult),
                                     r=[bhn, bgate, bYm], w=[byo])
                            for half in range(2):
                                pt, bp = psx[0].next()
                                ptb = pt[:].bitcast(BF16)
                                for kk in range(8):
                                    k = half * 8 + kk
                                    P.op("pe", lambda e: e.transpose(ptb[:, kk * 128:(kk + 1) * 128], yo[:, k * 128:(k + 1) * 128], identb[:]),
                                         r=[byo, b_identb], w=[bp])
                                ov = YOT[:, half * 8:(half + 1) * 8, j * 128:(j + 1) * 128]
                                iv = ptb[:, 0:1024].rearrange("p (k t) -> p k t", k=8)
                                if half == 0:
                                    P.op("act", lambda e: e.copy(ov, iv), w=[bYOT, bp])
                                else:
                                    P.op("dve", lambda e: e.tensor_copy(ov, iv), w=[bYOT, bp])
                        P.rec = None
                        psx[0] = psM
                        P.zip_emit([lst_M, lst_R, pend_out])
                        pend_out = lst_O
                    P.zip_emit([pend_out])
                    if with_out and dr == 1:
                        P.dma(yoT_d.rearrange("(k p) n -> p k n", p=128)[:, :, base:base + NT], YOT[:, :, 0:NT], r=[bYOT], w=[B_yoT], sb=bYOT)

            scan_pass(0)
            scan_pass(1)
        barrier()

        with ExitStack() as ph:
            gt1b, b_gt1b = sbt(ph, "gt1b", [128, D], F32)
            P.dma(gt1b[:], ada_d[0:1, 2 * D:3 * D].partition_broadcast(128), r=[B_ada], w=[b_gt1b], sb=b_gt1b)
            XTr = Ring(nc, ph, "cXT", [128, KC, 512], BF16, 1)
            YTr = Ring(nc, ph, "cYT", [128, KC, 512], BF16, 1)
            MTr = Ring(nc, ph, "cMT", [128, KC, 512], BF16, 1, multi=True)
            Wr = Ring(nc, ph, "cW", [128, KC, 512], BF16, 3)
            sgr = Ring(nc, ph, "sg", [128, 512], F32, 2)
            t1r = Ring(nc, ph, "t1", [128, 512], F32, 2)
            t2r = Ring(nc, ph, "t2", [128, 512], F32, 2)
            xr = Ring(nc, ph, "cx", [128, D], F32, 4, multi=True)
            for s in range(NST):
                base = s * 512
                XT, bXT = XTr.next()
                YT, bYT = YTr.next()
                MT, bMT = MTr.next()
                P.dma(XT[:], xmT_d.rearrange("(k p) n -> p k n", p=128)[:, :, 1 + base:1 + base + 512], r=[B_xmT], w=[bXT], sb=bXT)
                P.dma(YT[:], yoT_d.rearrange("(k p) n -> p k n", p=128)[:, :, base:base + 512], r=[B_yoT], w=[bYT], sb=bYT)
                xs = []
                for j in range(4):
                    xt, bx = xr.next()
                    P.dma(xt[:], x_d[base + j * 128:base + (j + 1) * 128, :], w=[bx], sb=bx)
                    xs.append((xt, bx))
                for cb in range(4):
                    Wgr, bWgr = Wr.next()
                    P.dma(Wgr[:], winb_d[:, O_GR + cb * 512:O_GR + (cb + 1) * 512].rearrange("(k p) n -> p k n", p=128), r=[B_winb], w=[bWgr], sb=bWgr)
                    Wgm, bWgm = Wr.next()
                    P.dma(Wgm[:], winb_d[:, O_GM + cb * 512:O_GM + (cb + 1) * 512].rearrange("(k p) n -> p k n", p=128), r=[B_winb], w=[bWgm], sb=bWgm)
                    Wo, bWo = Wr.next()
                    P.dma(Wo[:, 0:8, :], wrob_d[:, cb * 512:(cb + 1) * 512].rearrange("(k p) n -> p k n", p=128), r=[B_wrob], w=[bWo], sb=bWo)
                    P.dma(Wo[:, 8:16, :], wmob_d[:, cb * 512:(cb + 1) * 512].rearrange("(k p) n -> p k n", p=128), r=[B_wmob], w=[bWo], sb=bWo)
                    for m in range(4):
                        db = cb * 4 + m
                        res = []
                        for (Wg, bWg, k0, tr_) in ((Wgr, bWgr, 0, t1r), (Wgm, bWgm, 8, t2r)):
                            pg_, bpg_ = psr.next()
                            for k in range(KC):
                                P.op("pe", lambda e: e.matmul(pg_[:, 0:512], lhsT=Wg[:, k, m * 128:(m + 1) * 128], rhs=XT[:, k, :],
                                                              start=(k == 0), stop=(k == KC - 1)), r=[bWg, bXT], w=[bpg_])
                            sg, bsg = sgr.next()
                            P.op("act", lambda e: e.activation(sg[:], pg_[:, 0:512], AF.Sigmoid), w=[bsg, bpg_])
                            pp, bpp = psr.next()
                            for k in range(8):
                                P.op("pe", lambda e: e.matmul(pp[:, 0:512], lhsT=Wo[:, k0 + k, m * 128:(m + 1) * 128], rhs=YT[:, k0 + k, :],
                                                              start=(k == 0), stop=(k == 7)), r=[bWo, bYT], w=[bpp])
                            tt, btt = tr_.next()
                            P.op("dve", lambda e: e.tensor_tensor(tt[:], pp[:, 0:512], sg[:], ALU.mult), r=[bsg], w=[btt, bpp])
                            res.append((tt, btt))
                        P.op("pool", lambda e: e.tensor_tensor(MT[:, db, :], res[0][0][:], res[1][0][:], ALU.add), r=[res[0][1], res[1][1]], w=[bMT])
                for cb in range(4):
                    Wo, bWo = Wr.next()
                    P.dma(Wo[:], woutb_d[:, cb * 512:(cb + 1) * 512].rearrange("(k p) n -> p k n", p=128), r=[B_woutb], w=[bWo], sb=bWo)
                    for j in range(4):
                        po, bpo = psr.next()
                        for k in range(KC):
                            P.op("pe", lambda e: e.matmul(po[:, 0:512], lhsT=MT[:, k, j * 128:(j + 1) * 128], rhs=Wo[:, k, :],
                                                          start=(k == 0), stop=(k == KC - 1)), r=[bMT, bWo], w=[bpo])
                        tt, btt = t1r.next()
                        P.op("dve", lambda e: e.tensor_tensor(tt[:], po[:, 0:512], gt1b[:, cb * 512:(cb + 1) * 512], ALU.mult), r=[b_gt1b], w=[btt, bpo])
                        xt, bx = xs[j]
                        P.op("pool", lambda e: e.tensor_tensor(xt[:, cb * 512:(cb + 1) * 512], xt[:, cb * 512:(cb + 1) * 512], tt[:], ALU.add), r=[btt], w=[bx])
                for j in range(4):
                    xt, bx = xs[j]
                    P.dma(x1_d[base + j * 128:base + (j + 1) * 128, :], xt[:], r=[bx], w=[B_x1], sb=bx)
        barrier()

        with ExitStack() as ph:
            Wq, b_Wq = sbt(ph, "Wq", [128, KC, D], BF16)
            for cb in range(4):
                P.dma(Wq[:, :, cb * 512:(cb + 1) * 512], pqb_d[:, cb * 512:(cb + 1) * 512].rearrange("(k p) n -> p k n", p=128), r=[B_pqb], w=[b_Wq], sb=b_Wq)
            keysT, b_keysT = sbt(ph, "keysT", [128, 16, 128], BF16)
            with ExitStack() as ph2:
                kf, b_kf = sbt(ph2, "kf", [128, 16, 128], F32)
                P.dma(kf[:], pk_d.rearrange("(g k) d -> k g d", k=128), w=[b_kf], sb=b_kf)
                for g4 in range(4):
                    pt, bp = psr.next()
                    for gg in range(4):
                        g = g4 * 4 + gg
                        P.op("pe", lambda e: e.transpose(pt[:, gg * 128:(gg + 1) * 128], kf[:, g, :], identf), r=[b_kf, b_cst], w=[bp])
                    P.op("dve", lambda e: e.tensor_copy(keysT[:, g4 * 4:(g4 + 1) * 4, :], pt[:, 0:512].rearrange("p (g k) -> p g k", g=4)), w=[b_keysT, bp])
            barrier()
            gt2b, b_gt2b = sbt(ph, "gt2b", [128, D], F32)
            fgb, b_fgb = sbt(ph, "fgb", [128, D], F32)
            P.dma(gt2b[:], ada_d[0:1, 5 * D:6 * D].partition_broadcast(128), r=[B_ada], w=[b_gt2b], sb=b_gt2b)
            P.dma(fgb[:], fg_d.partition_broadcast(128), w=[b_fgb], sb=b_fgb)
            iota16, b_iota = sbt(ph, "iota16", [128, 16], F32)
            for i in range(16):
                P.op("dve", lambda e: e.memset(iota16[:, i:i + 1], float(i)), w=[b_iota])
            x1r = Ring(nc, ph, "px1", [128, D], F32, 1)
            hnbr = Ring(nc, ph, "phn", [128, D], BF16, 1)
            hbr = Ring(nc, ph, "phb", [128, D], BF16, 2, multi=True)
            hTr = Ring(nc, ph, "phT", [128, KC, 128], BF16, 1, multi=True)
            qTr = Ring(nc, ph, "pqT", [128, 16, 128], BF16, 1, multi=True)
            Scr = Ring(nc, ph, "pSc", [128, 16, 128], F32, 1, multi=True)
            wkr = Ring(nc, ph, "pwk", [128, 16, 128], F32, 1)
            mxr = Ring(nc, ph, "pmx", [128, 16, 16], F32, 1)
            mir = Ring(nc, ph, "pmi", [128, 16, 16], U32, 1)
            mifr = Ring(nc, ph, "pmif", [128, 16, 16], F32, 1)
            bsr = Ring(nc, ph, "pbs", [128, 8, 16], F32, 1)
            bjr = Ring(nc, ph, "pbj", [128, 8, 16], U32, 1)
            ijr = Ring(nc, ph, "pij", [128, 4, 8, 16], F32, 1)
            iju = Ring(nc, ph, "piju", [128, 2, 8, 16], U32, 1)
            eqr = Ring(nc, ph, "peq", [128, 4, 16, 16], F32, 1)
            eidr = Ring(nc, ph, "peid", [128, 128], I32, 2)
            gwr = Ring(nc, ph, "pgw", [128, 2, 128], F32, 2)
            avr = Ring(nc, ph, "pav", [128, 3, 128], F32, 2, multi=True)
            Gr_ = Ring(nc, ph, "pG", [128, 2 * D], BF16, 6)
            jkr = Ring(nc, ph, "pjk", [128, D], BF16, 1)
            dgr = Ring(nc, ph, "pdg", [128, 128], BF16, 4)
            str_ = Ring(nc, ph, "pst", [128, 4], F32, 4)
            pacc = [psr.items[4 + i] for i in range(4)]
            psr.items = psr.items[0:4]
            psr.i = 0

            def rms_rstd(src, bsrc):
                st, bs = str_.next()
                jk, bjk = jkr.next()
                P.op("dve", lambda e: e.scalar_tensor_tensor(out=jk[:], in0=src[:], scalar=1.0, in1=src[:], op0=ALU.mult, op1=ALU.mult, accum_out=st[:, 0:1]),
                     r=[bsrc], w=[bjk, bs])
                P.op("dve", lambda e: e.tensor_scalar(st[:, 1:2], st[:, 0:1], 1.0 / D, EPS, op0=ALU.mult, op1=ALU.add), w=[bs])
                P.op("act", lambda e: e.activation(st[:, 2:3], st[:, 1:2], AF.Sqrt), w=[bs])
                P.op("dve", lambda e: e.reciprocal(st[:, 3:4], st[:, 2:3]), w=[bs])
                return st, bs

            def stage1(blk, res):
                x1, bx1 = x1r.next()
                P.dma(x1[:], x1_d[blk * 128:(blk + 1) * 128, :], r=[B_x1], w=[bx1], sb=bx1)
                st, bs = rms_rstd(x1, bx1)
                yield
                hnb, bhnb = hnbr.next()
                P.op("act", lambda e: e.activation(hnb[:], x1[:], AF.Identity, scale=st[:, 3:4]), r=[bx1, bs], w=[bhnb])
                yield
                hT, bhT = hTr.next()
                for half in range(2):
                    pt, bp = psr.next()
                    ptb = pt[:].bitcast(BF16)
                    for kk in range(8):
                        k = half * 8 + kk
                        P.op("pe", lambda e: e.transpose(ptb[:, kk * 128:(kk + 1) * 128], hnb[:, k * 128:(k + 1) * 128], identb[:]), r=[bhnb, b_identb], w=[bp])
                    for kk in range(8):
                        k = half * 8 + kk
                        if half == 0:
                            P.op("act", lambda e: e.activation(hT[:, k, :], ptb[:, kk * 128:(kk + 1) * 128], AF.Identity, bias=modF[:, 5, k:k + 1], scale=modF[:, 4, k:k + 1]),
                                 r=[b_modF], w=[bhT, bp])
                        else:
                            P.op("dve", lambda e: e.tensor_scalar(hT[:, k, :], ptb[:, kk * 128:(kk + 1) * 128], modF[:, 4, k:k + 1], modF[:, 5, k:k + 1], op0=ALU.mult, op1=ALU.add),
                                 r=[b_modF], w=[bhT, bp])
                    yield
                hb, bhb = hbr.next()
                for half in range(2):
                    pt, bp = psr.next()
                    ptb = pt[:].bitcast(BF16)
                    for kk in range(8):
                        k = half * 8 + kk
                        P.op("pe", lambda e: e.transpose(ptb[:, kk * 128:(kk + 1) * 128], hT[:, k, :], identb[:]), r=[bhT, b_identb], w=[bp])
                    P.op("act", lambda e: e.copy(hb[:, half * 1024:(half + 1) * 1024], ptb[:, 0:1024]), w=[bhb, bp])
                    yield
                qT, bqT = qTr.next()
                for g4 in range(4):
                    pt, bp = psr.next()
                    for gg in range(4):
                        g = g4 * 4 + gg
                        for k in range(KC):
                            P.op("pe", lambda e: e.matmul(pt[:, gg * 128:(gg + 1) * 128], lhsT=Wq[:, k, g * 128:(g + 1) * 128], rhs=hT[:, k, :],
                                                          start=(k == 0), stop=(k == KC - 1)), r=[b_Wq, bhT], w=[bp])
                    P.op("act", lambda e: e.copy(qT[:, g4 * 4:(g4 + 1) * 4, :], pt[:, 0:512].rearrange("p (g t) -> p g t", g=4)), w=[bqT, bp])
                    yield
                Sc, bSc = Scr.next()
                for g4 in range(4):
                    pt, bp = psr.next()
                    for gg in range(4):
                        g = g4 * 4 + gg
                        P.op("pe", lambda e: e.matmul(pt[:, gg * 128:(gg + 1) * 128], lhsT=qT[:, g, :], rhs=keysT[:, g, :], start=True, stop=True),
                             r=[bqT, b_keysT], w=[bp])
                    P.op("act", lambda e: e.copy(Sc[:, g4 * 4:(g4 + 1) * 4, :], pt[:, 0:512].rearrange("p (g t) -> p g t", g=4)), w=[bSc, bp])
                    yield
                mx, bmx = mxr.next()
                mi, bmi = mir.next()
                wk, bwk = wkr.next()
                for g in range(16):
                    P.op("dve", lambda e: e.max(out=mx[:, g, 0:8], in_=Sc[:, g, :]), r=[bSc], w=[bmx])
                    yield
                    P.op("dve", lambda e: e.max_index(out=mi[:, g, 0:8], in_max=mx[:, g, 0:8], in_values=Sc[:, g, :]), r=[bSc, bmx], w=[bmi])
                    yield
                    P.op("dve", lambda e: e.match_replace(out=wk[:, g, :], in_to_replace=mx[:, g, 0:8], in_values=Sc[:, g, :], imm_value=NEG), r=[bSc, bmx], w=[bwk])
                    yield
                    P.op("dve", lambda e: e.max(out=mx[:, g, 8:16], in_=wk[:, g, :]), r=[bwk], w=[bmx])
                    yield
                    P.op("dve", lambda e: e.max_index(out=mi[:, g, 8:16], in_max=mx[:, g, 8:16], in_values=wk[:, g, :]), r=[bwk, bmx], w=[bmi])
                    yield
                    yield
                mif, bmif = mifr.next()
                P.op("dve", lambda e: e.tensor_copy(mif[:], mi[:]), r=[bmi], w=[bmif])
                cand_t, bcand = wkr.next()
                cand = cand_t[:].rearrange("p (h a) k -> p h (a k)", a=2)
                mxv = mx[:].rearrange("p (h t) i -> p h t i", t=2)
                P.op("pool", lambda e: e.tensor_tensor(cand.rearrange("p h (i j) -> p h i j", i=16),
                                                      mxv[:, :, 0, :].unsqueeze(3).to_broadcast([128, 8, 16, 16]),
                                                      mxv[:, :, 1, :].unsqueeze(2).to_broadcast([128, 8, 16, 16]), ALU.add), r=[bmx], w=[bcand])
                yield
                bs_, bbs = bsr.next()
                bj, bbj = bjr.next()
                cw2_t, bcw2 = Scr.next()
                cw2 = cw2_t[:].rearrange("p (h a) k -> p h (a k)", a=2)
                for h in range(8):
                    P.op("dve", lambda e: e.max(out=bs_[:, h, 0:8], in_=cand[:, h, :]), r=[bcand], w=[bbs])
                    yield
                    P.op("dve", lambda e: e.max_index(out=bj[:, h, 0:8], in_max=bs_[:, h, 0:8], in_values=cand[:, h, :]), r=[bcand, bbs], w=[bbj])
                    yield
                    P.op("dve", lambda e: e.match_replace(out=cw2[:, h, :], in_to_replace=bs_[:, h, 0:8], in_values=cand[:, h, :], imm_value=NEG), r=[bcand, bbs], w=[bcw2])
                    yield
                    P.op("dve", lambda e: e.max(out=bs_[:, h, 8:16], in_=cw2[:, h, :]), r=[bcw2], w=[bbs])
                    yield
                    P.op("dve", lambda e: e.max_index(out=bj[:, h, 8:16], in_max=bs_[:, h, 8:16], in_values=cw2[:, h, :]), r=[bcw2, bbs], w=[bbj])
                    yield
                    yield
                ju, bju = iju.next()
                ij, bij = ijr.next()
                P.op("dve", lambda e: e.tensor_single_scalar(ju[:, 0], bj[:], 4, op=ALU.logical_shift_right), r=[bbj], w=[bju])
                P.op("dve", lambda e: e.tensor_single_scalar(ju[:, 1], bj[:], 15, op=ALU.bitwise_and), r=[bbj], w=[bju])
                P.op("dve", lambda e: e.tensor_copy(ij[:, 0:2], ju[:]), r=[bju], w=[bij])
                mifv = mif[:].rearrange("p (h t) i -> p h t i", t=2)
                for t in range(2):
                    for hh in range(2):
                        hs = slice(hh * 4, (hh + 1) * 4)
                        eq, beq = eqr.next()
                        P.op("dve", lambda e: e.tensor_tensor(eq[:], ij[:, t, hs].unsqueeze(3).to_broadcast([128, 4, 16, 16]),
                                                               iota16[:].unsqueeze(1).unsqueeze(1).to_broadcast([128, 4, 16, 16]), ALU.is_equal), r=[bij, b_iota], w=[beq])
                        P.op("pool", lambda e: e.tensor_tensor(eq[:], eq[:], mifv[:, hs, t, :].unsqueeze(2).to_broadcast([128, 4, 16, 16]), ALU.mult), r=[bmif], w=[beq])
                        P.op("dve", lambda e: e.tensor_reduce(out=ij[:, 2 + t, hs], in_=eq[:], axis=AX.X, op=ALU.add), r=[beq], w=[bij])
                        yield
                P.op("dve", lambda e: e.scalar_tensor_tensor(out=ij[:, 0], in0=ij[:, 2], scalar=128.0, in1=ij[:, 3], op0=ALU.mult, op1=ALU.add), w=[bij])
                eid, beid = eidr.next()
                P.op("dve", lambda e: e.tensor_copy(eid[:], ij[:, 0].rearrange("p h k -> p (h k)")), r=[bij], w=[beid])
                yield
                gw, bgw = gwr.next()
                gwv = gw[:, 0, :].rearrange("p (h k) -> p h k", h=8)
                P.op("dve", lambda e: e.tensor_tensor(gwv, bs_[:], bs_[:, :, 0:1].to_broadcast([128, 8, 16]), ALU.subtract), r=[bbs], w=[bgw])
                P.op("act", lambda e: e.activation(gw[:, 0, :], gw[:, 0, :], AF.Exp), w=[bgw])
                P.op("dve", lambda e: e.tensor_reduce(out=gw[:, 1, 0:8], in_=gwv, axis=AX.X, op=ALU.add), w=[bgw])
                P.op("dve", lambda e: e.reciprocal(gw[:, 1, 0:8], gw[:, 1, 0:8]), w=[bgw])
                P.op("dve", lambda e: e.tensor_tensor(gwv, gwv, gw[:, 1, 0:8].unsqueeze(2).to_broadcast([128, 8, 16]), ALU.mult), w=[bgw])
                res.update(hb=hb, bhb=bhb, eid=eid, beid=beid, gw=gw, bgw=bgw)

            def stage2(blk, s_, bg=None):
                hb, bhb, eid, beid, gw, bgw = (s_[k_] for k_ in ("hb", "bhb", "eid", "beid", "gw", "bgw"))
                av, bav = avr.next()
                jk, bjk = jkr.next()
                pend = None
                bsl = [Buf("sl%d" % i_) for i_ in range(128)]
                for b__ in bsl:
                    b__.w = dict(bav.base)

                def tail(slot, Gt, bGt):
                    P.op("dve", lambda e: e.tensor_tensor(av[:, 2, slot:slot + 1], av[:, 1, slot:slot + 1], gw[:, 0, slot:slot + 1], ALU.mult), r=[bsl[slot], bgw], w=[bsl[slot]])
                    dg, bdg = dgr.next()
                    P.op("act", lambda e: e.activation(dg[:], identb[:], AF.Identity, scale=av[:, 2, slot:slot + 1]), r=[b_identb, bsl[slot]], w=[bdg])
                    for cb in range(4):
                        po, bpo = pacc[cb]
                        P.op("pe", lambda e: e.matmul(po[:, 0:512], lhsT=dg[:], rhs=Gt[:, D + cb * 512:D + (cb + 1) * 512], start=(slot == 0), stop=(slot == 127)),
                             r=[bdg, bGt], w=[bpo])

                for slot in range(128):
                    Gt, bGt = Gr_.next()
                    P.gather(Gt[:], pcomb_d, eid[:, slot:slot + 1].bitcast(U32), r=[beid, B_pdnb, B_pupb], w=[bGt], sb=bGt)
                    P.op("dve", lambda e: e.scalar_tensor_tensor(out=jk[:], in0=Gt[:, 0:D], scalar=1.0, in1=hb[:], op0=ALU.mult, op1=ALU.mult,
                                                                 accum_out=av[:, 0, slot:slot + 1]), r=[bGt, bhb], w=[bsl[slot]])
                    P.op("act", lambda e: e.activation(av[:, 1, slot:slot + 1], av[:, 0, slot:slot + 1], AF.Gelu), r=[bsl[slot]], w=[bsl[slot]])
                    if pend is not None:
                        tail(*pend)
                    pend = (slot, Gt, bGt)
                    if bg is not None and slot >= 4:
                        next(bg, None)
                        next(bg, None)
                tail(*pend)
                if bg is not None:
                    for _ in bg:
                        pass
                for b__ in bsl:
                    for k__, v__ in list(b__.w.items()) + list(b__.r.items()):
                        if bav.w.get(k__, 0) < v__:
                            bav.w[k__] = v__
                tmG, btm = Gr_.next()
                tm = tmG[:].bitcast(F32)
                x1G, bx1 = Gr_.next()
                x1 = x1G[:].bitcast(F32)
                P.dma(x1, x1_d[blk * 128:(blk + 1) * 128, :], r=[B_x1], w=[bx1], sb=bx1)
                for cb in range(4):
                    po, bpo = pacc[cb]
                    P.op("dve", lambda e: e.tensor_tensor(tm[:, cb * 512:(cb + 1) * 512], po[:, 0:512], gt2b[:, cb * 512:(cb + 1) * 512], ALU.mult), r=[b_gt2b], w=[btm, bpo])
                P.op("pool", lambda e: e.tensor_tensor(x1, x1, tm, ALU.add), r=[btm], w=[bx1])
                st2, bs2 = str_.next()
                jk, bjk = jkr.next()
                P.op("dve", lambda e: e.scalar_tensor_tensor(out=jk[:], in0=x1, scalar=1.0, in1=x1, op0=ALU.mult, op1=ALU.mult, accum_out=st2[:, 0:1]),
                     r=[bx1], w=[bjk, bs2])
                P.op("dve", lambda e: e.tensor_scalar(st2[:, 1:2], st2[:, 0:1], 1.0 / D, EPS, op0=ALU.mult, op1=ALU.add), w=[bs2])
                P.op("act", lambda e: e.activation(st2[:, 2:3], st2[:, 1:2], AF.Sqrt), w=[bs2])
                P.op("dve", lambda e: e.reciprocal(st2[:, 3:4], st2[:, 2:3]), w=[bs2])
                P.op("pool", lambda e: e.scalar_tensor_tensor(out=tm, in0=x1, scalar=st2[:, 3:4], in1=fgb[:], op0=ALU.mult, op1=ALU.mult), r=[bx1, bs2, b_fgb], w=[btm]) if False else \
                    P.op("dve", lambda e: e.scalar_tensor_tensor(out=tm, in0=x1, scalar=st2[:, 3:4], in1=fgb[:], op0=ALU.mult, op1=ALU.mult), r=[bx1, bs2, b_fgb], w=[btm])
                P.dma(out_d[blk * 128:(blk + 1) * 128, :], tm, r=[btm], w=[B_out], sb=btm)

            NBLK = H // 128
            cur = {}
            for _ in stage1(0, cur):
                pass
            for blk in range(NBLK):
                nxt = {}
                bg = stage1(blk + 1, nxt) if blk + 1 < NBLK else None
                stage2(blk, cur, bg)
                cur = nxt
        P.finish()
    return nc


_PROG_CACHE = {}


def _host_inputs(inp, L, cores):
    f32 = np.float32
    x = np.asarray(inp["x"], f32)
    ctx = np.asarray(inp["ctx"], f32)
    c = np.asarray(inp["c"], f32)
    c_ctx = np.asarray(inp["c_ctx"], f32)
    w_in = np.ascontiguousarray(np.asarray(inp["w_in"], f32)[0])
    shared = dict(
        w_ada=np.ascontiguousarray(np.asarray(inp["w_ada"], f32)[0]),
        b_ada=np.asarray(inp["b_ada"], f32).reshape(1, -1),
        norm1_g=np.asarray(inp["norm1_g"], f32).reshape(1, -1),
        w_in=w_in,
        w_ret_out=np.ascontiguousarray(np.asarray(inp["w_ret_out"], f32)[0]),
        w_mlstm_out=np.ascontiguousarray(np.asarray(inp["w_mlstm_out"], f32)[0]),
        w_out=np.ascontiguousarray(np.asarray(inp["w_out"], f32)[0]),
        norm2_g=np.asarray(inp["norm2_g"], f32).reshape(1, -1),
        peer_query=np.ascontiguousarray(np.asarray(inp["peer_query"], f32)[0]),
        peer_keys=np.ascontiguousarray(np.asarray(inp["peer_keys"], f32)[0].reshape(16 * 128, 128)),
        peer_down=np.ascontiguousarray(np.asarray(inp["peer_down"], f32)[0]),
        peer_up=np.ascontiguousarray(np.asarray(inp["peer_up"], f32)[0]),
        final_g=np.asarray(inp["final_g"], f32).reshape(1, -1),
    )
    p = np.arange(128)
    ident = (p[:, None] == p[None, :]).astype(f32)
    triA = (p[:, None] <= p[None, :]).astype(f32)
    triB = (p[:, None] >= p[None, :]).astype(f32)
    negA = (triB - 1.0) * 1.0e30
    negB = (triA - 1.0) * 1.0e30
    ones = np.ones((128, 128), f32)
    cst = np.ascontiguousarray(np.stack([ident, triA, triB, negA, negB, ones], axis=1).astype(f32))
    pos = np.zeros((128, 4), f32)
    pos[:, 0] = p + 1.0
    pos[:, 1] = 128.0 - p
    shared["cst"] = cst
    shared["pos"] = pos
    w_mg = w_in[:, O_MG:O_MG + 16]
    gbias = np.asarray(inp["m_gate_bias"], f32)[0].reshape(16)
    rdec = np.asarray(inp["ret_decay"], f32)[0]
    mconv = np.asarray(inp["m_conv"], f32)[0]
    n_ax = 16
    freq = (10000.0 ** (-np.arange(n_ax, dtype=np.float64) / n_ax))
    maps = []
    for (b, half) in cores:
        t = np.arange(L) if half == 0 else np.arange(L - 1, -1, -1)
        row = (t // 64).astype(np.float64)
        col = (t % 64).astype(np.float64)
        ang = np.concatenate([row[:, None] * freq, col[:, None] * freq], axis=-1)
        rot = np.concatenate([np.cos(ang), np.sin(ang)], axis=-1).astype(f32)
        m = dict(shared)
        if half == 0:
            m["x"] = np.ascontiguousarray(x[b])
            m["ctx"] = np.ascontiguousarray(ctx[b])
            m["w_mg"] = np.ascontiguousarray(w_mg)
            m["gbias"] = gbias.reshape(1, 16).copy()
            m["ret_decay"] = rdec.reshape(1, 16).copy()
            m["m_conv"] = np.ascontiguousarray(mconv)
        else:
            m["x"] = np.ascontiguousarray(x[b, ::-1])
            m["ctx"] = np.ascontiguousarray(ctx[b, ::-1])
            m["w_mg"] = np.ascontiguousarray(np.concatenate([w_mg[:, 8:16], w_mg[:, 0:8]], axis=1))
            m["gbias"] = np.concatenate([gbias[8:16], gbias[0:8]]).reshape(1, 16).copy()
            m["ret_decay"] = np.concatenate([rdec[1], rdec[0]]).reshape(1, 16).copy()
            m["m_conv"] = np.ascontiguousarray(mconv[::-1])
        m["cvec"] = np.ascontiguousarray(np.stack([c[b], c_ctx], axis=0))
        m["rot"] = rot
        maps.append(m)
    return maps


def kernel(**inputs):
    x = np.asarray(inputs["x"])
    B, L, _ = x.shape
    cores = [(b, h) for b in range(B) for h in range(2)]
    if L not in _PROG_CACHE:
        _PROG_CACHE[L] = build_program(L)
    nc = _PROG_CACHE[L]
    maps = _host_inputs(inputs, L, cores)
    res = run_bass_kernel_spmd(nc, maps, core_ids=list(range(len(cores))))
    out = np.empty((B, L, D), np.float32)
    Hh = L // 2
    for i, (b, h) in enumerate(cores):
        o = np.asarray(res.results[i]["out"], np.float32)
        if h == 0:
            out[b, 0:Hh] = o
        else:
            out[b, Hh:L] = o[::-1]
    return out
```

```python
import math
from contextlib import ExitStack
import numpy as np
import concourse.bass as bass
import concourse.mybir as mybir
from concourse.bass_utils import run_bass_kernel_spmd

F32 = mybir.dt.float32
BF16 = mybir.dt.bfloat16
I32 = mybir.dt.int32
U32 = mybir.dt.uint32
ALU = mybir.AluOpType
AF = mybir.ActivationFunctionType
AX = mybir.AxisListType

D = 2048
KC = 16
EPS = 1e-6
NEG = -1.0e30


class Buf:
    __slots__ = ("name", "w", "r", "multi", "sem", "base")

    def __init__(self, name, multi=False):
        self.name = name
        self.w = {}
        self.r = {}
        self.multi = multi
        self.sem = None
        self.base = {}

    def fresh(self):
        base = dict(self.w)
        for k, v in self.r.items():
            if base.get(k, 0) < v:
                base[k] = v
        self.base = base
        self.w = {}
        self.r = {}


class Prog:
    def __init__(self, nc, es):
        self.nc = nc
        self.es = es
        self.eng = {"pe": nc.tensor, "dve": nc.vector, "act": nc.scalar, "pool": nc.gpsimd, "sp": nc.sync}
        self.sems = {}
        self.cnt = {}
        self.waited = {e: {} for e in self.eng}
        for e in self.eng:
            self.sems[e] = es.enter_context(nc.semaphore("s_" + e))
            self.cnt[e] = 0
        self.ndsem = 0
        self.ninst = 0
        self.rec = None

    class _RecEng:
        def __init__(self):
            self.call = None

        def __getattr__(self, name):
            def f(*a, **k):
                self.call = (name, a, k)
                return None
            return f

    def zip_emit(self, lists):
        saved, self.rec = self.rec, None
        lists = [l for l in lists if l]
        idx = [0] * len(lists)
        total = sum(len(l) for l in lists)
        for _ in range(total):
            best = min((idx[i] / len(lists[i]), i) for i in range(len(lists)) if idx[i] < len(lists[i]))[1]
            it = lists[best][idx[best]]
            idx[best] += 1
            if it[0] == "fresh":
                it[1].fresh()
            elif it[0] == "op":
                _, e, name, a_, k_, r, w = it
                self.op(e, lambda eng, name=name, a_=a_, k_=k_: getattr(eng, name)(*a_, **k_), r, w)
            else:
                _, out, in_, r, w, sb, e, kw = it
                self.dma(out, in_, r=r, w=w, sb=sb, e=e, **kw)
        self.rec = saved

    def _deps(self, r, w):
        deps = {}

        def add(k, v):
            if deps.get(k, 0) < v:
                deps[k] = v
        for b in r:
            for k, v in b.w.items():
                add(k, v)
        for b in w:
            if not b.multi:
                for k, v in b.w.items():
                    add(k, v)
                for k, v in b.r.items():
                    add(k, v)
            else:
                for k, v in b.base.items():
                    add(k, v)
        return deps

    def _wait(self, e, deps):
        wd = self.waited[e]
        for k, v in deps.items():
            if e == "pe" and k == "pe":
                continue
            if wd.get(k, 0) >= v:
                continue
            self.eng[e].wait_ge(self.sems[k], v)
            wd[k] = v
            self.ninst += 1

    def _mark(self, key, val, r, w):
        for b in r:
            if b.r.get(key, 0) < val:
                b.r[key] = val
        for b in w:
            if b.multi:
                if b.w.get(key, 0) < val:
                    b.w[key] = val
            else:
                b.w = {key: val}
                b.r = {}

    def op(self, e, fn, r=(), w=()):
        if self.rec is not None:
            pe = Prog._RecEng()
            fn(pe)
            name, a_, k_ = pe.call
            self.rec.append(("op", e, name, a_, k_, tuple(r), tuple(w)))
            return None
        self._wait(e, self._deps(r, w))
        ins = fn(self.eng[e])
        self.cnt[e] += 1
        ins.then_inc(self.sems[e], 1)
        self.ninst += 1
        self._mark(e, self.cnt[e], r, w)
        return ins

    def dma(self, out, in_, r=(), w=(), sb=None, e="sp", **kw):
        if self.rec is not None:
            self.rec.append(("dma", out, in_, tuple(r), tuple(w), sb, e, kw))
            return None
        if sb.sem is None:
            key = "d%d" % self.ndsem
            self.ndsem += 1
            self.sems[key] = self.es.enter_context(self.nc.semaphore("s_" + key))
            self.cnt[key] = 0
            sb.sem = key
        key = sb.sem
        self._wait(e, self._deps(r, w))
        ins = self.eng[e].dma_start(out=out, in_=in_, **kw)
        self.cnt[key] += 16
        ins.then_inc(self.sems[key], 16)
        self.ninst += 1
        self._mark(key, self.cnt[key], r, w)
        return ins

    def gather(self, out, table, idx_ap, r=(), w=(), sb=None):
        if sb.sem is None:
            key = "d%d" % self.ndsem
            self.ndsem += 1
            self.sems[key] = self.es.enter_context(self.nc.semaphore("s_" + key))
            self.cnt[key] = 0
            sb.sem = key
        key = sb.sem
        e = "pool"
        self._wait(e, self._deps(r, w))
        ins = self.nc.gpsimd.indirect_dma_start(
            out=out, out_offset=None, in_=table,
            in_offset=bass.IndirectOffsetOnAxis(ap=idx_ap, axis=0))
        self.cnt[key] += 16
        ins.then_inc(self.sems[key], 16)
        self.ninst += 1
        self._mark(key, self.cnt[key], r, w)
        return ins

    def finish(self):
        for k, v in self.cnt.items():
            if v > 0 and k != "sp":
                self.nc.sync.wait_ge(self.sems[k], v)


R_HEADS, R_DK, R_DV = 8, 64, 128
M_HEADS, M_DK, M_DV = 4, 128, 256
IN_SPLITS = (512, 512, 1024, 1024, 512, 512, 1024, 1024, 16, 2048, 2048)
IN_OFF = [0]
for _s in IN_SPLITS:
    IN_OFF.append(IN_OFF[-1] + _s)
(O_RQ, O_RK, O_RV, O_RG, O_MQ, O_MK, O_MV, O_MO, O_MG, O_GR, O_GM, IN_COLS) = IN_OFF
P_HEADS, P_NK, P_TOPK = 8, 128, 16
NEXP = 16384
CL = 256


class Ring:
    prog = None

    def __init__(self, nc, es, name, shape, dt, n, psum=False, multi=False):
        self.items = []
        self.multi = multi
        for i in range(n):
            if psum:
                t = es.enter_context(nc.psum_tensor("r_%s%d" % (name, i), shape, dt))
            else:
                t = es.enter_context(nc.sbuf_tensor("r_%s%d" % (name, i), shape, dt))
            self.items.append((t, Buf("%s%d" % (name, i), multi)))
        self.i = 0

    def next(self):
        it = self.items[self.i % len(self.items)]
        self.i += 1
        if self.multi:
            if Ring.prog is not None and Ring.prog.rec is not None:
                Ring.prog.rec.append(("fresh", it[1]))
            else:
                it[1].fresh()
        return it


class RingView:
    def __init__(self, items):
        self.items = list(items)
        self.i = 0

    def next(self):
        it = self.items[self.i % len(self.items)]
        self.i += 1
        return it


def build_program(L, dbg=False):
    H = L // 2
    NST = H // 512
    nc = bass.Bass("TRN2", target_bir_lowering=False)

    def din(name, shape, dt=F32):
        return nc.dram_tensor(name, list(shape), dt, kind="ExternalInput").ap()

    def dscr(name, shape, dt):
        return nc.dram_tensor(name, list(shape), dt, kind=("ExternalOutput" if dbg else "Internal")).ap()

    x_d = din("x", [L, D])
    ctx_d = din("ctx", [CL, D])
    cvec_d = din("cvec", [2, D])
    wada_d = din("w_ada", [D, 6 * D])
    bada_d = din("b_ada", [1, 6 * D])
    g1_d = din("norm1_g", [1, D])
    win_d = din("w_in", [D, IN_COLS])
    wmg_d = din("w_mg", [D, 16])
    gbias_d = din("gbias", [1, 16])
    rdec_d = din("ret_decay", [1, 16])
    mconv_d = din("m_conv", [3, 1024])
    wro_d = din("w_ret_out", [1024, D])
    wmo_d = din("w_mlstm_out", [1024, D])
    wout_d = din("w_out", [D, D])
    g2_d = din("norm2_g", [1, D])
    pq_d = din("peer_query", [D, D])
    pk_d = din("peer_keys", [16 * 128, 128])
    pdn_d = din("peer_down", [NEXP, D])
    pup_d = din("peer_up", [NEXP, D])
    fg_d = din("final_g", [1, D])
    rot_d = din("rot", [L, 64])
    cst_d = din("cst", [128, 6, 128])
    pos_d = din("pos", [128, 4])
    out_d = nc.dram_tensor("out", [H, D], F32, kind="ExternalOutput").ap()

    xmT_d = dscr("xmT", [D, L + 2], BF16)
    cmT_d = dscr("cmT", [D, CL + 2], BF16)
    ada_d = dscr("ada", [2, 6 * D], F32)
    winb_d = dscr("winb", [D, IN_COLS], BF16)
    wrob_d = dscr("wrob", [1024, D], BF16)
    wmob_d = dscr("wmob", [1024, D], BF16)
    woutb_d = dscr("woutb", [D, D], BF16)
    pqb_d = dscr("pqb", [D, D], BF16)
    pcomb_d = dscr("pcomb", [NEXP, 2 * D], BF16)
    yA_d = dscr("yA", [H, D], F32)
    yoT_d = dscr("yoT", [D, H], BF16)
    x1_d = dscr("x1", [H, D], F32)
    B_xmT, B_cmT, B_ada = Buf("xmT_d", True), Buf("cmT_d", True), Buf("ada_d")
    B_winb, B_wrob, B_wmob, B_woutb, B_pqb = (Buf(n, True) for n in ("winb", "wrob", "wmob", "woutb", "pqb"))
    B_pdnb, B_pupb = Buf("pdnb", True), Buf("pupb", True)
    B_yA, B_yoT, B_x1, B_out = Buf("yA", True), Buf("yoT", True), Buf("x1", True), Buf("out", True)

    with ExitStack() as es:
        P = Prog(nc, es)
        Ring.prog = P

        def barrier():
            for e in ("pe", "dve", "act", "pool", "sp"):
                P._wait(e, {k: v for k, v in P.cnt.items() if v > 0 and k != e})

        def sbt(st, name, shape, dt):
            return st.enter_context(nc.sbuf_tensor("t_" + name, shape, dt)), Buf(name)

        cst, b_cst = sbt(es, "cst", [128, 6, 128], F32)
        pos, b_pos = sbt(es, "pos", [128, 4], F32)
        identb, b_identb = sbt(es, "identb", [128, 128], BF16)
        P.dma(cst[:], cst_d, w=[b_cst], sb=b_cst)
        P.dma(pos[:], pos_d, w=[b_pos], sb=b_pos)
        P.op("dve", lambda e: e.tensor_copy(identb[:], cst[:, 0, :]), r=[b_cst], w=[b_identb])
        identf = cst[:, 0, :]
        tri = [cst[:, 1, :], cst[:, 2, :]]
        negm = [cst[:, 3, :], cst[:, 4, :]]
        onesf = cst[:, 5, :]
        modF, b_modF = sbt(es, "modF", [128, 6, KC], F32)
        psr = Ring(nc, es, "ps", [128, 512], F32, 8, psum=True)

        with ExitStack() as ph:
            cT, b_cT = sbt(ph, "cT", [128, 2, KC], F32)
            cTs, b_cTs = sbt(ph, "cTs", [128, 2, KC], F32)
            for r_ in range(2):
                P.dma(cT[:, r_, :], cvec_d[r_:r_ + 1, :].rearrange("r (k p) -> p (r k)", p=128), w=[b_cT], sb=b_cT,
                      allow_slow_non_contiguous=True)
            P.op("act", lambda e: e.activation(cTs[:], cT[:], AF.Silu), r=[b_cT], w=[b_cTs])
            adasb, b_adasb = sbt(ph, "adasb", [2, 6 * D], F32)
            badasb, b_badasb = sbt(ph, "badasb", [2, 6 * D], F32)
            P.dma(badasb[:], bada_d.partition_broadcast(2), w=[b_badasb], sb=b_badasb)
            wring = Ring(nc, ph, "wada", [128, KC, 512], F32, 2)
            for nb in range(24):
                wt, bw = wring.next()
                P.dma(wt[:], wada_d[:, nb * 512:(nb + 1) * 512].rearrange("(k p) n -> p k n", p=128),
                      w=[bw], sb=bw)
                pt, bp = psr.next()
                for k in range(KC):
                    P.op("pe", lambda e: e.matmul(pt[0:2, :], lhsT=cTs[:, :, k], rhs=wt[:, k, :],
                                                  start=(k == 0), stop=(k == KC - 1)),
                         r=[b_cTs, bw], w=[bp])
                P.op("dve", lambda e: e.tensor_tensor(adasb[:, nb * 512:(nb + 1) * 512], pt[0:2, :],
                                                      badasb[:, nb * 512:(nb + 1) * 512], ALU.add),
                     r=[bp, b_badasb], w=[b_adasb])
            P.dma(ada_d, adasb[:], r=[b_adasb], w=[B_ada], sb=b_adasb)
            raw, b_raw = sbt(ph, "rawmod", [128, 8, KC], F32)
            srcs = [ada_d[0:1, 0:D], ada_d[0:1, D:2 * D], ada_d[0:1, 3 * D:4 * D], ada_d[0:1, 4 * D:5 * D],
                    ada_d[1:2, 0:D], ada_d[1:2, D:2 * D], g1_d, g2_d]
            for i, s in enumerate(srcs):
                P.dma(raw[:, i, :], s.rearrange("r (k p) -> p (r k)", p=128), r=[B_ada], w=[b_raw], sb=b_raw,
                      allow_slow_non_contiguous=True)
            for (dst, sc, g, sh) in ((0, 1, 6, 0), (2, 5, 6, 4), (4, 3, 7, 2)):
                P.op("dve", lambda e: e.scalar_tensor_tensor(out=modF[:, dst, :], in0=raw[:, sc, :], scalar=1.0,
                                                             in1=raw[:, g, :], op0=ALU.add, op1=ALU.mult),
                     r=[b_raw], w=[b_modF])
                P.op("dve", lambda e: e.tensor_copy(modF[:, dst + 1, :], raw[:, sh, :]), r=[b_raw], w=[b_modF])
        barrier()

        with ExitStack() as ph:
            lst_b, lst_c = [], []
            P.rec = lst_b
            FMAX = 4096
            cin = Ring(nc, ph, "cvin", [128, FMAX], F32, 5)
            cout = Ring(nc, ph, "cvout", [128, FMAX], BF16, 5)
            cnt = [0]

            def convert(pairs, bdst):
                for (sa, da) in pairs:
                    fsz = sa.shape[1]
                    ti, bi = cin.next()
                    to, bo = cout.next()
                    P.dma(ti[:, 0:fsz], sa, w=[bi], sb=bi)
                    eng = ("dve", "act")[cnt[0] % 2]
                    cnt[0] += 1
                    if eng == "act":
                        P.op("act", lambda e: e.copy(to[:, 0:fsz], ti[:, 0:fsz]), r=[bi], w=[bo])
                    else:
                        P.op(eng, lambda e: e.tensor_copy(to[:, 0:fsz], ti[:, 0:fsz]), r=[bi], w=[bo])
                    if len(da.shape) == 3:
                        P.dma(da, to[:, 0:fsz].rearrange("p (r c) -> p r c", r=da.shape[1]), r=[bo], w=[bdst], sb=bo)
                    else:
                        P.dma(da, to[:, 0:fsz], r=[bo], w=[bdst], sb=bo)

            convert([(win_d[rb * 128:(rb + 1) * 128, cb * 2564:(cb + 1) * 2564], winb_d[rb * 128:(rb + 1) * 128, cb * 2564:(cb + 1) * 2564])
                     for rb in range(16) for cb in range(4)], B_winb)
            for s_, d_, b_ in ((wro_d, wrob_d, B_wrob), (wmo_d, wmob_d, B_wmob), (wout_d, woutb_d, B_woutb), (pq_d, pqb_d, B_pqb)):
                sv_ = s_.rearrange("(rb p r) c -> rb p (r c)", p=128, r=2)
                dv_ = d_.rearrange("(rb p r) c -> rb p (r c)", p=128, r=2)
                convert([(sv_[i], dv_[i]) for i in range(sv_.shape[0])], b_)
            for s_, c0_, b_ in ((pdn_d, 0, B_pdnb), (pup_d, D, B_pupb)):
                sv_ = s_.rearrange("(rb p r) c -> rb p (r c)", p=128, r=2)
                dv_ = pcomb_d.rearrange("(rb p r) c -> rb p r c", p=128, r=2)
                convert([(sv_[i], dv_[i][:, :, c0_:c0_ + D]) for i in range(sv_.shape[0])], b_)
            P.rec = lst_c
            xin = Ring(nc, ph, "xin", [128, D], F32, 3)
            xnr = Ring(nc, ph, "xn", [128, D], BF16, 2)
            jk, b_jk = sbt(ph, "jk", [128, D], BF16)
            stat = Ring(nc, ph, "stat", [128, 4], F32, 3)
            xts = Ring(nc, ph, "xts", [128, KC, 512], BF16, 2, multi=True)
            zt, b_zt = sbt(ph, "zt", [128, KC, 1], BF16)
            P.op("pool", lambda e: e.memset(zt[:], 0.0), w=[b_zt])
            for dst, bd, n in ((xmT_d, B_xmT, L), (cmT_d, B_cmT, CL)):
                v = dst.rearrange("(k p) n -> p k n", p=128)
                P.dma(v[:, :, 0:1], zt[:], r=[b_zt], w=[bd], sb=b_zt, allow_slow_non_contiguous=True)
                P.dma(v[:, :, n + 1:n + 2], zt[:], r=[b_zt], w=[bd], sb=b_zt, allow_slow_non_contiguous=True)
            units = [(ctx_d, cmT_d, B_cmT, 0, CL, 2)] + [(x_d, xmT_d, B_xmT, s * 512, 512, 0) for s in range(L // 512)]
            ecnt = 0
            for (src, dst, bd, base, NT, mi) in units:
                XT, bXT = xts.next()
                for j in range(NT // 128):
                    xt, bx = xin.next()
                    P.dma(xt[:], src[base + j * 128: base + (j + 1) * 128, :], w=[bx], sb=bx)
                    st, bs = stat.next()
                    P.op("dve", lambda e: e.scalar_tensor_tensor(out=jk[:], in0=xt[:], scalar=1.0, in1=xt[:],
                                                                 op0=ALU.mult, op1=ALU.mult, accum_out=st[:, 0:1]),
                         r=[bx], w=[b_jk, bs])
                    P.op("dve", lambda e: e.tensor_scalar(st[:, 1:2], st[:, 0:1], 1.0 / D, EPS, op0=ALU.mult, op1=ALU.add),
                         r=[bs], w=[bs])
                    P.op("act", lambda e: e.activation(st[:, 2:3], st[:, 1:2], AF.Sqrt), r=[bs], w=[bs])
                    P.op("dve", lambda e: e.reciprocal(st[:, 3:4], st[:, 2:3]), r=[bs], w=[bs])
                    xn, bxn = xnr.next()
                    P.op("act", lambda e: e.activation(xn[:], xt[:], AF.Identity, scale=st[:, 3:4]), r=[bx, bs], w=[bxn])
                    for half in range(2):
                        pt, bp = psr.next()
                        ptb = pt[:].bitcast(BF16)
                        for kk in range(8):
                            k = half * 8 + kk
                            P.op("pe", lambda e: e.transpose(ptb[:, kk * 128:(kk + 1) * 128], xn[:, k * 128:(k + 1) * 128], identb[:]),
                                 r=[bxn, b_identb], w=[bp])
                        for kk in range(8):
                            k = half * 8 + kk
                            o = XT[:, k, j * 128:(j + 1) * 128]
                            i_ = ptb[:, kk * 128:(kk + 1) * 128]
                            if half == 0:
                                P.op("act", lambda e: e.activation(o, i_, AF.Identity, bias=modF[:, mi + 1, k:k + 1], scale=modF[:, mi, k:k + 1]),
                                     r=[b_modF], w=[bXT, bp])
                            else:
                                P.op("dve", lambda e: e.tensor_scalar(o, i_, modF[:, mi, k:k + 1], modF[:, mi + 1, k:k + 1], op0=ALU.mult, op1=ALU.add),
                                     r=[b_modF], w=[bXT, bp])
                P.dma(dst.rearrange("(k p) n -> p k n", p=128)[:, :, 1 + base:1 + base + NT], XT[:, :, 0:NT], r=[bXT], w=[bd], sb=bXT)
            P.rec = None
            P.zip_emit([lst_b, lst_c])
        barrier()

        with ExitStack() as ph:
            mhalf, b_mhalf = sbt(ph, "mhalf", [128, 16], F32)
            P.op("pool", lambda e: e.memset(mhalf[:], -0.5), w=[b_mhalf])
            rd, b_rd = sbt(ph, "rd", [128, 16], F32)
            lgn, b_lgn = sbt(ph, "lgn", [128, 16], F32)
            tmp16, b_tmp16 = sbt(ph, "tmp16", [128, 16], F32)
            dec, b_dec = sbt(ph, "dec", [128, 2, 3, 8], F32)
            P.dma(rd[:], rdec_d.partition_broadcast(128), w=[b_rd], sb=b_rd)
            P.op("act", lambda e: e.activation(rd[:], rd[:], AF.Exp, scale=-1.0), r=[b_rd], w=[b_rd])
            P.op("dve", lambda e: e.tensor_scalar(lgn[:], rd[:], -1.0 / 8, 1.0 / 7, op0=ALU.mult, op1=ALU.add), r=[b_rd], w=[b_lgn])
            for cf in (6, 5, 4, 3, 2, 1):
                P.op("dve", lambda e: e.tensor_tensor(tmp16[:], lgn[:], rd[:], ALU.mult), r=[b_lgn, b_rd], w=[b_tmp16])
                P.op("dve", lambda e: e.tensor_scalar(lgn[:], tmp16[:], -1.0, 1.0 / cf, op0=ALU.mult, op1=ALU.add), r=[b_tmp16], w=[b_lgn])
            P.op("dve", lambda e: e.tensor_tensor(lgn[:], lgn[:], rd[:], ALU.mult), r=[b_lgn, b_rd], w=[b_lgn])
            for dr in range(2):
                P.op("dve", lambda e: e.tensor_scalar(tmp16[:, 0:8], lgn[:, dr * 8:(dr + 1) * 8], pos[:, dr:dr + 1], None, op0=ALU.mult),
                     r=[b_lgn, b_pos], w=[b_tmp16])
                P.op("act", lambda e: e.activation(dec[:, dr, 0, :], tmp16[:, 0:8], AF.Exp, scale=-1.0), r=[b_tmp16], w=[b_dec])
                P.op("act", lambda e: e.activation(dec[:, dr, 1, :], tmp16[:, 0:8], AF.Exp, bias=-math.log(8.0)), r=[b_tmp16], w=[b_dec])
                P.op("act", lambda e: e.activation(dec[:, dr, 2, :], lgn[:, dr * 8:(dr + 1) * 8], AF.Exp, scale=-128.0), r=[b_lgn], w=[b_dec])
            cw, b_cw = sbt(ph, "cw", [128, 3, 8], F32)
            for j_ in range(3):
                P.dma(cw[:, j_, :], mconv_d[j_:j_ + 1, :].rearrange("r (b p) -> p (r b)", p=128), w=[b_cw], sb=b_cw, allow_slow_non_contiguous=True)
            gb, b_gb = sbt(ph, "gb", [128, 16], F32)
            P.dma(gb[:], gbias_d.partition_broadcast(128), w=[b_gb], sb=b_gb)
            wmgf, b_wmgf = sbt(ph, "wmgf", [128, KC, 16], F32)
            wmg, b_wmg = sbt(ph, "wmg", [128, KC, 16], BF16)
            P.dma(wmgf[:], wmg_d.rearrange("(k p) n -> p k n", p=128), w=[b_wmgf], sb=b_wmgf)
            P.op("dve", lambda e: e.tensor_copy(wmg[:], wmgf[:]), r=[b_wmgf], w=[b_wmg])
            S32, b_S32 = sbt(ph, "S32", [64, 8, 128], F32)
            Sbf, b_Sbf = sbt(ph, "Sbf", [64, 8, 128], BF16)
            C32, b_C32 = sbt(ph, "C32", [128, 4, 257], F32)
            Cbf, b_Cbf = sbt(ph, "Cbf", [128, 4, 257], BF16)
            mprev, b_mprev = sbt(ph, "mprev", [128, 4], F32)
            XTr = Ring(nc, ph, "XT", [128, KC, 514], BF16, 1)
            Wbr = Ring(nc, ph, "Wb", [128, KC, 512], BF16, 2)
            RKr = Ring(nc, ph, "RK", [128, 4, 512], BF16, 1, multi=True)
            RQr = Ring(nc, ph, "RQ", [128, 4, 512], BF16, 1, multi=True)
            RVr = Ring(nc, ph, "RVs", [128, 4, 8, 128], BF16, 1, multi=True)
            MQKr = Ring(nc, ph, "MQK", [128, 8, 512], BF16, 1, multi=True)
            MVr = Ring(nc, ph, "MV", [128, 4, 1024], BF16, 1, multi=True)
            RGr = Ring(nc, ph, "RG", [128, 4, 1024], BF16, 1, multi=True)
            MOr = Ring(nc, ph, "MO", [128, 4, 1024], BF16, 1, multi=True)
            Gr = Ring(nc, ph, "G", [128, 4, 16], F32, 1, multi=True)
            rotr = Ring(nc, ph, "rot", [128, 4, 64], F32, 2)
            rawr = Ring(nc, ph, "raw", [128, 514], F32, 2)
            accr = Ring(nc, ph, "cacc", [128, 512], F32, 2)
            rtr = Ring(nc, ph, "rtmp", [128, 2, 8, 2, 32], F32, 1)
            QTr = Ring(nc, ph, "QT", [64, 8, 128], BF16, 2)
            KTr = Ring(nc, ph, "KT", [64, 8, 128], BF16, 2)
            ATr = Ring(nc, ph, "AT", [128, 8, 128], BF16, 1)
            ATmr = Ring(nc, ph, "ATm", [128, 4, 128], BF16, 2)
            MKtr = Ring(nc, ph, "MKt", [128, 4, 128], BF16, 2)
            Vmr = Ring(nc, ph, "Vm", [128, 4, 257], BF16, 2)
            gsr = Ring(nc, ph, "gs", [128, 16, 4], F32, 2)
            spur = Ring(nc, ph, "spu", [128, 2, 4, 4], F32, 2)
            Dr = Ring(nc, ph, "Dg", [128, 4, 128], F32, 1)
            Umr = Ring(nc, ph, "Um", [128, 4, 128], F32, 1)
            Yr = Ring(nc, ph, "Y", [128, D], F32, 2)
            Ym_bufs = [Buf("Ym0"), Buf("Ym1")]
            YAr = Ring(nc, ph, "YA", [128, D], F32, 1)
            sqr = Ring(nc, ph, "sq", [128, D], F32, 1)
            yor = Ring(nc, ph, "yo", [128, D], BF16, 1)
            nsr = Ring(nc, ph, "ns", [128, 4, 12], F32, 2)
            YOTr = Ring(nc, ph, "YOT", [128, KC, 512], BF16, 1, multi=True)
            dnr = Ring(nc, ph, "dn", [128, 4, 4], F32, 2)

            def proj_tok(XT, bXT, j, Wb, bW, width=512):
                pt, bp = psr.next()
                for k in range(KC):
                    P.op("pe", lambda e: e.matmul(pt[:, 0:width], lhsT=XT[:, k, 1 + j * 128:1 + (j + 1) * 128], rhs=Wb[:, k, 0:width],
                                                  start=(k == 0), stop=(k == KC - 1)), r=[bXT, bW], w=[bp])
                return pt, bp

            def load_w(off):
                Wb, bW = Wbr.next()
                P.dma(Wb[:], winb_d[:, off:off + 512].rearrange("(k p) n -> p k n", p=128), r=[B_winb], w=[bW], sb=bW)
                return Wb, bW

            def scan_pass(dr):
                maskT = tri[dr]
                qdec = dec[:, dr, 0, :]
                kdec = dec[:, dr, 1, :]
                cdec = dec[:, dr, 2, :]
                P.op("pool", lambda e: e.memset(S32[:], 0.0), w=[b_S32])
                P.op("pool", lambda e: e.memset(Sbf[:], 0.0), w=[b_Sbf])
                P.op("pool", lambda e: e.memset(C32[:], 0.0), w=[b_C32])
                P.op("pool", lambda e: e.memset(Cbf[:], 0.0), w=[b_Cbf])
                P.op("pool", lambda e: e.memset(mprev[:], 0.0), w=[b_mprev])
                if dr == 0:
                    units = [(True, 0, CL, False)] + [(False, s * 512, 512, True) for s in range(NST)]
                else:
                    units = [(True, 0, CL, False)] + [(False, s * 512, 512, False) for s in range(2 * NST - 1, NST - 1, -1)] \
                        + [(False, s * 512, 512, True) for s in range(NST - 1, -1, -1)]
                for (is_ctx, base, NT, with_out) in units:
                    nch = NT // 128
                    srcT, bsrc = (cmT_d, B_cmT) if is_ctx else (xmT_d, B_xmT)
                    XT, bXT = XTr.next()
                    P.dma(XT[:, :, 0:NT + 2], srcT.rearrange("(k p) n -> p k n", p=128)[:, :, base:base + NT + 2], r=[bsrc], w=[bXT], sb=bXT)
                    if not is_ctx:
                        rot, brot = rotr.next()
                        P.dma(rot[:, 0:nch, :], rot_d[base:base + NT, :].rearrange("(j p) c -> p j c", p=128), w=[brot], sb=brot)
                    RK, bRK = RKr.next()
                    RQ, bRQ = RQr.next()
                    RVs, bRV = RVr.next()
                    MQK, bMQK = MQKr.next()
                    MV, bMV = MVr.next()
                    G, bG = Gr.next()
                    RG, bRG = RGr.next()
                    MO, bMO = MOr.next()

                    def rotary(pt, bp, dst, bdst, j):
                        if is_ctx:
                            P.op("act", lambda e: e.copy(dst[:, j, :], pt[:, 0:512]), w=[bdst, bp])
                            return
                        tm, btm = rtr.next()
                        pv = pt[:, 0:512].rearrange("p (h t i) -> p h t i", h=8, t=2)
                        cosb = rot[:, j, 0:32].unsqueeze(1).unsqueeze(1).to_broadcast([128, 8, 2, 32])
                        sinb = rot[:, j, 32:64].unsqueeze(1).unsqueeze(1).to_broadcast([128, 8, 2, 32])
                        P.op("dve", lambda e: e.tensor_tensor(tm[:, 0], pv, cosb, ALU.mult), r=[brot], w=[btm, bp])
                        P.op("dve", lambda e: e.tensor_tensor(tm[:, 1], pv, sinb, ALU.mult), r=[brot], w=[btm, bp])
                        dv = dst[:, j, :].rearrange("p (h t i) -> p h t i", h=8, t=2)
                        P.op("pool", lambda e: e.tensor_tensor(dv[:, :, 0, :], tm[:, 0, :, 0, :], tm[:, 1, :, 1, :], ALU.subtract), r=[btm], w=[bdst])
                        P.op("pool", lambda e: e.tensor_tensor(dv[:, :, 1, :], tm[:, 0, :, 1, :], tm[:, 1, :, 0, :], ALU.add), r=[btm], w=[bdst])

                    Wb, bW = load_w(O_RK)
                    for j in range(nch):
                        pt, bp = proj_tok(XT, bXT, j, Wb, bW)
                        rotary(pt, bp, RK, bRK, j)
                    for hb in range(2):
                        Wb, bW = load_w(O_RV + hb * 512)
                        for j in range(nch):
                            pt, bp = proj_tok(XT, bXT, j, Wb, bW)
                            P.op("dve", lambda e: e.tensor_tensor(RVs[:, j, hb * 4:(hb + 1) * 4, :], pt[:, 0:512].rearrange("p (h e) -> p h e", h=4),
                                                                  kdec[:, hb * 4:(hb + 1) * 4].unsqueeze(2).to_broadcast([128, 4, 128]), ALU.mult),
                                 r=[b_dec], w=[bRV, bp])
                    for hb in range(2):
                        Wb, bW = load_w(O_MV + hb * 512)
                        for j in range(nch):
                            pt, bp = proj_tok(XT, bXT, j, Wb, bW)
                            P.op("act", lambda e: e.copy(MV[:, j, hb * 512:(hb + 1) * 512], pt[:, 0:512]), w=[bMV, bp])
                    for j in range(nch):
                        pt, bp = proj_tok(XT, bXT, j, wmg, b_wmg, width=16)
                        P.op("dve", lambda e: e.tensor_tensor(G[:, j, :], pt[:, 0:16], gb[:], ALU.add), r=[b_gb], w=[bG, bp])
                    spu, bspu = spur.next()
                    P.op("act", lambda e: e.activation(spu[:, 1, 0:nch, :], G[:, 0:nch, dr * 8 + 4:dr * 8 + 8], AF.Exp, scale=-1.0), r=[bG], w=[bspu])
                    P.op("act", lambda e: e.activation(spu[:, 0, 0:nch, :], spu[:, 1, 0:nch, :], AF.Ln, bias=1.0), w=[bspu])
                    fm_blocks = [(O_MK, 4)] + ([(O_MQ, 0)] if with_out else [])
                    nh = (NT + 2) // 2
                    for (off, b0) in fm_blocks:
                        Wb, bW = load_w(off)
                        for hb in range(4):
                            raw, braw = rawr.next()
                            for half in range(2):
                                pt, bp = psr.next()
                                for k in range(KC):
                                    P.op("pe", lambda e: e.matmul(pt[:, 0:nh], lhsT=Wb[:, k, hb * 128:(hb + 1) * 128], rhs=XT[:, k, half * nh:(half + 1) * nh],
                                                                  start=(k == 0), stop=(k == KC - 1)), r=[bXT, bW], w=[bp])
                                if half == 0:
                                    P.op("act", lambda e: e.copy(raw[:, 0:nh], pt[:, 0:nh]), w=[braw, bp])
                                else:
                                    P.op("dve", lambda e: e.tensor_copy(raw[:, nh:2 * nh], pt[:, 0:nh]), w=[braw, bp])
                            b = b0 + hb
                            ac, bac = accr.next()
                            P.op("act", lambda e: e.activation(ac[:, 0:NT], raw[:, 1:NT + 1], AF.Identity, scale=cw[:, 1, b:b + 1]), r=[braw, b_cw], w=[bac])
                            P.op("dve", lambda e: e.scalar_tensor_tensor(out=ac[:, 0:NT], in0=raw[:, 0:NT], scalar=cw[:, 0, b:b + 1], in1=ac[:, 0:NT],
                                                                         op0=ALU.mult, op1=ALU.add), r=[braw, b_cw], w=[bac])
                            P.op("dve", lambda e: e.scalar_tensor_tensor(out=ac[:, 0:NT], in0=raw[:, 2:NT + 2], scalar=cw[:, 2, b:b + 1], in1=ac[:, 0:NT],
                                                                         op0=ALU.mult, op1=ALU.add), r=[braw, b_cw], w=[bac])
                            P.op("act", lambda e: e.activation(MQK[:, b, 0:NT], ac[:, 0:NT], AF.Silu), r=[bac], w=[bMQK])
                    if with_out:
                        Wb, bW = load_w(O_RQ)
                        for j in range(nch):
                            pt, bp = proj_tok(XT, bXT, j, Wb, bW)
                            rotary(pt, bp, RQ, bRQ, j)
                        if dr == 1:
                            for (off, dstt, bdd, fn) in ((O_RG, RG, bRG, AF.Silu), (O_MO, MO, bMO, AF.Sigmoid)):
                                for hb in range(2):
                                    Wb, bW = load_w(off + hb * 512)
                                    for j in range(nch):
                                        pt, bp = proj_tok(XT, bXT, j, Wb, bW)
                                        P.op("act", lambda e: e.activation(dstt[:, j, hb * 512:(hb + 1) * 512], pt[:, 0:512], fn), w=[bdd, bp])
                        YOT, bYOT = YOTr.next()
                    order = range(nch) if dr == 0 else range(nch - 1, -1, -1)
                    pend_out = []
                    psM, psR, psO = RingView(psr.items[0:4]), RingView(psr.items[4:7]), RingView(psr.items[7:8])
                    psx = [psM]
                    for j in order:
                        tok0 = base + j * 128
                        if with_out:
                            Y, bY = Yr.next()
                            bYm = Ym_bufs[(Yr.i - 1) % len(Yr.items)]
                        lst_R, lst_M, lst_O = [], [], []
                        P.rec = lst_R
                        psx[0] = psR
                        if with_out:
                            QT, bQT = QTr.next()
                            KT, bKT = KTr.next()
                            for (srcq, bsq, dT, bdT, eng) in ((RQ, bRQ, QT, bQT, "act"), (RK, bRK, KT, bKT, "dve")):
                                pt, bp = psx[0].next()
                                ptb = pt[:].bitcast(BF16)
                                for h in range(8):
                                    P.op("pe", lambda e: e.transpose(ptb[0:64, h * 128:(h + 1) * 128], srcq[:, j, h * 64:(h + 1) * 64], identb[:]),
                                         r=[bsq, b_identb], w=[bp])
                                if eng == "act":
                                    P.op("act", lambda e: e.copy(dT[:].rearrange("p h c -> p (h c)"), ptb[0:64, :]), w=[bdT, bp])
                                else:
                                    P.op("dve", lambda e: e.tensor_copy(dT[:].rearrange("p h c -> p (h c)"), ptb[0:64, :]), w=[bdT, bp])
                            AT, bAT = ATr.next()
                            for hb in range(2):
                                pa, bpa = psx[0].next()
                                for hh in range(4):
                                    h = hb * 4 + hh
                                    P.op("pe", lambda e: e.matmul(pa[:, hh * 128:(hh + 1) * 128], lhsT=KT[:, h, :], rhs=QT[:, h, :], start=True, stop=True),
                                         r=[bKT, bQT], w=[bpa])
                                P.op("dve", lambda e: e.tensor_tensor(AT[:, hb * 4:(hb + 1) * 4, :], pa[:, 0:512].rearrange("p (h c) -> p h c", h=4),
                                                                      maskT.unsqueeze(1).to_broadcast([128, 4, 128]), ALU.mult), r=[b_cst], w=[bAT, bpa])
                            for hb in range(2):
                                py, bpy = psx[0].next()
                                for hh in range(4):
                                    h = hb * 4 + hh
                                    P.op("pe", lambda e: e.matmul(py[:, hh * 128:(hh + 1) * 128], lhsT=AT[:, h, :], rhs=RVs[:, j, h, :], start=True, stop=False),
                                         r=[bAT, bRV], w=[bpy])
                                    P.op("pe", lambda e: e.matmul(py[:, hh * 128:(hh + 1) * 128], lhsT=QT[:, h, :], rhs=Sbf[:, h, :], start=False, stop=True),
                                         r=[bQT, b_Sbf], w=[bpy])
                                P.op("dve", lambda e: e.tensor_tensor(Y[:, hb * 512:(hb + 1) * 512].rearrange("p (h e) -> p h e", h=4),
                                                                      py[:, 0:512].rearrange("p (h e) -> p h e", h=4),
                                                                      qdec[:, hb * 4:(hb + 1) * 4].unsqueeze(2).to_broadcast([128, 4, 128]), ALU.mult),
                                     r=[b_dec], w=[bY, bpy])
                        for hb in range(2):
                            pS, bpS = psx[0].next()
                            for hh in range(4):
                                h = hb * 4 + hh
                                P.op("pe", lambda e: e.matmul(pS[0:64, hh * 128:(hh + 1) * 128], lhsT=RK[:, j, h * 64:(h + 1) * 64], rhs=RVs[:, j, h, :],
                                                              start=True, stop=True), r=[bRK, bRV], w=[bpS])
                            sv = S32[:, hb * 4:(hb + 1) * 4, :]
                            P.op("dve", lambda e: e.tensor_tensor(sv, sv, pS[0:64, 0:512].rearrange("p (h e) -> p h e", h=4), ALU.add), w=[b_S32, bpS])
                            P.op("pool", lambda e: e.tensor_tensor(sv, sv, cdec[0:64, hb * 4:(hb + 1) * 4].unsqueeze(2).to_broadcast([64, 4, 128]), ALU.mult),
                                 r=[b_dec], w=[b_S32])
                            P.op("act", lambda e: e.copy(Sbf[:, hb * 4:(hb + 1) * 4, :], sv), r=[b_S32], w=[b_Sbf])
                        P.rec = lst_M
                        psx[0] = psM
                        gs, bgs = gsr.next()
                        zi = G[:, j, dr * 8:dr * 8 + 4]
                        zf = G[:, j, dr * 8 + 4:dr * 8 + 8]
                        pg, bpg = psx[0].next()
                        P.op("pe", lambda e: e.matmul(pg[:, 0:4], lhsT=tri[dr], rhs=spu[:, 0, j, :], start=True, stop=True), r=[bspu, b_cst], w=[bpg])
                        P.op("pe", lambda e: e.matmul(pg[:, 4:8], lhsT=onesf, rhs=spu[:, 0, j, :], start=True, stop=True), r=[bspu, b_cst], w=[bpg])
                        cs = pg[:, 0:4]
                        tot = pg[:, 4:8]
                        P.op("dve", lambda e: e.tensor_tensor(gs[:, 2, :], zi, cs, ALU.add), r=[bG], w=[bgs, bpg])
                        Dg, bDg = Dr.next()
                        P.op("dve", lambda e: e.tensor_tensor(Dg[:], identf.unsqueeze(1).to_broadcast([128, 4, 128]),
                                                              gs[:, 2, :].unsqueeze(2).to_broadcast([128, 4, 128]), ALU.mult), r=[b_cst, bgs], w=[bDg])
                        pu, bpu = psx[0].next()
                        P.op("pe", lambda e: e.matmul(pu[:, 0:512], lhsT=onesf, rhs=Dg[:].rearrange("p h s -> p (h s)"), start=True, stop=True),
                             r=[bDg, b_cst], w=[bpu])
                        puv = pu[:, 0:512].rearrange("p (h s) -> p h s", h=4)
                        P.op("dve", lambda e: e.tensor_reduce(out=gs[:, 5, :], in_=puv, axis=AX.X, op=ALU.max), w=[bgs, bpu])
                        Um, bUm = Umr.next()
                        P.op("dve", lambda e: e.tensor_tensor(Um[:], puv, negm[dr].unsqueeze(1).to_broadcast([128, 4, 128]), ALU.add), r=[b_cst], w=[bUm, bpu])
                        P.op("dve", lambda e: e.tensor_reduce(out=gs[:, 6, :], in_=Um[:], axis=AX.X, op=ALU.max), r=[bUm], w=[bgs])
                        P.op("dve", lambda e: e.tensor_tensor(gs[:, 3, :], gs[:, 6, :], mprev[:], ALU.max), r=[b_mprev], w=[bgs])
                        P.op("dve", lambda e: e.tensor_tensor(gs[:, 4, :], gs[:, 5, :], mprev[:], ALU.max), r=[b_mprev], w=[bgs])
                        P.op("dve", lambda e: e.tensor_tensor(gs[:, 11, :], gs[:, 2, :], gs[:, 4, :], ALU.subtract), w=[bgs])
                        P.op("act", lambda e: e.activation(gs[:, 7, :], gs[:, 11, :], AF.Exp, bias=-0.5 * math.log(128.0)), w=[bgs])
                        P.op("dve", lambda e: e.tensor_tensor(gs[:, 12, :], gs[:, 4, :], gs[:, 3, :], ALU.subtract), w=[bgs])
                        P.op("act", lambda e: e.activation(gs[:, 8, :], gs[:, 12, :], AF.Exp), w=[bgs])
                        P.op("dve", lambda e: e.tensor_tensor(gs[:, 13, :], mprev[:], gs[:, 4, :], ALU.subtract), r=[b_mprev], w=[bgs])
                        P.op("act", lambda e: e.activation(gs[:, 9, :], gs[:, 13, :], AF.Exp), w=[bgs])
                        P.op("dve", lambda e: e.tensor_tensor(gs[:, 14, :], cs, gs[:, 3, :], ALU.subtract), w=[bgs, bpg])
                        P.op("act", lambda e: e.activation(gs[:, 10, :], gs[:, 14, :], AF.Exp), w=[bgs])
                        P.op("dve", lambda e: e.tensor_tensor(mprev[:], gs[:, 4, :], tot, ALU.subtract), r=[bgs], w=[b_mprev, bpg])
                        Vm, bVm = Vmr.next()
                        P.op("dve", lambda e: e.tensor_tensor(Vm[:, :, 0:256], MV[:, j, :].rearrange("p (h e) -> p h e", h=4),
                                                              gs[:, 7, :].unsqueeze(2).to_broadcast([128, 4, 256]), ALU.mult), r=[bMV, bgs], w=[bVm])
                        P.op("pool", lambda e: e.tensor_copy(Vm[:, :, 256:257], gs[:, 7, :].unsqueeze(2)), r=[bgs], w=[bVm])
                        P.op("pool", lambda e: e.tensor_tensor(C32[:], C32[:], gs[:, 9, :].unsqueeze(2).to_broadcast([128, 4, 257]), ALU.mult), r=[bgs], w=[b_C32])
                        P.op("act", lambda e: e.copy(Cbf[:], C32[:]), r=[b_C32], w=[b_Cbf])
                        MKt, bMKt = MKtr.next()
                        pt, bp = psx[0].next()
                        ptb = pt[:].bitcast(BF16)
                        for h in range(4):
                            P.op("pe", lambda e: e.transpose(ptb[:, h * 128:(h + 1) * 128], MQK[:, 4 + h, j * 128:(j + 1) * 128], identb[:]),
                                 r=[bMQK, b_identb], w=[bp])
                        P.op("act", lambda e: e.copy(MKt[:].rearrange("p h d -> p (h d)"), ptb[:, 0:512]), w=[bMKt, bp])
                        if with_out:
                            ATm, bATm = ATmr.next()
                            pa, bpa = psx[0].next()
                            for h in range(4):
                                P.op("pe", lambda e: e.matmul(pa[:, h * 128:(h + 1) * 128], lhsT=MQK[:, 4 + h, j * 128:(j + 1) * 128],
                                                              rhs=MQK[:, h, j * 128:(j + 1) * 128], start=True, stop=True), r=[bMQK], w=[bpa])
                            P.op("dve", lambda e: e.tensor_tensor(ATm[:], pa[:, 0:512].rearrange("p (h c) -> p h c", h=4),
                                                                  maskT.unsqueeze(1).to_broadcast([128, 4, 128]), ALU.mult), r=[b_cst], w=[bATm, bpa])
                            dn, bdn = dnr.next()
                            for h in range(4):
                                ph_, bph = psx[0].next()
                                P.op("pe", lambda e: e.matmul(ph_[:, 0:257], lhsT=ATm[:, h, :], rhs=Vm[:, h, :], start=True, stop=False), r=[bATm, bVm], w=[bph])
                                P.op("pe", lambda e: e.matmul(ph_[:, 0:257], lhsT=MQK[:, h, j * 128:(j + 1) * 128], rhs=Cbf[:, h, :], start=False, stop=True),
                                     r=[bMQK, b_Cbf], w=[bph])
                                P.op("act", lambda e: e.activation(dn[:, h, 0:1], ph_[:, 256:257], AF.Abs), w=[bdn, bph])
                                P.op("dve", lambda e: e.tensor_scalar(dn[:, h, 1:2], dn[:, h, 0:1], gs[:, 8, h:h + 1], gs[:, 10, h:h + 1], op0=ALU.mult, op1=ALU.max),
                                     r=[bgs], w=[bdn])
                                P.op("dve", lambda e: e.reciprocal(dn[:, h, 2:3], dn[:, h, 1:2]), w=[bdn])
                                P.op("dve", lambda e: e.tensor_tensor(dn[:, h, 3:4], dn[:, h, 2:3], gs[:, 8, h:h + 1], ALU.mult), r=[bgs], w=[bdn])
                                P.op("act", lambda e: e.activation(Y[:, 1024 + h * 256:1024 + (h + 1) * 256], ph_[:, 0:256], AF.Identity, scale=dn[:, h, 3:4]),
                                     r=[bdn], w=[bYm, bph])
                        for h in range(4):
                            pc, bpc = psx[0].next()
                            P.op("pe", lambda e: e.matmul(pc[:, 0:257], lhsT=MKt[:, h, :], rhs=Vm[:, h, :], start=True, stop=True), r=[bMKt, bVm], w=[bpc])
                            P.op("dve", lambda e: e.tensor_tensor(C32[:, h, :], C32[:, h, :], pc[:, 0:257], ALU.add), w=[b_C32, bpc])
                        P.rec = lst_O
                        psx[0] = psO
                        if with_out and dr == 0:
                            P.dma(yA_d[tok0:tok0 + 128, :], Y[:], r=[bY, bYm], w=[B_yA], sb=bY)
                        if with_out and dr == 1:
                            YA, bYA = YAr.next()
                            P.dma(YA[:], yA_d[tok0:tok0 + 128, :], r=[B_yA], w=[bYA], sb=bYA)
                            P.op("dve", lambda e: e.tensor_tensor(Y[:], Y[:], YA[:], ALU.add), r=[bYA], w=[bY, bYm])
                            ns, bns = nsr.next()
                            hn, bhn = Y, bY
                            sq, bsq = sqr.next()
                            yo, byo = yor.next()
                            for (gi, c0, ng, gw) in ((0, 0, 8, 128), (1, 1024, 4, 256)):
                                yv = Y[:, c0:c0 + 1024].rearrange("p (g e) -> p g e", g=ng)
                                hv = hn[:, c0:c0 + 1024].rearrange("p (g e) -> p g e", g=ng)
                                sv2 = sq[:, c0:c0 + 1024].rearrange("p (g e) -> p g e", g=ng)
                                st_ = ns[:, gi * 2:gi * 2 + 2, :]
                                P.op("dve", lambda e: e.tensor_reduce(out=st_[:, 0, 0:ng], in_=yv, axis=AX.X, op=ALU.add), r=[bY], w=[bns])
                                P.op("dve", lambda e: e.tensor_scalar(st_[:, 0, 0:ng], st_[:, 0, 0:ng], 1.0 / gw, None, op0=ALU.mult), w=[bns])
                                P.op("dve", lambda e: e.tensor_tensor(hv, yv, st_[:, 0, 0:ng].unsqueeze(2).to_broadcast([128, ng, gw]), ALU.subtract),
                                     r=[bY, bns], w=[bhn])
                                P.op("pool", lambda e: e.tensor_tensor(sv2, hv, hv, ALU.mult), r=[bhn], w=[bsq])
                                P.op("dve", lambda e: e.tensor_reduce(out=st_[:, 1, 0:ng], in_=sv2, axis=AX.X, op=ALU.add), r=[bsq], w=[bns])
                                P.op("dve", lambda e: e.tensor_scalar(st_[:, 1, 0:ng], st_[:, 1, 0:ng], 1.0 / gw, EPS, op0=ALU.mult, op1=ALU.add), w=[bns])
                                P.op("pool", lambda e: e.tensor_tensor(st_[:, 1, 0:ng], st_[:, 1, 0:ng], mhalf[:, 0:ng], ALU.pow), r=[b_mhalf], w=[bns])
                                P.op("dve", lambda e: e.tensor_tensor(hv, hv, st_[:, 1, 0:ng].unsqueeze(2).to_broadcast([128, ng, gw]), ALU.mult),
                                     r=[bns], w=[bhn])
                                gate, bgate = (RG, bRG) if gi == 0 else (MO, bMO)
                                P.op("pool", lambda e: e.tensor_tensor(yo[:, c0:c0 + 1024], hn[:, c0:c0 + 1024], gate[:, j, :], ALU.mult),
                                     r=[bhn, bgate, bYm], w=[byo])
                            for half in range(2):
                                pt, bp = psx[0].next()
                                ptb = pt[:].bitcast(BF16)
                                for kk in range(8):
                                    k = half * 8 + kk
                                    P.op("pe", lambda e: e.transpose(ptb[:, kk * 128:(kk + 1) * 128], yo[:, k * 128:(k + 1) * 128], identb[:]),
                                         r=[byo, b_identb], w=[bp])
                                ov = YOT[:, half * 8:(half + 1) * 8, j * 128:(j + 1) * 128]
                                iv = ptb[:, 0:1024].rearrange("p (k t) -> p k t", k=8)
                                if half == 0:
                                    P.op("act", lambda e: e.copy(ov, iv), w=[bYOT, bp])
                                else:
                                    P.op("dve", lambda e: e.tensor_copy(ov, iv), w=[bYOT, bp])
                        P.rec = None
                        psx[0] = psM
                        P.zip_emit([lst_M, lst_R, pend_out])
                        pend_out = lst_O
                    P.zip_emit([pend_out])
                    if with_out and dr == 1:
                        P.dma(yoT_d.rearrange("(k p) n -> p k n", p=128)[:, :, base:base + NT], YOT[:, :, 0:NT], r=[bYOT], w=[B_yoT], sb=bYOT)

            scan_pass(0)
            scan_pass(1)
        barrier()

        with ExitStack() as ph:
            gt1b, b_gt1b = sbt(ph, "gt1b", [128, D], F32)
            P.dma(gt1b[:], ada_d[0:1, 2 * D:3 * D].partition_broadcast(128), r=[B_ada], w=[b_gt1b], sb=b_gt1b)
            XTr = Ring(nc, ph, "cXT", [128, KC, 512], BF16, 1)
            YTr = Ring(nc, ph, "cYT", [128, KC, 512], BF16, 1)
            MTr = Ring(nc, ph, "cMT", [128, KC, 512], BF16, 1, multi=True)
            Wr = Ring(nc, ph, "cW", [128, KC, 512], BF16, 3)
            sgr = Ring(nc, ph, "sg", [128, 512], F32, 2)
            t1r = Ring(nc, ph, "t1", [128, 512], F32, 2)
            t2r = Ring(nc, ph, "t2", [128, 512], F32, 2)
            xr = Ring(nc, ph, "cx", [128, D], F32, 4, multi=True)
            for s in range(NST):
                base = s * 512
                XT, bXT = XTr.next()
                YT, bYT = YTr.next()
                MT, bMT = MTr.next()
                P.dma(XT[:], xmT_d.rearrange("(k p) n -> p k n", p=128)[:, :, 1 + base:1 + base + 512], r=[B_xmT], w=[bXT], sb=bXT)
                P.dma(YT[:], yoT_d.rearrange("(k p) n -> p k n", p=128)[:, :, base:base + 512], r=[B_yoT], w=[bYT], sb=bYT)
                xs = []
                for j in range(4):
                    xt, bx = xr.next()
                    P.dma(xt[:], x_d[base + j * 128:base + (j + 1) * 128, :], w=[bx], sb=bx)
                    xs.append((xt, bx))
                for cb in range(4):
                    Wgr, bWgr = Wr.next()
                    P.dma(Wgr[:], winb_d[:, O_GR + cb * 512:O_GR + (cb + 1) * 512].rearrange("(k p) n -> p k n", p=128), r=[B_winb], w=[bWgr], sb=bWgr)
                    Wgm, bWgm = Wr.next()
                    P.dma(Wgm[:], winb_d[:, O_GM + cb * 512:O_GM + (cb + 1) * 512].rearrange("(k p) n -> p k n", p=128), r=[B_winb], w=[bWgm], sb=bWgm)
                    Wo, bWo = Wr.next()
                    P.dma(Wo[:, 0:8, :], wrob_d[:, cb * 512:(cb + 1) * 512].rearrange("(k p) n -> p k n", p=128), r=[B_wrob], w=[bWo], sb=bWo)
                    P.dma(Wo[:, 8:16, :], wmob_d[:, cb * 512:(cb + 1) * 512].rearrange("(k p) n -> p k n", p=128), r=[B_wmob], w=[bWo], sb=bWo)
                    for m in range(4):
                        db = cb * 4 + m
                        res = []
                        for (Wg, bWg, k0, tr_) in ((Wgr, bWgr, 0, t1r), (Wgm, bWgm, 8, t2r)):
                            pg_, bpg_ = psr.next()
                            for k in range(KC):
                                P.op("pe", lambda e: e.matmul(pg_[:, 0:512], lhsT=Wg[:, k, m * 128:(m + 1) * 128], rhs=XT[:, k, :],
                                                              start=(k == 0), stop=(k == KC - 1)), r=[bWg, bXT], w=[bpg_])
                            sg, bsg = sgr.next()
                            P.op("act", lambda e: e.activation(sg[:], pg_[:, 0:512], AF.Sigmoid), w=[bsg, bpg_])
                            pp, bpp = psr.next()
                            for k in range(8):
                                P.op("pe", lambda e: e.matmul(pp[:, 0:512], lhsT=Wo[:, k0 + k, m * 128:(m + 1) * 128], rhs=YT[:, k0 + k, :],
                                                              start=(k == 0), stop=(k == 7)), r=[bWo, bYT], w=[bpp])
                            tt, btt = tr_.next()
                            P.op("dve", lambda e: e.tensor_tensor(tt[:], pp[:, 0:512], sg[:], ALU.mult), r=[bsg], w=[btt, bpp])
                            res.append((tt, btt))
                        P.op("pool", lambda e: e.tensor_tensor(MT[:, db, :], res[0][0][:], res[1][0][:], ALU.add), r=[res[0][1], res[1][1]], w=[bMT])
                for cb in range(4):
                    Wo, bWo = Wr.next()
                    P.dma(Wo[:], woutb_d[:, cb * 512:(cb + 1) * 512].rearrange("(k p) n -> p k n", p=128), r=[B_woutb], w=[bWo], sb=bWo)
                    for j in range(4):
                        po, bpo = psr.next()
                        for k in range(KC):
                            P.op("pe", lambda e: e.matmul(po[:, 0:512], lhsT=MT[:, k, j * 128:(j + 1) * 128], rhs=Wo[:, k, :],
                                                          start=(k == 0), stop=(k == KC - 1)), r=[bMT, bWo], w=[bpo])
                        tt, btt = t1r.next()
                        P.op("dve", lambda e: e.tensor_tensor(tt[:], po[:, 0:512], gt1b[:, cb * 512:(cb + 1) * 512], ALU.mult), r=[b_gt1b], w=[btt, bpo])
                        xt, bx = xs[j]
                        P.op("pool", lambda e: e.tensor_tensor(xt[:, cb * 512:(cb + 1) * 512], xt[:, cb * 512:(cb + 1) * 512], tt[:], ALU.add), r=[btt], w=[bx])
                for j in range(4):
                    xt, bx = xs[j]
                    P.dma(x1_d[base + j * 128:base + (j + 1) * 128, :], xt[:], r=[bx], w=[B_x1], sb=bx)
        barrier()

        with ExitStack() as ph:
            Wq, b_Wq = sbt(ph, "Wq", [128, KC, D], BF16)
            for cb in range(4):
                P.dma(Wq[:, :, cb * 512:(cb + 1) * 512], pqb_d[:, cb * 512:(cb + 1) * 512].rearrange("(k p) n -> p k n", p=128), r=[B_pqb], w=[b_Wq], sb=b_Wq)
            keysT, b_keysT = sbt(ph, "keysT", [128, 16, 128], BF16)
            with ExitStack() as ph2:
                kf, b_kf = sbt(ph2, "kf", [128, 16, 128], F32)
                P.dma(kf[:], pk_d.rearrange("(g k) d -> k g d", k=128), w=[b_kf], sb=b_kf)
                for g4 in range(4):
                    pt, bp = psr.next()
                    for gg in range(4):
                        g = g4 * 4 + gg
                        P.op("pe", lambda e: e.transpose(pt[:, gg * 128:(gg + 1) * 128], kf[:, g, :], identf), r=[b_kf, b_cst], w=[bp])
                    P.op("dve", lambda e: e.tensor_copy(keysT[:, g4 * 4:(g4 + 1) * 4, :], pt[:, 0:512].rearrange("p (g k) -> p g k", g=4)), w=[b_keysT, bp])
            barrier()
            gt2b, b_gt2b = sbt(ph, "gt2b", [128, D], F32)
            fgb, b_fgb = sbt(ph, "fgb", [128, D], F32)
            P.dma(gt2b[:], ada_d[0:1, 5 * D:6 * D].partition_broadcast(128), r=[B_ada], w=[b_gt2b], sb=b_gt2b)
            P.dma(fgb[:], fg_d.partition_broadcast(128), w=[b_fgb], sb=b_fgb)
            iota16, b_iota = sbt(ph, "iota16", [128, 16], F32)
            for i in range(16):
                P.op("dve", lambda e: e.memset(iota16[:, i:i + 1], float(i)), w=[b_iota])
            x1r = Ring(nc, ph, "px1", [128, D], F32, 1)
            hnbr = Ring(nc, ph, "phn", [128, D], BF16, 1)
            hbr = Ring(nc, ph, "phb", [128, D], BF16, 2, multi=True)
            hTr = Ring(nc, ph, "phT", [128, KC, 128], BF16, 1, multi=True)
            qTr = Ring(nc, ph, "pqT", [128, 16, 128], BF16, 1, multi=True)
            Scr = Ring(nc, ph, "pSc", [128, 16, 128], F32, 1, multi=True)
            wkr = Ring(nc, ph, "pwk", [128, 16, 128], F32, 1)
            mxr = Ring(nc, ph, "pmx", [128, 16, 16], F32, 1)
            mir = Ring(nc, ph, "pmi", [128, 16, 16], U32, 1)
            mifr = Ring(nc, ph, "pmif", [128, 16, 16], F32, 1)
            bsr = Ring(nc, ph, "pbs", [128, 8, 16], F32, 1)
            bjr = Ring(nc, ph, "pbj", [128, 8, 16], U32, 1)
            ijr = Ring(nc, ph, "pij", [128, 4, 8, 16], F32, 1)
            iju = Ring(nc, ph, "piju", [128, 2, 8, 16], U32, 1)
            eqr = Ring(nc, ph, "peq", [128, 4, 16, 16], F32, 1)
            eidr = Ring(nc, ph, "peid", [128, 128], I32, 2)
            gwr = Ring(nc, ph, "pgw", [128, 2, 128], F32, 2)
            avr = Ring(nc, ph, "pav", [128, 3, 128], F32, 2, multi=True)
            Gr_ = Ring(nc, ph, "pG", [128, 2 * D], BF16, 6)
            jkr = Ring(nc, ph, "pjk", [128, D], BF16, 1)
            dgr = Ring(nc, ph, "pdg", [128, 128], BF16, 4)
            str_ = Ring(nc, ph, "pst", [128, 4], F32, 4)
            pacc = [psr.items[4 + i] for i in range(4)]
            psr.items = psr.items[0:4]
            psr.i = 0

            def rms_rstd(src, bsrc):
                st, bs = str_.next()
                jk, bjk = jkr.next()
                P.op("dve", lambda e: e.scalar_tensor_tensor(out=jk[:], in0=src[:], scalar=1.0, in1=src[:], op0=ALU.mult, op1=ALU.mult, accum_out=st[:, 0:1]),
                     r=[bsrc], w=[bjk, bs])
                P.op("dve", lambda e: e.tensor_scalar(st[:, 1:2], st[:, 0:1], 1.0 / D, EPS, op0=ALU.mult, op1=ALU.add), w=[bs])
                P.op("act", lambda e: e.activation(st[:, 2:3], st[:, 1:2], AF.Sqrt), w=[bs])
                P.op("dve", lambda e: e.reciprocal(st[:, 3:4], st[:, 2:3]), w=[bs])
                return st, bs

            def stage1(blk, res):
                x1, bx1 = x1r.next()
                P.dma(x1[:], x1_d[blk * 128:(blk + 1) * 128, :], r=[B_x1], w=[bx1], sb=bx1)
                st, bs = rms_rstd(x1, bx1)
                yield
                hnb, bhnb = hnbr.next()
                P.op("act", lambda e: e.activation(hnb[:], x1[:], AF.Identity, scale=st[:, 3:4]), r=[bx1, bs], w=[bhnb])
                yield
                hT, bhT = hTr.next()
                for half in range(2):
                    pt, bp = psr.next()
                    ptb = pt[:].bitcast(BF16)
                    for kk in range(8):
                        k = half * 8 + kk
                        P.op("pe", lambda e: e.transpose(ptb[:, kk * 128:(kk + 1) * 128], hnb[:, k * 128:(k + 1) * 128], identb[:]), r=[bhnb, b_identb], w=[bp])
                    for kk in range(8):
                        k = half * 8 + kk
                        if half == 0:
                            P.op("act", lambda e: e.activation(hT[:, k, :], ptb[:, kk * 128:(kk + 1) * 128], AF.Identity, bias=modF[:, 5, k:k + 1], scale=modF[:, 4, k:k + 1]),
                                 r=[b_modF], w=[bhT, bp])
                        else:
                            P.op("dve", lambda e: e.tensor_scalar(hT[:, k, :], ptb[:, kk * 128:(kk + 1) * 128], modF[:, 4, k:k + 1], modF[:, 5, k:k + 1], op0=ALU.mult, op1=ALU.add),
                                 r=[b_modF], w=[bhT, bp])
                    yield
                hb, bhb = hbr.next()
                for half in range(2):
                    pt, bp = psr.next()
                    ptb = pt[:].bitcast(BF16)
                    for kk in range(8):
                        k = half * 8 + kk
                        P.op("pe", lambda e: e.transpose(ptb[:, kk * 128:(kk + 1) * 128], hT[:, k, :], identb[:]), r=[bhT, b_identb], w=[bp])
                    P.op("act", lambda e: e.copy(hb[:, half * 1024:(half + 1) * 1024], ptb[:, 0:1024]), w=[bhb, bp])
                    yield
                qT, bqT = qTr.next()
                for g4 in range(4):
                    pt, bp = psr.next()
                    for gg in range(4):
                        g = g4 * 4 + gg
                        for k in range(KC):
                            P.op("pe", lambda e: e.matmul(pt[:, gg * 128:(gg + 1) * 128], lhsT=Wq[:, k, g * 128:(g + 1) * 128], rhs=hT[:, k, :],
                                                          start=(k == 0), stop=(k == KC - 1)), r=[b_Wq, bhT], w=[bp])
                    P.op("act", lambda e: e.copy(qT[:, g4 * 4:(g4 + 1) * 4, :], pt[:, 0:512].rearrange("p (g t) -> p g t", g=4)), w=[bqT, bp])
                    yield
                Sc, bSc = Scr.next()
                for g4 in range(4):
                    pt, bp = psr.next()
                    for gg in range(4):
                        g = g4 * 4 + gg
                        P.op("pe", lambda e: e.matmul(pt[:, gg * 128:(gg + 1) * 128], lhsT=qT[:, g, :], rhs=keysT[:, g, :], start=True, stop=True),
                             r=[bqT, b_keysT], w=[bp])
                    P.op("act", lambda e: e.copy(Sc[:, g4 * 4:(g4 + 1) * 4, :], pt[:, 0:512].rearrange("p (g t) -> p g t", g=4)), w=[bSc, bp])
                    yield
                mx, bmx = mxr.next()
                mi, bmi = mir.next()
                wk, bwk = wkr.next()
                for g in range(16):
                    P.op("dve", lambda e: e.max(out=mx[:, g, 0:8], in_=Sc[:, g, :]), r=[bSc], w=[bmx])
                    yield
                    P.op("dve", lambda e: e.max_index(out=mi[:, g, 0:8], in_max=mx[:, g, 0:8], in_values=Sc[:, g, :]), r=[bSc, bmx], w=[bmi])
                    yield
                    P.op("dve", lambda e: e.match_replace(out=wk[:, g, :], in_to_replace=mx[:, g, 0:8], in_values=Sc[:, g, :], imm_value=NEG), r=[bSc, bmx], w=[bwk])
                    yield
                    P.op("dve", lambda e: e.max(out=mx[:, g, 8:16], in_=wk[:, g, :]), r=[bwk], w=[bmx])
                    yield
                    P.op("dve", lambda e: e.max_index(out=mi[:, g, 8:16], in_max=mx[:, g, 8:16], in_values=wk[:, g, :]), r=[bwk, bmx], w=[bmi])
                    yield
                    yield
                mif, bmif = mifr.next()
                P.op("dve", lambda e: e.tensor_copy(mif[:], mi[:]), r=[bmi], w=[bmif])
                cand_t, bcand = wkr.next()
                cand = cand_t[:].rearrange("p (h a) k -> p h (a k)", a=2)
                mxv = mx[:].rearrange("p (h t) i -> p h t i", t=2)
                P.op("pool", lambda e: e.tensor_tensor(cand.rearrange("p h (i j) -> p h i j", i=16),
                                                      mxv[:, :, 0, :].unsqueeze(3).to_broadcast([128, 8, 16, 16]),
                                                      mxv[:, :, 1, :].unsqueeze(2).to_broadcast([128, 8, 16, 16]), ALU.add), r=[bmx], w=[bcand])
                yield
                bs_, bbs = bsr.next()
                bj, bbj = bjr.next()
                cw2_t, bcw2 = Scr.next()
                cw2 = cw2_t[:].rearrange("p (h a) k -> p h (a k)", a=2)
                for h in range(8):
                    P.op("dve", lambda e: e.max(out=bs_[:, h, 0:8], in_=cand[:, h, :]), r=[bcand], w=[bbs])
                    yield
                    P.op("dve", lambda e: e.max_index(out=bj[:, h, 0:8], in_max=bs_[:, h, 0:8], in_values=cand[:, h, :]), r=[bcand, bbs], w=[bbj])
                    yield
                    P.op("dve", lambda e: e.match_replace(out=cw2[:, h, :], in_to_replace=bs_[:, h, 0:8], in_values=cand[:, h, :], imm_value=NEG), r=[bcand, bbs], w=[bcw2])
                    yield
                    P.op("dve", lambda e: e.max(out=bs_[:, h, 8:16], in_=cw2[:, h, :]), r=[bcw2], w=[bbs])
                    yield
                    P.op("dve", lambda e: e.max_index(out=bj[:, h, 8:16], in_max=bs_[:, h, 8:16], in_values=cw2[:, h, :]), r=[bcw2, bbs], w=[bbj])
                    yield
                    yield
                ju, bju = iju.next()
                ij, bij = ijr.next()
                P.op("dve", lambda e: e.tensor_single_scalar(ju[:, 0], bj[:], 4, op=ALU.logical_shift_right), r=[bbj], w=[bju])
                P.op("dve", lambda e: e.tensor_single_scalar(ju[:, 1], bj[:], 15, op=ALU.bitwise_and), r=[bbj], w=[bju])
                P.op("dve", lambda e: e.tensor_copy(ij[:, 0:2], ju[:]), r=[bju], w=[bij])
                mifv = mif[:].rearrange("p (h t) i -> p h t i", t=2)
                for t in range(2):
                    for hh in range(2):
                        hs = slice(hh * 4, (hh + 1) * 4)
                        eq, beq = eqr.next()
                        P.op("dve", lambda e: e.tensor_tensor(eq[:], ij[:, t, hs].unsqueeze(3).to_broadcast([128, 4, 16, 16]),
                                                               iota16[:].unsqueeze(1).unsqueeze(1).to_broadcast([128, 4, 16, 16]), ALU.is_equal), r=[bij, b_iota], w=[beq])
                        P.op("pool", lambda e: e.tensor_tensor(eq[:], eq[:], mifv[:, hs, t, :].unsqueeze(2).to_broadcast([128, 4, 16, 16]), ALU.mult), r=[bmif], w=[beq])
                        P.op("dve", lambda e: e.tensor_reduce(out=ij[:, 2 + t, hs], in_=eq[:], axis=AX.X, op=ALU.add), r=[beq], w=[bij])
                        yield
                P.op("dve", lambda e: e.scalar_tensor_tensor(out=ij[:, 0], in0=ij[:, 2], scalar=128.0, in1=ij[:, 3], op0=ALU.mult, op1=ALU.add), w=[bij])
                eid, beid = eidr.next()
                P.op("dve", lambda e: e.tensor_copy(eid[:], ij[:, 0].rearrange("p h k -> p (h k)")), r=[bij], w=[beid])
                yield
                gw, bgw = gwr.next()
                gwv = gw[:, 0, :].rearrange("p (h k) -> p h k", h=8)
                P.op("dve", lambda e: e.tensor_tensor(gwv, bs_[:], bs_[:, :, 0:1].to_broadcast([128, 8, 16]), ALU.subtract), r=[bbs], w=[bgw])
                P.op("act", lambda e: e.activation(gw[:, 0, :], gw[:, 0, :], AF.Exp), w=[bgw])
                P.op("dve", lambda e: e.tensor_reduce(out=gw[:, 1, 0:8], in_=gwv, axis=AX.X, op=ALU.add), w=[bgw])
                P.op("dve", lambda e: e.reciprocal(gw[:, 1, 0:8], gw[:, 1, 0:8]), w=[bgw])
                P.op("dve", lambda e: e.tensor_tensor(gwv, gwv, gw[:, 1, 0:8].unsqueeze(2).to_broadcast([128, 8, 16]), ALU.mult), w=[bgw])
                res.update(hb=hb, bhb=bhb, eid=eid, beid=beid, gw=gw, bgw=bgw)

            def stage2(blk, s_, bg=None):
                hb, bhb, eid, beid, gw, bgw = (s_[k_] for k_ in ("hb", "bhb", "eid", "beid", "gw", "bgw"))
                av, bav = avr.next()
                jk, bjk = jkr.next()
                pend = None
                bsl = [Buf("sl%d" % i_) for i_ in range(128)]
                for b__ in bsl:
                    b__.w = dict(bav.base)

                def tail(slot, Gt, bGt):
                    P.op("dve", lambda e: e.tensor_tensor(av[:, 2, slot:slot + 1], av[:, 1, slot:slot + 1], gw[:, 0, slot:slot + 1], ALU.mult), r=[bsl[slot], bgw], w=[bsl[slot]])
                    dg, bdg = dgr.next()
                    P.op("act", lambda e: e.activation(dg[:], identb[:], AF.Identity, scale=av[:, 2, slot:slot + 1]), r=[b_identb, bsl[slot]], w=[bdg])
                    for cb in range(4):
                        po, bpo = pacc[cb]
                        P.op("pe", lambda e: e.matmul(po[:, 0:512], lhsT=dg[:], rhs=Gt[:, D + cb * 512:D + (cb + 1) * 512], start=(slot == 0), stop=(slot == 127)),
                             r=[bdg, bGt], w=[bpo])

                for slot in range(128):
                    Gt, bGt = Gr_.next()
                    P.gather(Gt[:], pcomb_d, eid[:, slot:slot + 1].bitcast(U32), r=[beid, B_pdnb, B_pupb], w=[bGt], sb=bGt)
                    P.op("dve", lambda e: e.scalar_tensor_tensor(out=jk[:], in0=Gt[:, 0:D], scalar=1.0, in1=hb[:], op0=ALU.mult, op1=ALU.mult,
                                                                 accum_out=av[:, 0, slot:slot + 1]), r=[bGt, bhb], w=[bsl[slot]])
                    P.op("act", lambda e: e.activation(av[:, 1, slot:slot + 1], av[:, 0, slot:slot + 1], AF.Gelu), r=[bsl[slot]], w=[bsl[slot]])
                    if pend is not None:
                        tail(*pend)
                    pend = (slot, Gt, bGt)
                    if bg is not None and slot >= 4:
                        next(bg, None)
                        next(bg, None)
                tail(*pend)
                if bg is not None:
                    for _ in bg:
                        pass
                for b__ in bsl:
                    for k__, v__ in list(b__.w.items()) + list(b__.r.items()):
                        if bav.w.get(k__, 0) < v__:
                            bav.w[k__] = v__
                tmG, btm = Gr_.next()
                tm = tmG[:].bitcast(F32)
                x1G, bx1 = Gr_.next()
                x1 = x1G[:].bitcast(F32)
                P.dma(x1, x1_d[blk * 128:(blk + 1) * 128, :], r=[B_x1], w=[bx1], sb=bx1)
                for cb in range(4):
                    po, bpo = pacc[cb]
                    P.op("dve", lambda e: e.tensor_tensor(tm[:, cb * 512:(cb + 1) * 512], po[:, 0:512], gt2b[:, cb * 512:(cb + 1) * 512], ALU.mult), r=[b_gt2b], w=[btm, bpo])
                P.op("pool", lambda e: e.tensor_tensor(x1, x1, tm, ALU.add), r=[btm], w=[bx1])
                st2, bs2 = str_.next()
                jk, bjk = jkr.next()
                P.op("dve", lambda e: e.scalar_tensor_tensor(out=jk[:], in0=x1, scalar=1.0, in1=x1, op0=ALU.mult, op1=ALU.mult, accum_out=st2[:, 0:1]),
                     r=[bx1], w=[bjk, bs2])
                P.op("dve", lambda e: e.tensor_scalar(st2[:, 1:2], st2[:, 0:1], 1.0 / D, EPS, op0=ALU.mult, op1=ALU.add), w=[bs2])
                P.op("act", lambda e: e.activation(st2[:, 2:3], st2[:, 1:2], AF.Sqrt), w=[bs2])
                P.op("dve", lambda e: e.reciprocal(st2[:, 3:4], st2[:, 2:3]), w=[bs2])
                P.op("pool", lambda e: e.scalar_tensor_tensor(out=tm, in0=x1, scalar=st2[:, 3:4], in1=fgb[:], op0=ALU.mult, op1=ALU.mult), r=[bx1, bs2, b_fgb], w=[btm]) if False else \
                    P.op("dve", lambda e: e.scalar_tensor_tensor(out=tm, in0=x1, scalar=st2[:, 3:4], in1=fgb[:], op0=ALU.mult, op1=ALU.mult), r=[bx1, bs2, b_fgb], w=[btm])
                P.dma(out_d[blk * 128:(blk + 1) * 128, :], tm, r=[btm], w=[B_out], sb=btm)

            NBLK = H // 128
            cur = {}
            for _ in stage1(0, cur):
                pass
            for blk in range(NBLK):
                nxt = {}
                bg = stage1(blk + 1, nxt) if blk + 1 < NBLK else None
                stage2(blk, cur, bg)
                cur = nxt
        P.finish()
    return nc


_PROG_CACHE = {}


def _host_inputs(inp, L, cores):
    f32 = np.float32
    x = np.asarray(inp["x"], f32)
    ctx = np.asarray(inp["ctx"], f32)
    c = np.asarray(inp["c"], f32)
    c_ctx = np.asarray(inp["c_ctx"], f32)
    w_in = np.ascontiguousarray(np.asarray(inp["w_in"], f32)[0])
    shared = dict(
        w_ada=np.ascontiguousarray(np.asarray(inp["w_ada"], f32)[0]),
        b_ada=np.asarray(inp["b_ada"], f32).reshape(1, -1),
        norm1_g=np.asarray(inp["norm1_g"], f32).reshape(1, -1),
        w_in=w_in,
        w_ret_out=np.ascontiguousarray(np.asarray(inp["w_ret_out"], f32)[0]),
        w_mlstm_out=np.ascontiguousarray(np.asarray(inp["w_mlstm_out"], f32)[0]),
        w_out=np.ascontiguousarray(np.asarray(inp["w_out"], f32)[0]),
        norm2_g=np.asarray(inp["norm2_g"], f32).reshape(1, -1),
        peer_query=np.ascontiguousarray(np.asarray(inp["peer_query"], f32)[0]),
        peer_keys=np.ascontiguousarray(np.asarray(inp["peer_keys"], f32)[0].reshape(16 * 128, 128)),
        peer_down=np.ascontiguousarray(np.asarray(inp["peer_down"], f32)[0]),
        peer_up=np.ascontiguousarray(np.asarray(inp["peer_up"], f32)[0]),
        final_g=np.asarray(inp["final_g"], f32).reshape(1, -1),
    )
    p = np.arange(128)
    ident = (p[:, None] == p[None, :]).astype(f32)
    triA = (p[:, None] <= p[None, :]).astype(f32)
    triB = (p[:, None] >= p[None, :]).astype(f32)
    negA = (triB - 1.0) * 1.0e30
    negB = (triA - 1.0) * 1.0e30
    ones = np.ones((128, 128), f32)
    cst = np.ascontiguousarray(np.stack([ident, triA, triB, negA, negB, ones], axis=1).astype(f32))
    pos = np.zeros((128, 4), f32)
    pos[:, 0] = p + 1.0
    pos[:, 1] = 128.0 - p
    shared["cst"] = cst
    shared["pos"] = pos
    w_mg = w_in[:, O_MG:O_MG + 16]
    gbias = np.asarray(inp["m_gate_bias"], f32)[0].reshape(16)
    rdec = np.asarray(inp["ret_decay"], f32)[0]
    mconv = np.asarray(inp["m_conv"], f32)[0]
    n_ax = 16
    freq = (10000.0 ** (-np.arange(n_ax, dtype=np.float64) / n_ax))
    maps = []
    for (b, half) in cores:
        t = np.arange(L) if half == 0 else np.arange(L - 1, -1, -1)
        row = (t // 64).astype(np.float64)
        col = (t % 64).astype(np.float64)
        ang = np.concatenate([row[:, None] * freq, col[:, None] * freq], axis=-1)
        rot = np.concatenate([np.cos(ang), np.sin(ang)], axis=-1).astype(f32)
        m = dict(shared)
        if half == 0:
            m["x"] = np.ascontiguousarray(x[b])
            m["ctx"] = np.ascontiguousarray(ctx[b])
            m["w_mg"] = np.ascontiguousarray(w_mg)
            m["gbias"] = gbias.reshape(1, 16).copy()
            m["ret_decay"] = rdec.reshape(1, 16).copy()
            m["m_conv"] = np.ascontiguousarray(mconv)
        else:
            m["x"] = np.ascontiguousarray(x[b, ::-1])
            m["ctx"] = np.ascontiguousarray(ctx[b, ::-1])
            m["w_mg"] = np.ascontiguousarray(np.concatenate([w_mg[:, 8:16], w_mg[:, 0:8]], axis=1))
            m["gbias"] = np.concatenate([gbias[8:16], gbias[0:8]]).reshape(1, 16).copy()
            m["ret_decay"] = np.concatenate([rdec[1], rdec[0]]).reshape(1, 16).copy()
            m["m_conv"] = np.ascontiguousarray(mconv[::-1])
        m["cvec"] = np.ascontiguousarray(np.stack([c[b], c_ctx], axis=0))
        m["rot"] = rot
        maps.append(m)
    return maps


def kernel(**inputs):
    x = np.asarray(inputs["x"])
    B, L, _ = x.shape
    cores = [(b, h) for b in range(B) for h in range(2)]
    if L not in _PROG_CACHE:
        _PROG_CACHE[L] = build_program(L)
    nc = _PROG_CACHE[L]
    maps = _host_inputs(inputs, L, cores)
    res = run_bass_kernel_spmd(nc, maps, core_ids=list(range(len(cores))))
    out = np.empty((B, L, D), np.float32)
    Hh = L // 2
    for i, (b, h) in enumerate(cores):
        o = np.asarray(res.results[i]["out"], np.float32)
        if h == 0:
            out[b, 0:Hh] = o
        else:
            out[b, Hh:L] = o[::-1]
    return out
```

```python
import math
from contextlib import ExitStack
import numpy as np
import concourse.bass as bass
import concourse.mybir as mybir
from concourse.bass_utils import run_bass_kernel_spmd

F32 = mybir.dt.float32
BF16 = mybir.dt.bfloat16
I32 = mybir.dt.int32
U32 = mybir.dt.uint32
ALU = mybir.AluOpType
AF = mybir.ActivationFunctionType
AX = mybir.AxisListType

D = 2048
KC = 16
EPS = 1e-6
NEG = -1.0e30


class Buf:
    __slots__ = ("name", "w", "r", "multi", "sem", "base")

    def __init__(self, name, multi=False):
        self.name = name
        self.w = {}
        self.r = {}
        self.multi = multi
        self.sem = None
        self.base = {}

    def fresh(self):
        base = dict(self.w)
        for k, v in self.r.items():
            if base.get(k, 0) < v:
                base[k] = v
        self.base = base
        self.w = {}
        self.r = {}


class Prog:
    def __init__(self, nc, es):
        self.nc = nc
        self.es = es
        self.eng = {"pe": nc.tensor, "dve": nc.vector, "act": nc.scalar, "pool": nc.gpsimd, "sp": nc.sync}
        self.sems = {}
        self.cnt = {}
        self.waited = {e: {} for e in self.eng}
        for e in self.eng:
            self.sems[e] = es.enter_context(nc.semaphore("s_" + e))
            self.cnt[e] = 0
        self.ndsem = 0
        self.ninst = 0
        self.rec = None

    class _RecEng:
        def __init__(self):
            self.call = None

        def __getattr__(self, name):
            def f(*a, **k):
                self.call = (name, a, k)
                return None
            return f

    def zip_emit(self, lists):
        saved, self.rec = self.rec, None
        lists = [l for l in lists if l]
        idx = [0] * len(lists)
        total = sum(len(l) for l in lists)
        for _ in range(total):
            best = min((idx[i] / len(lists[i]), i) for i in range(len(lists)) if idx[i] < len(lists[i]))[1]
            it = lists[best][idx[best]]
            idx[best] += 1
            if it[0] == "fresh":
                it[1].fresh()
            elif it[0] == "op":
                _, e, name, a_, k_, r, w = it
                self.op(e, lambda eng, name=name, a_=a_, k_=k_: getattr(eng, name)(*a_, **k_), r, w)
            else:
                _, out, in_, r, w, sb, e, kw = it
                self.dma(out, in_, r=r, w=w, sb=sb, e=e, **kw)
        self.rec = saved

    def _deps(self, r, w):
        deps = {}

        def add(k, v):
            if deps.get(k, 0) < v:
                deps[k] = v
        for b in r:
            for k, v in b.w.items():
                add(k, v)
        for b in w:
            if not b.multi:
                for k, v in b.w.items():
                    add(k, v)
                for k, v in b.r.items():
                    add(k, v)
            else:
                for k, v in b.base.items():
                    add(k, v)
        return deps

    def _wait(self, e, deps):
        wd = self.waited[e]
        for k, v in deps.items():
            if e == "pe" and k == "pe":
                continue
            if wd.get(k, 0) >= v:
                continue
            self.eng[e].wait_ge(self.sems[k], v)
            wd[k] = v
            self.ninst += 1

    def _mark(self, key, val, r, w):
        for b in r:
            if b.r.get(key, 0) < val:
                b.r[key] = val
        for b in w:
            if b.multi:
                if b.w.get(key, 0) < val:
                    b.w[key] = val
            else:
                b.w = {key: val}
                b.r = {}

    def op(self, e, fn, r=(), w=()):
        if self.rec is not None:
            pe = Prog._RecEng()
            fn(pe)
            name, a_, k_ = pe.call
            self.rec.append(("op", e, name, a_, k_, tuple(r), tuple(w)))
            return None
        self._wait(e, self._deps(r, w))
        ins = fn(self.eng[e])
        self.cnt[e] += 1
        ins.then_inc(self.sems[e], 1)
        self.ninst += 1
        self._mark(e, self.cnt[e], r, w)
        return ins

    def dma(self, out, in_, r=(), w=(), sb=None, e="sp", **kw):
        if self.rec is not None:
            self.rec.append(("dma", out, in_, tuple(r), tuple(w), sb, e, kw))
            return None
        if sb.sem is None:
            key = "d%d" % self.ndsem
            self.ndsem += 1
            self.sems[key] = self.es.enter_context(self.nc.semaphore("s_" + key))
            self.cnt[key] = 0
            sb.sem = key
        key = sb.sem
        self._wait(e, self._deps(r, w))
        ins = self.eng[e].dma_start(out=out, in_=in_, **kw)
        self.cnt[key] += 16
        ins.then_inc(self.sems[key], 16)
        self.ninst += 1
        self._mark(key, self.cnt[key], r, w)
        return ins

    def gather(self, out, table, idx_ap, r=(), w=(), sb=None):
        if sb.sem is None:
            key = "d%d" % self.ndsem
            self.ndsem += 1
            self.sems[key] = self.es.enter_context(self.nc.semaphore("s_" + key))
            self.cnt[key] = 0
            sb.sem = key
        key = sb.sem
        e = "pool"
        self._wait(e, self._deps(r, w))
        ins = self.nc.gpsimd.indirect_dma_start(
            out=out, out_offset=None, in_=table,
            in_offset=bass.IndirectOffsetOnAxis(ap=idx_ap, axis=0))
        self.cnt[key] += 16
        ins.then_inc(self.sems[key], 16)
        self.ninst += 1
        self._mark(key, self.cnt[key], r, w)
        return ins

    def finish(self):
        for k, v in self.cnt.items():
            if v > 0 and k != "sp":
                self.nc.sync.wait_ge(self.sems[k], v)


R_HEADS, R_DK, R_DV = 8, 64, 128
M_HEADS, M_DK, M_DV = 4, 128, 256
IN_SPLITS = (512, 512, 1024, 1024, 512, 512, 1024, 1024, 16, 2048, 2048)
IN_OFF = [0]
for _s in IN_SPLITS:
    IN_OFF.append(IN_OFF[-1] + _s)
(O_RQ, O_RK, O_RV, O_RG, O_MQ, O_MK, O_MV, O_MO, O_MG, O_GR, O_GM, IN_COLS) = IN_OFF
P_HEADS, P_NK, P_TOPK = 8, 128, 16
NEXP = 16384
CL = 256


class Ring:
    prog = None

    def __init__(self, nc, es, name, shape, dt, n, psum=False, multi=False):
        self.items = []
        self.multi = multi
        for i in range(n):
            if psum:
                t = es.enter_context(nc.psum_tensor("r_%s%d" % (name, i), shape, dt))
            else:
                t = es.enter_context(nc.sbuf_tensor("r_%s%d" % (name, i), shape, dt))
            self.items.append((t, Buf("%s%d" % (name, i), multi)))
        self.i = 0

    def next(self):
        it = self.items[self.i % len(self.items)]
        self.i += 1
        if self.multi:
            if Ring.prog is not None and Ring.prog.rec is not None:
                Ring.prog.rec.append(("fresh", it[1]))
            else:
                it[1].fresh()
        return it


class RingView:
    def __init__(self, items):
        self.items = list(items)
        self.i = 0

    def next(self):
        it = self.items[self.i % len(self.items)]
        self.i += 1
        return it


def build_program(L, dbg=False):
    H = L // 2
    NST = H // 512
    nc = bass.Bass("TRN2", target_bir_lowering=False)

    def din(name, shape, dt=F32):
        return nc.dram_tensor(name, list(shape), dt, kind="ExternalInput").ap()

    def dscr(name, shape, dt):
        return nc.dram_tensor(name, list(shape), dt, kind=("ExternalOutput" if dbg else "Internal")).ap()

    x_d = din("x", [L, D])
    ctx_d = din("ctx", [CL, D])
    cvec_d = din("cvec", [2, D])
    wada_d = din("w_ada", [D, 6 * D])
    bada_d = din("b_ada", [1, 6 * D])
    g1_d = din("norm1_g", [1, D])
    win_d = din("w_in", [D, IN_COLS])
    wmg_d = din("w_mg", [D, 16])
    gbias_d = din("gbias", [1, 16])
    rdec_d = din("ret_decay", [1, 16])
    mconv_d = din("m_conv", [3, 1024])
    wro_d = din("w_ret_out", [1024, D])
    wmo_d = din("w_mlstm_out", [1024, D])
    wout_d = din("w_out", [D, D])
    g2_d = din("norm2_g", [1, D])
    pq_d = din("peer_query", [D, D])
    pk_d = din("peer_keys", [16 * 128, 128])
    pdn_d = din("peer_down", [NEXP, D])
    pup_d = din("peer_up", [NEXP, D])
    fg_d = din("final_g", [1, D])
    rot_d = din("rot", [L, 64])
    cst_d = din("cst", [128, 6, 128])
    pos_d = din("pos", [128, 4])
    out_d = nc.dram_tensor("out", [H, D], F32, kind="ExternalOutput").ap()

    xmT_d = dscr("xmT", [D, L + 2], BF16)
    cmT_d = dscr("cmT", [D, CL + 2], BF16)
    ada_d = dscr("ada", [2, 6 * D], F32)
    winb_d = dscr("winb", [D, IN_COLS], BF16)
    wrob_d = dscr("wrob", [1024, D], BF16)
    wmob_d = dscr("wmob", [1024, D], BF16)
    woutb_d = dscr("woutb", [D, D], BF16)
    pqb_d = dscr("pqb", [D, D], BF16)
    pcomb_d = dscr("pcomb", [NEXP, 2 * D], BF16)
    yA_d = dscr("yA", [H, D], F32)
    yoT_d = dscr("yoT", [D, H], BF16)
    x1_d = dscr("x1", [H, D], F32)
    B_xmT, B_cmT, B_ada = Buf("xmT_d", True), Buf("cmT_d", True), Buf("ada_d")
    B_winb, B_wrob, B_wmob, B_woutb, B_pqb = (Buf(n, True) for n in ("winb", "wrob", "wmob", "woutb", "pqb"))
    B_pdnb, B_pupb = Buf("pdnb", True), Buf("pupb", True)
    B_yA, B_yoT, B_x1, B_out = Buf("yA", True), Buf("yoT", True), Buf("x1", True), Buf("out", True)

    with ExitStack() as es:
        P = Prog(nc, es)
        Ring.prog = P

        def barrier():
            for e in ("pe", "dve", "act", "pool", "sp"):
                P._wait(e, {k: v for k, v in P.cnt.items() if v > 0 and k != e})

        def sbt(st, name, shape, dt):
            return st.enter_context(nc.sbuf_tensor("t_" + name, shape, dt)), Buf(name)

        cst, b_cst = sbt(es, "cst", [128, 6, 128], F32)
        pos, b_pos = sbt(es, "pos", [128, 4], F32)
        identb, b_identb = sbt(es, "identb", [128, 128], BF16)
        P.dma(cst[:], cst_d, w=[b_cst], sb=b_cst)
        P.dma(pos[:], pos_d, w=[b_pos], sb=b_pos)
        P.op("dve", lambda e: e.tensor_copy(identb[:], cst[:, 0, :]), r=[b_cst], w=[b_identb])
        identf = cst[:, 0, :]
        tri = [cst[:, 1, :], cst[:, 2, :]]
        negm = [cst[:, 3, :], cst[:, 4, :]]
        onesf = cst[:, 5, :]
        modF, b_modF = sbt(es, "modF", [128, 6, KC], F32)
        psr = Ring(nc, es, "ps", [128, 512], F32, 8, psum=True)

        with ExitStack() as ph:
            cT, b_cT = sbt(ph, "cT", [128, 2, KC], F32)
            cTs, b_cTs = sbt(ph, "cTs", [128, 2, KC], F32)
            for r_ in range(2):
                P.dma(cT[:, r_, :], cvec_d[r_:r_ + 1, :].rearrange("r (k p) -> p (r k)", p=128), w=[b_cT], sb=b_cT,
                      allow_slow_non_contiguous=True)
            P.op("act", lambda e: e.activation(cTs[:], cT[:], AF.Silu), r=[b_cT], w=[b_cTs])
            adasb, b_adasb = sbt(ph, "adasb", [2, 6 * D], F32)
            badasb, b_badasb = sbt(ph, "badasb", [2, 6 * D], F32)
            P.dma(badasb[:], bada_d.partition_broadcast(2), w=[b_badasb], sb=b_badasb)
            wring = Ring(nc, ph, "wada", [128, KC, 512], F32, 2)
            for nb in range(24):
                wt, bw = wring.next()
                P.dma(wt[:], wada_d[:, nb * 512:(nb + 1) * 512].rearrange("(k p) n -> p k n", p=128),
                      w=[bw], sb=bw)
                pt, bp = psr.next()
                for k in range(KC):
                    P.op("pe", lambda e: e.matmul(pt[0:2, :], lhsT=cTs[:, :, k], rhs=wt[:, k, :],
                                                  start=(k == 0), stop=(k == KC - 1)),
                         r=[b_cTs, bw], w=[bp])
                P.op("dve", lambda e: e.tensor_tensor(adasb[:, nb * 512:(nb + 1) * 512], pt[0:2, :],
                                                      badasb[:, nb * 512:(nb + 1) * 512], ALU.add),
                     r=[bp, b_badasb], w=[b_adasb])
            P.dma(ada_d, adasb[:], r=[b_adasb], w=[B_ada], sb=b_adasb)
            raw, b_raw = sbt(ph, "rawmod", [128, 8, KC], F32)
            srcs = [ada_d[0:1, 0:D], ada_d[0:1, D:2 * D], ada_d[0:1, 3 * D:4 * D], ada_d[0:1, 4 * D:5 * D],
                    ada_d[1:2, 0:D], ada_d[1:2, D:2 * D], g1_d, g2_d]
            for i, s in enumerate(srcs):
                P.dma(raw[:, i, :], s.rearrange("r (k p) -> p (r k)", p=128), r=[B_ada], w=[b_raw], sb=b_raw,
                      allow_slow_non_contiguous=True)
            for (dst, sc, g, sh) in ((0, 1, 6, 0), (2, 5, 6, 4), (4, 3, 7, 2)):
                P.op("dve", lambda e: e.scalar_tensor_tensor(out=modF[:, dst, :], in0=raw[:, sc, :], scalar=1.0,
                                                             in1=raw[:, g, :], op0=ALU.add, op1=ALU.mult),
                     r=[b_raw], w=[b_modF])
                P.op("dve", lambda e: e.tensor_copy(modF[:, dst + 1, :], raw[:, sh, :]), r=[b_raw], w=[b_modF])
        barrier()

        with ExitStack() as ph:
            lst_b, lst_c = [], []
            P.rec = lst_b
            FMAX = 4096
            cin = Ring(nc, ph, "cvin", [128, FMAX], F32, 5)
            cout = Ring(nc, ph, "cvout", [128, FMAX], BF16, 5)
            cnt = [0]

            def convert(pairs, bdst):
                for (sa, da) in pairs:
                    fsz = sa.shape[1]
                    ti, bi = cin.next()
                    to, bo = cout.next()
                    P.dma(ti[:, 0:fsz], sa, w=[bi], sb=bi)
                    eng = ("dve", "act")[cnt[0] % 2]
                    cnt[0] += 1
                    if eng == "act":
                        P.op("act", lambda e: e.copy(to[:, 0:fsz], ti[:, 0:fsz]), r=[bi], w=[bo])
                    else:
                        P.op(eng, lambda e: e.tensor_copy(to[:, 0:fsz], ti[:, 0:fsz]), r=[bi], w=[bo])
                    if len(da.shape) == 3:
                        P.dma(da, to[:, 0:fsz].rearrange("p (r c) -> p r c", r=da.shape[1]), r=[bo], w=[bdst], sb=bo)
                    else:
                        P.dma(da, to[:, 0:fsz], r=[bo], w=[bdst], sb=bo)

            convert([(win_d[rb * 128:(rb + 1) * 128, cb * 2564:(cb + 1) * 2564], winb_d[rb * 128:(rb + 1) * 128, cb * 2564:(cb + 1) * 2564])
                     for rb in range(16) for cb in range(4)], B_winb)
            for s_, d_, b_ in ((wro_d, wrob_d, B_wrob), (wmo_d, wmob_d, B_wmob), (wout_d, woutb_d, B_woutb), (pq_d, pqb_d, B_pqb)):
                sv_ = s_.rearrange("(rb p r) c -> rb p (r c)", p=128, r=2)
                dv_ = d_.rearrange("(rb p r) c -> rb p (r c)", p=128, r=2)
                convert([(sv_[i], dv_[i]) for i in range(sv_.shape[0])], b_)
            for s_, c0_, b_ in ((pdn_d, 0, B_pdnb), (pup_d, D, B_pupb)):
                sv_ = s_.rearrange("(rb p r) c -> rb p (r c)", p=128, r=2)
                dv_ = pcomb_d.rearrange("(rb p r) c -> rb p r c", p=128, r=2)
                convert([(sv_[i], dv_[i][:, :, c0_:c0_ + D]) for i in range(sv_.shape[0])], b_)
            P.rec = lst_c
            xin = Ring(nc, ph, "xin", [128, D], F32, 3)
            xnr = Ring(nc, ph, "xn", [128, D], BF16, 2)
            jk, b_jk = sbt(ph, "jk", [128, D], BF16)
            stat = Ring(nc, ph, "stat", [128, 4], F32, 3)
            xts = Ring(nc, ph, "xts", [128, KC, 512], BF16, 2, multi=True)
            zt, b_zt = sbt(ph, "zt", [128, KC, 1], BF16)
            P.op("pool", lambda e: e.memset(zt[:], 0.0), w=[b_zt])
            for dst, bd, n in ((xmT_d, B_xmT, L), (cmT_d, B_cmT, CL)):
                v = dst.rearrange("(k p) n -> p k n", p=128)
                P.dma(v[:, :, 0:1], zt[:], r=[b_zt], w=[bd], sb=b_zt, allow_slow_non_contiguous=True)
                P.dma(v[:, :, n + 1:n + 2], zt[:], r=[b_zt], w=[bd], sb=b_zt, allow_slow_non_contiguous=True)
            units = [(ctx_d, cmT_d, B_cmT, 0, CL, 2)] + [(x_d, xmT_d, B_xmT, s * 512, 512, 0) for s in range(L // 512)]
            ecnt = 0
            for (src, dst, bd, base, NT, mi) in units:
                XT, bXT = xts.next()
                for j in range(NT // 128):
                    xt, bx = xin.next()
                    P.dma(xt[:], src[base + j * 128: base + (j + 1) * 128, :], w=[bx], sb=bx)
                    st, bs = stat.next()
                    P.op("dve", lambda e: e.scalar_tensor_tensor(out=jk[:], in0=xt[:], scalar=1.0, in1=xt[:],
                                                                 op0=ALU.mult, op1=ALU.mult, accum_out=st[:, 0:1]),
                         r=[bx], w=[b_jk, bs])
                    P.op("dve", lambda e: e.tensor_scalar(st[:, 1:2], st[:, 0:1], 1.0 / D, EPS, op0=ALU.mult, op1=ALU.add),
                         r=[bs], w=[bs])
                    P.op("act", lambda e: e.activation(st[:, 2:3], st[:, 1:2], AF.Sqrt), r=[bs], w=[bs])
                    P.op("dve", lambda e: e.reciprocal(st[:, 3:4], st[:, 2:3]), r=[bs], w=[bs])
                    xn, bxn = xnr.next()
                    P.op("act", lambda e: e.activation(xn[:], xt[:], AF.Identity, scale=st[:, 3:4]), r=[bx, bs], w=[bxn])
                    for half in range(2):
                        pt, bp = psr.next()
                        ptb = pt[:].bitcast(BF16)
                        for kk in range(8):
                            k = half * 8 + kk
                            P.op("pe", lambda e: e.transpose(ptb[:, kk * 128:(kk + 1) * 128], xn[:, k * 128:(k + 1) * 128], identb[:]),
                                 r=[bxn, b_identb], w=[bp])
                        for kk in range(8):
                            k = half * 8 + kk
                            o = XT[:, k, j * 128:(j + 1) * 128]
                            i_ = ptb[:, kk * 128:(kk + 1) * 128]
                            if half == 0:
                                P.op("act", lambda e: e.activation(o, i_, AF.Identity, bias=modF[:, mi + 1, k:k + 1], scale=modF[:, mi, k:k + 1]),
                                     r=[b_modF], w=[bXT, bp])
                            else:
                                P.op("dve", lambda e: e.tensor_scalar(o, i_, modF[:, mi, k:k + 1], modF[:, mi + 1, k:k + 1], op0=ALU.mult, op1=ALU.add),
                                     r=[b_modF], w=[bXT, bp])
                P.dma(dst.rearrange("(k p) n -> p k n", p=128)[:, :, 1 + base:1 + base + NT], XT[:, :, 0:NT], r=[bXT], w=[bd], sb=bXT)
            P.rec = None
            P.zip_emit([lst_b, lst_c])
        barrier()

        with ExitStack() as ph:
            mhalf, b_mhalf = sbt(ph, "mhalf", [128, 16], F32)
            P.op("pool", lambda e: e.memset(mhalf[:], -0.5), w=[b_mhalf])
            rd, b_rd = sbt(ph, "rd", [128, 16], F32)
            lgn, b_lgn = sbt(ph, "lgn", [128, 16], F32)
            tmp16, b_tmp16 = sbt(ph, "tmp16", [128, 16], F32)
            dec, b_dec = sbt(ph, "dec", [128, 2, 3, 8], F32)
            P.dma(rd[:], rdec_d.partition_broadcast(128), w=[b_rd], sb=b_rd)
            P.op("act", lambda e: e.activation(rd[:], rd[:], AF.Exp, scale=-1.0), r=[b_rd], w=[b_rd])
            P.op("dve", lambda e: e.tensor_scalar(lgn[:], rd[:], -1.0 / 8, 1.0 / 7, op0=ALU.mult, op1=ALU.add), r=[b_rd], w=[b_lgn])
            for cf in (6, 5, 4, 3, 2, 1):
                P.op("dve", lambda e: e.tensor_tensor(tmp16[:], lgn[:], rd[:], ALU.mult), r=[b_lgn, b_rd], w=[b_tmp16])
                P.op("dve", lambda e: e.tensor_scalar(lgn[:], tmp16[:], -1.0, 1.0 / cf, op0=ALU.mult, op1=ALU.add), r=[b_tmp16], w=[b_lgn])
            P.op("dve", lambda e: e.tensor_tensor(lgn[:], lgn[:], rd[:], ALU.mult), r=[b_lgn, b_rd], w=[b_lgn])
            for dr in range(2):
                P.op("dve", lambda e: e.tensor_scalar(tmp16[:, 0:8], lgn[:, dr * 8:(dr + 1) * 8], pos[:, dr:dr + 1], None, op0=ALU.mult),
                     r=[b_lgn, b_pos], w=[b_tmp16])
                P.op("act", lambda e: e.activation(dec[:, dr, 0, :], tmp16[:, 0:8], AF.Exp, scale=-1.0), r=[b_tmp16], w=[b_dec])
                P.op("act", lambda e: e.activation(dec[:, dr, 1, :], tmp16[:, 0:8], AF.Exp, bias=-math.log(8.0)), r=[b_tmp16], w=[b_dec])
                P.op("act", lambda e: e.activation(dec[:, dr, 2, :], lgn[:, dr * 8:(dr + 1) * 8], AF.Exp, scale=-128.0), r=[b_lgn], w=[b_dec])
            cw, b_cw = sbt(ph, "cw", [128, 3, 8], F32)
            for j_ in range(3):
                P.dma(cw[:, j_, :], mconv_d[j_:j_ + 1, :].rearrange("r (b p) -> p (r b)", p=128), w=[b_cw], sb=b_cw, allow_slow_non_contiguous=True)
            gb, b_gb = sbt(ph, "gb", [128, 16], F32)
            P.dma(gb[:], gbias_d.partition_broadcast(128), w=[b_gb], sb=b_gb)
            wmgf, b_wmgf = sbt(ph, "wmgf", [128, KC, 16], F32)
            wmg, b_wmg = sbt(ph, "wmg", [128, KC, 16], BF16)
            P.dma(wmgf[:], wmg_d.rearrange("(k p) n -> p k n", p=128), w=[b_wmgf], sb=b_wmgf)
            P.op("dve", lambda e: e.tensor_copy(wmg[:], wmgf[:]), r=[b_wmgf], w=[b_wmg])
            S32, b_S32 = sbt(ph, "S32", [64, 8, 128], F32)
            Sbf, b_Sbf = sbt(ph, "Sbf", [64, 8, 128], BF16)
            C32, b_C32 = sbt(ph, "C32", [128, 4, 257], F32)
            Cbf, b_Cbf = sbt(ph, "Cbf", [128, 4, 257], BF16)
            mprev, b_mprev = sbt(ph, "mprev", [128, 4], F32)
            XTr = Ring(nc, ph, "XT", [128, KC, 514], BF16, 1)
            Wbr = Ring(nc, ph, "Wb", [128, KC, 512], BF16, 2)
            RKr = Ring(nc, ph, "RK", [128, 4, 512], BF16, 1, multi=True)
            RQr = Ring(nc, ph, "RQ", [128, 4, 512], BF16, 1, multi=True)
            RVr = Ring(nc, ph, "RVs", [128, 4, 8, 128], BF16, 1, multi=True)
            MQKr = Ring(nc, ph, "MQK", [128, 8, 512], BF16, 1, multi=True)
            MVr = Ring(nc, ph, "MV", [128, 4, 1024], BF16, 1, multi=True)
            RGr = Ring(nc, ph, "RG", [128, 4, 1024], BF16, 1, multi=True)
            MOr = Ring(nc, ph, "MO", [128, 4, 1024], BF16, 1, multi=True)
            Gr = Ring(nc, ph, "G", [128, 4, 16], F32, 1, multi=True)
            rotr = Ring(nc, ph, "rot", [128, 4, 64], F32, 2)
            rawr = Ring(nc, ph, "raw", [128, 514], F32, 2)
            accr = Ring(nc, ph, "cacc", [128, 512], F32, 2)
            rtr = Ring(nc, ph, "rtmp", [128, 2, 8, 2, 32], F32, 1)
            QTr = Ring(nc, ph, "QT", [64, 8, 128], BF16, 2)
            KTr = Ring(nc, ph, "KT", [64, 8, 128], BF16, 2)
            ATr = Ring(nc, ph, "AT", [128, 8, 128], BF16, 1)
            ATmr = Ring(nc, ph, "ATm", [128, 4, 128], BF16, 2)
            MKtr = Ring(nc, ph, "MKt", [128, 4, 128], BF16, 2)
            Vmr = Ring(nc, ph, "Vm", [128, 4, 257], BF16, 2)
            gsr = Ring(nc, ph, "gs", [128, 16, 4], F32, 2)
            spur = Ring(nc, ph, "spu", [128, 2, 4, 4], F32, 2)
            Dr = Ring(nc, ph, "Dg", [128, 4, 128], F32, 1)
            Umr = Ring(nc, ph, "Um", [128, 4, 128], F32, 1)
            Yr = Ring(nc, ph, "Y", [128, D], F32, 2)
            Ym_bufs = [Buf("Ym0"), Buf("Ym1")]
            YAr = Ring(nc, ph, "YA", [128, D], F32, 1)
            sqr = Ring(nc, ph, "sq", [128, D], F32, 1)
            yor = Ring(nc, ph, "yo", [128, D], BF16, 1)
            nsr = Ring(nc, ph, "ns", [128, 4, 12], F32, 2)
            YOTr = Ring(nc, ph, "YOT", [128, KC, 512], BF16, 1, multi=True)
            dnr = Ring(nc, ph, "dn", [128, 4, 4], F32, 2)

            def proj_tok(XT, bXT, j, Wb, bW, width=512):
                pt, bp = psr.next()
                for k in range(KC):
                    P.op("pe", lambda e: e.matmul(pt[:, 0:width], lhsT=XT[:, k, 1 + j * 128:1 + (j + 1) * 128], rhs=Wb[:, k, 0:width],
                                                  start=(k == 0), stop=(k == KC - 1)), r=[bXT, bW], w=[bp])
                return pt, bp

            def load_w(off):
                Wb, bW = Wbr.next()
                P.dma(Wb[:], winb_d[:, off:off + 512].rearrange("(k p) n -> p k n", p=128), r=[B_winb], w=[bW], sb=bW)
                return Wb, bW

            def scan_pass(dr):
                maskT = tri[dr]
                qdec = dec[:, dr, 0, :]
                kdec = dec[:, dr, 1, :]
                cdec = dec[:, dr, 2, :]
                P.op("pool", lambda e: e.memset(S32[:], 0.0), w=[b_S32])
                P.op("pool", lambda e: e.memset(Sbf[:], 0.0), w=[b_Sbf])
                P.op("pool", lambda e: e.memset(C32[:], 0.0), w=[b_C32])
                P.op("pool", lambda e: e.memset(Cbf[:], 0.0), w=[b_Cbf])
                P.op("pool", lambda e: e.memset(mprev[:], 0.0), w=[b_mprev])
                if dr == 0:
                    units = [(True, 0, CL, False)] + [(False, s * 512, 512, True) for s in range(NST)]
                else:
                    units = [(True, 0, CL, False)] + [(False, s * 512, 512, False) for s in range(2 * NST - 1, NST - 1, -1)] \
                        + [(False, s * 512, 512, True) for s in range(NST - 1, -1, -1)]
                for (is_ctx, base, NT, with_out) in units:
                    nch = NT // 128
                    srcT, bsrc = (cmT_d, B_cmT) if is_ctx else (xmT_d, B_xmT)
                    XT, bXT = XTr.next()
                    P.dma(XT[:, :, 0:NT + 2], srcT.rearrange("(k p) n -> p k n", p=128)[:, :, base:base + NT + 2], r=[bsrc], w=[bXT], sb=bXT)
                    if not is_ctx:
                        rot, brot = rotr.next()
                        P.dma(rot[:, 0:nch, :], rot_d[base:base + NT, :].rearrange("(j p) c -> p j c", p=128), w=[brot], sb=brot)
                    RK, bRK = RKr.next()
                    RQ, bRQ = RQr.next()
                    RVs, bRV = RVr.next()
                    MQK, bMQK = MQKr.next()
                    MV, bMV = MVr.next()
                    G, bG = Gr.next()
                    RG, bRG = RGr.next()
                    MO, bMO = MOr.next()

                    def rotary(pt, bp, dst, bdst, j):
                        if is_ctx:
                            P.op("act", lambda e: e.copy(dst[:, j, :], pt[:, 0:512]), w=[bdst, bp])
                            return
                        tm, btm = rtr.next()
                        pv = pt[:, 0:512].rearrange("p (h t i) -> p h t i", h=8, t=2)
                        cosb = rot[:, j, 0:32].unsqueeze(1).unsqueeze(1).to_broadcast([128, 8, 2, 32])
                        sinb = rot[:, j, 32:64].unsqueeze(1).unsqueeze(1).to_broadcast([128, 8, 2, 32])
                        P.op("dve", lambda e: e.tensor_tensor(tm[:, 0], pv, cosb, ALU.mult), r=[brot], w=[btm, bp])
                        P.op("dve", lambda e: e.tensor_tensor(tm[:, 1], pv, sinb, ALU.mult), r=[brot], w=[btm, bp])
                        dv = dst[:, j, :].rearrange("p (h t i) -> p h t i", h=8, t=2)
                        P.op("pool", lambda e: e.tensor_tensor(dv[:, :, 0, :], tm[:, 0, :, 0, :], tm[:, 1, :, 1, :], ALU.subtract), r=[btm], w=[bdst])
                        P.op("pool", lambda e: e.tensor_tensor(dv[:, :, 1, :], tm[:, 0, :, 1, :], tm[:, 1, :, 0, :], ALU.add), r=[btm], w=[bdst])

                    Wb, bW = load_w(O_RK)
                    for j in range(nch):
                        pt, bp = proj_tok(XT, bXT, j, Wb, bW)
                        rotary(pt, bp, RK, bRK, j)
                    for hb in range(2):
                        Wb, bW = load_w(O_RV + hb * 512)
                        for j in range(nch):
                            pt, bp = proj_tok(XT, bXT, j, Wb, bW)
                            P.op("dve", lambda e: e.tensor_tensor(RVs[:, j, hb * 4:(hb + 1) * 4, :], pt[:, 0:512].rearrange("p (h e) -> p h e", h=4),
                                                                  kdec[:, hb * 4:(hb + 1) * 4].unsqueeze(2).to_broadcast([128, 4, 128]), ALU.mult),
                                 r=[b_dec], w=[bRV, bp])
                    for hb in range(2):
                        Wb, bW = load_w(O_MV + hb * 512)
                        for j in range(nch):
                            pt, bp = proj_tok(XT, bXT, j, Wb, bW)
                            P.op("act", lambda e: e.copy(MV[:, j, hb * 512:(hb + 1) * 512], pt[:, 0:512]), w=[bMV, bp])
                    for j in range(nch):
                        pt, bp = proj_tok(XT, bXT, j, wmg, b_wmg, width=16)
                        P.op("dve", lambda e: e.tensor_tensor(G[:, j, :], pt[:, 0:16], gb[:], ALU.add), r=[b_gb], w=[bG, bp])
                    spu, bspu = spur.next()
                    P.op("act", lambda e: e.activation(spu[:, 1, 0:nch, :], G[:, 0:nch, dr * 8 + 4:dr * 8 + 8], AF.Exp, scale=-1.0), r=[bG], w=[bspu])
                    P.op("act", lambda e: e.activation(spu[:, 0, 0:nch, :], spu[:, 1, 0:nch, :], AF.Ln, bias=1.0), w=[bspu])
                    fm_blocks = [(O_MK, 4)] + ([(O_MQ, 0)] if with_out else [])
                    nh = (NT + 2) // 2
                    for (off, b0) in fm_blocks:
                        Wb, bW = load_w(off)
                        for hb in range(4):
                            raw, braw = rawr.next()
                            for half in range(2):
                                pt, bp = psr.next()
                                for k in range(KC):
                                    P.op("pe", lambda e: e.matmul(pt[:, 0:nh], lhsT=Wb[:, k, hb * 128:(hb + 1) * 128], rhs=XT[:, k, half * nh:(half + 1) * nh],
                                                                  start=(k == 0), stop=(k == KC - 1)), r=[bXT, bW], w=[bp])
                                if half == 0:
                                    P.op("act", lambda e: e.copy(raw[:, 0:nh], pt[:, 0:nh]), w=[braw, bp])
                                else:
                                    P.op("dve", lambda e: e.tensor_copy(raw[:, nh:2 * nh], pt[:, 0:nh]), w=[braw, bp])
                            b = b0 + hb
                            ac, bac = accr.next()
                            P.op("act", lambda e: e.activation(ac[:, 0:NT], raw[:, 1:NT + 1], AF.Identity, scale=cw[:, 1, b:b + 1]), r=[braw, b_cw], w=[bac])
                            P.op("dve", lambda e: e.scalar_tensor_tensor(out=ac[:, 0:NT], in0=raw[:, 0:NT], scalar=cw[:, 0, b:b + 1], in1=ac[:, 0:NT],
                                                                         op0=ALU.mult, op1=ALU.add), r=[braw, b_cw], w=[bac])
                            P.op("dve", lambda e: e.scalar_tensor_tensor(out=ac[:, 0:NT], in0=raw[:, 2:NT + 2], scalar=cw[:, 2, b:b + 1], in1=ac[:, 0:NT],
                                                                         op0=ALU.mult, op1=ALU.add), r=[braw, b_cw], w=[bac])
                            P.op("act", lambda e: e.activation(MQK[:, b, 0:NT], ac[:, 0:NT], AF.Silu), r=[bac], w=[bMQK])
                    if with_out:
                        Wb, bW = load_w(O_RQ)
                        for j in range(nch):
                            pt, bp = proj_tok(XT, bXT, j, Wb, bW)
                            rotary(pt, bp, RQ, bRQ, j)
                        if dr == 1:
                            for (off, dstt, bdd, fn) in ((O_RG, RG, bRG, AF.Silu), (O_MO, MO, bMO, AF.Sigmoid)):
                                for hb in range(2):
                                    Wb, bW = load_w(off + hb * 512)
                                    for j in range(nch):
                                        pt, bp = proj_tok(XT, bXT, j, Wb, bW)
                                        P.op("act", lambda e: e.activation(dstt[:, j, hb * 512:(hb + 1) * 512], pt[:, 0:512], fn), w=[bdd, bp])
                        YOT, bYOT = YOTr.next()
                    order = range(nch) if dr == 0 else range(nch - 1, -1, -1)
                    pend_out = []
                    psM, psR, psO = RingView(psr.items[0:4]), RingView(psr.items[4:7]), RingView(psr.items[7:8])
                    psx = [psM]
                    for j in order:
                        tok0 = base + j * 128
                        if with_out:
                            Y, bY = Yr.next()
                            bYm = Ym_bufs[(Yr.i - 1) % len(Yr.items)]
                        lst_R, lst_M, lst_O = [], [], []
                        P.rec = lst_R
                        psx[0] = psR
                        if with_out:
                            QT, bQT = QTr.next()
                            KT, bKT = KTr.next()
                            for (srcq, bsq, dT, bdT, eng) in ((RQ, bRQ, QT, bQT, "act"), (RK, bRK, KT, bKT, "dve")):
                                pt, bp = psx[0].next()
                                ptb = pt[:].bitcast(BF16)
                                for h in range(8):
                                    P.op("pe", lambda e: e.transpose(ptb[0:64, h * 128:(h + 1) * 128], srcq[:, j, h * 64:(h + 1) * 64], identb[:]),
                                         r=[bsq, b_identb], w=[bp])
                                if eng == "act":
                                    P.op("act", lambda e: e.copy(dT[:].rearrange("p h c -> p (h c)"), ptb[0:64, :]), w=[bdT, bp])
                                else:
                                    P.op("dve", lambda e: e.tensor_copy(dT[:].rearrange("p h c -> p (h c)"), ptb[0:64, :]), w=[bdT, bp])
                            AT, bAT = ATr.next()
                            for hb in range(2):
                                pa, bpa = psx[0].next()
                                for hh in range(4):
                                    h = hb * 4 + hh
                                    P.op("pe", lambda e: e.matmul(pa[:, hh * 128:(hh + 1) * 128], lhsT=KT[:, h, :], rhs=QT[:, h, :], start=True, stop=True),
                                         r=[bKT, bQT], w=[bpa])
                                P.op("dve", lambda e: e.tensor_tensor(AT[:, hb * 4:(hb + 1) * 4, :], pa[:, 0:512].rearrange("p (h c) -> p h c", h=4),
                                                                      maskT.unsqueeze(1).to_broadcast([128, 4, 128]), ALU.mult), r=[b_cst], w=[bAT, bpa])
                            for hb in range(2):
                                py, bpy = psx[0].next()
                                for hh in range(4):
                                    h = hb * 4 + hh
                                    P.op("pe", lambda e: e.matmul(py[:, hh * 128:(hh + 1) * 128], lhsT=AT[:, h, :], rhs=RVs[:, j, h, :], start=True, stop=False),
                                         r=[bAT, bRV], w=[bpy])
                                    P.op("pe", lambda e: e.matmul(py[:, hh * 128:(hh + 1) * 128], lhsT=QT[:, h, :], rhs=Sbf[:, h, :], start=False, stop=True),
                                         r=[bQT, b_Sbf], w=[bpy])
                                P.op("dve", lambda e: e.tensor_tensor(Y[:, hb * 512:(hb + 1) * 512].rearrange("p (h e) -> p h e", h=4),
                                                                      py[:, 0:512].rearrange("p (h e) -> p h e", h=4),
                                                                      qdec[:, hb * 4:(hb + 1) * 4].unsqueeze(2).to_broadcast([128, 4, 128]), ALU.mult),
                                     r=[b_dec], w=[bY, bpy])
                        for hb in range(2):
                            pS, bpS = psx[0].next()
                            for hh in range(4):
                                h = hb * 4 + hh
                                P.op("pe", lambda e: e.matmul(pS[0:64, hh * 128:(hh + 1) * 128], lhsT=RK[:, j, h * 64:(h + 1) * 64], rhs=RVs[:, j, h, :],
                                                              start=True, stop=True), r=[bRK, bRV], w=[bpS])
                            sv = S32[:, hb * 4:(hb + 1) * 4, :]
                            P.op("dve", lambda e: e.tensor_tensor(sv, sv, pS[0:64, 0:512].rearrange("p (h e) -> p h e", h=4), ALU.add), w=[b_S32, bpS])
                            P.op("pool", lambda e: e.tensor_tensor(sv, sv, cdec[0:64, hb * 4:(hb + 1) * 4].unsqueeze(2).to_broadcast([64, 4, 128]), ALU.mult),
                                 r=[b_dec], w=[b_S32])
                            P.op("act", lambda e: e.copy(Sbf[:, hb * 4:(hb + 1) * 4, :], sv), r=[b_S32], w=[b_Sbf])
                        P.rec = lst_M
                        psx[0] = psM
                        gs, bgs = gsr.next()
                        zi = G[:, j, dr * 8:dr * 8 + 4]
                        zf = G[:, j, dr * 8 + 4:dr * 8 + 8]
                        pg, bpg = psx[0].next()
                        P.op("pe", lambda e: e.matmul(pg[:, 0:4], lhsT=tri[dr], rhs=spu[:, 0, j, :], start=True, stop=True), r=[bspu, b_cst], w=[bpg])
                        P.op("pe", lambda e: e.matmul(pg[:, 4:8], lhsT=onesf, rhs=spu[:, 0, j, :], start=True, stop=True), r=[bspu, b_cst], w=[bpg])
                        cs = pg[:, 0:4]
                        tot = pg[:, 4:8]
                        P.op("dve", lambda e: e.tensor_tensor(gs[:, 2, :], zi, cs, ALU.add), r=[bG], w=[bgs, bpg])
                        Dg, bDg = Dr.next()
                        P.op("dve", lambda e: e.tensor_tensor(Dg[:], identf.unsqueeze(1).to_broadcast([128, 4, 128]),
                                                              gs[:, 2, :].unsqueeze(2).to_broadcast([128, 4, 128]), ALU.mult), r=[b_cst, bgs], w=[bDg])
                        pu, bpu = psx[0].next()
                        P.op("pe", lambda e: e.matmul(pu[:, 0:512], lhsT=onesf, rhs=Dg[:].rearrange("p h s -> p (h s)"), start=True, stop=True),
                             r=[bDg, b_cst], w=[bpu])
                        puv = pu[:, 0:512].rearrange("p (h s) -> p h s", h=4)
                        P.op("dve", lambda e: e.tensor_reduce(out=gs[:, 5, :], in_=puv, axis=AX.X, op=ALU.max), w=[bgs, bpu])
                        Um, bUm = Umr.next()
                        P.op("dve", lambda e: e.tensor_tensor(Um[:], puv, negm[dr].unsqueeze(1).to_broadcast([128, 4, 128]), ALU.add), r=[b_cst], w=[bUm, bpu])
                        P.op("dve", lambda e: e.tensor_reduce(out=gs[:, 6, :], in_=Um[:], axis=AX.X, op=ALU.max), r=[bUm], w=[bgs])
                        P.op("dve", lambda e: e.tensor_tensor(gs[:, 3, :], gs[:, 6, :], mprev[:], ALU.max), r=[b_mprev], w=[bgs])
                        P.op("dve", lambda e: e.tensor_tensor(gs[:, 4, :], gs[:, 5, :], mprev[:], ALU.max), r=[b_mprev], w=[bgs])
                        P.op("dve", lambda e: e.tensor_tensor(gs[:, 11, :], gs[:, 2, :], gs[:, 4, :], ALU.subtract), w=[bgs])
                        P.op("act", lambda e: e.activation(gs[:, 7, :], gs[:, 11, :], AF.Exp, bias=-0.5 * math.log(128.0)), w=[bgs])
                        P.op("dve", lambda e: e.tensor_tensor(gs[:, 12, :], gs[:, 4, :], gs[:, 3, :], ALU.subtract), w=[bgs])
                        P.op("act", lambda e: e.activation(gs[:, 8, :], gs[:, 12, :], AF.Exp), w=[bgs])
                        P.op("dve", lambda e: e.tensor_tensor(gs[:, 13, :], mprev[:], gs[:, 4, :], ALU.subtract), r=[b_mprev], w=[bgs])
                        P.op("act", lambda e: e.activation(gs[:, 9, :], gs[:, 13, :], AF.Exp), w=[bgs])
                        P.op("dve", lambda e: e.tensor_tensor(gs[:, 14, :], cs, gs[:, 3, :], ALU.subtract), w=[bgs, bpg])
                        P.op("act", lambda e: e.activation(gs[:, 10, :], gs[:, 14, :], AF.Exp), w=[bgs])
                        P.op("dve", lambda e: e.tensor_tensor(mprev[:], gs[:, 4, :], tot, ALU.subtract), r=[bgs], w=[b_mprev, bpg])
                        Vm, bVm = Vmr.next()
                        P.op("dve", lambda e: e.tensor_tensor(Vm[:, :, 0:256], MV[:, j, :].rearrange("p (h e) -> p h e", h=4),
                                                              gs[:, 7, :].unsqueeze(2).to_broadcast([128, 4, 256]), ALU.mult), r=[bMV, bgs], w=[bVm])
                        P.op("pool", lambda e: e.tensor_copy(Vm[:, :, 256:257], gs[:, 7, :].unsqueeze(2)), r=[bgs], w=[bVm])
                        P.op("pool", lambda e: e.tensor_tensor(C32[:], C32[:], gs[:, 9, :].unsqueeze(2).to_broadcast([128, 4, 257]), ALU.mult), r=[bgs], w=[b_C32])
                        P.op("act", lambda e: e.copy(Cbf[:], C32[:]), r=[b_C32], w=[b_Cbf])
                        MKt, bMKt = MKtr.next()
                        pt, bp = psx[0].next()
                        ptb = pt[:].bitcast(BF16)
                        for h in range(4):
                            P.op("pe", lambda e: e.transpose(ptb[:, h * 128:(h + 1) * 128], MQK[:, 4 + h, j * 128:(j + 1) * 128], identb[:]),
                                 r=[bMQK, b_identb], w=[bp])
                        P.op("act", lambda e: e.copy(MKt[:].rearrange("p h d -> p (h d)"), ptb[:, 0:512]), w=[bMKt, bp])
                        if with_out:
                            ATm, bATm = ATmr.next()
                            pa, bpa = psx[0].next()
                            for h in range(4):
                                P.op("pe", lambda e: e.matmul(pa[:, h * 128:(h + 1) * 128], lhsT=MQK[:, 4 + h, j * 128:(j + 1) * 128],
                                                              rhs=MQK[:, h, j * 128:(j + 1) * 128], start=True, stop=True), r=[bMQK], w=[bpa])
                            P.op("dve", lambda e: e.tensor_tensor(ATm[:], pa[:, 0:512].rearrange("p (h c) -> p h c", h=4),
                                                                  maskT.unsqueeze(1).to_broadcast([128, 4, 128]), ALU.mult), r=[b_cst], w=[bATm, bpa])
                            dn, bdn = dnr.next()
                            for h in range(4):
                                ph_, bph = psx[0].next()
                                P.op("pe", lambda e: e.matmul(ph_[:, 0:257], lhsT=ATm[:, h, :], rhs=Vm[:, h, :], start=True, stop=False), r=[bATm, bVm], w=[bph])
                                P.op("pe", lambda e: e.matmul(ph_[:, 0:257], lhsT=MQK[:, h, j * 128:(j + 1) * 128], rhs=Cbf[:, h, :], start=False, stop=True),
                                     r=[bMQK, b_Cbf], w=[bph])
                                P.op("act", lambda e: e.activation(dn[:, h, 0:1], ph_[:, 256:257], AF.Abs), w=[bdn, bph])
                                P.op("dve", lambda e: e.tensor_scalar(dn[:, h, 1:2], dn[:, h, 0:1], gs[:, 8, h:h + 1], gs[:, 10, h:h + 1], op0=ALU.mult, op1=ALU.max),
                                     r=[bgs], w=[bdn])
                                P.op("dve", lambda e: e.reciprocal(dn[:, h, 2:3], dn[:, h, 1:2]), w=[bdn])
                                P.op("dve", lambda e: e.tensor_tensor(dn[:, h, 3:4], dn[:, h, 2:3], gs[:, 8, h:h + 1], ALU.mult), r=[bgs], w=[bdn])
                                P.op("act", lambda e: e.activation(Y[:, 1024 + h * 256:1024 + (h + 1) * 256], ph_[:, 0:256], AF.Identity, scale=dn[:, h, 3:4]),
                                     r=[bdn], w=[bYm, bph])
                        for h in range(4):
                            pc, bpc = psx[0].next()
                            P.op("pe", lambda e: e.matmul(pc[:, 0:257], lhsT=MKt[:, h, :], rhs=Vm[:, h, :], start=True, stop=True), r=[bMKt, bVm], w=[bpc])
                            P.op("dve", lambda e: e.tensor_tensor(C32[:, h, :], C32[:, h, :], pc[:, 0:257], ALU.add), w=[b_C32, bpc])
                        P.rec = lst_O
                        psx[0] = psO
                        if with_out and dr == 0:
                            P.dma(yA_d[tok0:tok0 + 128, :], Y[:], r=[bY, bYm], w=[B_yA], sb=bY)
                        if with_out and dr == 1:
                            YA, bYA = YAr.next()
                            P.dma(YA[:], yA_d[tok0:tok0 + 128, :], r=[B_yA], w=[bYA], sb=bYA)
                            P.op("dve", lambda e: e.tensor_tensor(Y[:], Y[:], YA[:], ALU.add), r=[bYA], w=[bY, bYm])
                            ns, bns = nsr.next()
                            hn, bhn = Y, bY
                            sq, bsq = sqr.next()
                            yo, byo = yor.next()
                            for (gi, c0, ng, gw) in ((0, 0, 8, 128), (1, 1024, 4, 256)):
                                yv = Y[:, c0:c0 + 1024].rearrange("p (g e) -> p g e", g=ng)
                                hv = hn[:, c0:c0 + 1024].rearrange("p (g e) -> p g e", g=ng)
                                sv2 = sq[:, c0:c0 + 1024].rearrange("p (g e) -> p g e", g=ng)
                                st_ = ns[:, gi * 2:gi * 2 + 2, :]
                                P.op("dve", lambda e: e.tensor_reduce(out=st_[:, 0, 0:ng], in_=yv, axis=AX.X, op=ALU.add), r=[bY], w=[bns])
                                P.op("dve", lambda e: e.tensor_scalar(st_[:, 0, 0:ng], st_[:, 0, 0:ng], 1.0 / gw, None, op0=ALU.mult), w=[bns])
                                P.op("dve", lambda e: e.tensor_tensor(hv, yv, st_[:, 0, 0:ng].unsqueeze(2).to_broadcast([128, ng, gw]), ALU.subtract),
                                     r=[bY, bns], w=[bhn])
                                P.op("pool", lambda e: e.tensor_tensor(sv2, hv, hv, ALU.mult), r=[bhn], w=[bsq])
                                P.op("dve", lambda e: e.tensor_reduce(out=st_[:, 1, 0:ng], in_=sv2, axis=AX.X, op=ALU.add), r=[bsq], w=[bns])
                                P.op("dve", lambda e: e.tensor_scalar(st_[:, 1, 0:ng], st_[:, 1, 0:ng], 1.0 / gw, EPS, op0=ALU.mult, op1=ALU.add), w=[bns])
                                P.op("pool", lambda e: e.tensor_tensor(st_[:, 1, 0:ng], st_[:, 1, 0:ng], mhalf[:, 0:ng], ALU.pow), r=[b_mhalf], w=[bns])
                                P.op("dve", lambda e: e.tensor_tensor(hv, hv, st_[:, 1, 0:ng].unsqueeze(2).to_broadcast([128, ng, gw]), ALU.mult),
                                     r=[bns], w=[bhn])
                                gate, bgate = (RG, bRG) if gi == 0 else (MO, bMO)
                                P.op("pool", lambda e: e.tensor_tensor(yo[:, c0:c0 + 1024], hn[:, c0:c0 + 1024], gate[:, j, :], ALU.mult),
                                     r=[bhn, bgate, bYm], w=[byo])
                            for half in range(2):
                                pt, bp = psx[0].next()
                                ptb = pt[:].bitcast(BF16)
                                for kk in range(8):
                                    k = half * 8 + kk
                                    P.op("pe", lambda e: e.transpose(ptb[:, kk * 128:(kk + 1) * 128], yo[:, k * 128:(k + 1) * 128], identb[:]),
                                         r=[byo, b_identb], w=[bp])
                                ov = YOT[:, half * 8:(half + 1) * 8, j * 128:(j + 1) * 128]
                                iv = ptb[:, 0:1024].rearrange("p (k t) -> p k t", k=8)
                                if half == 0:
                                    P.op("act", lambda e: e.copy(ov, iv), w=[bYOT, bp])
                                else:
                                    P.op("dve", lambda e: e.tensor_copy(ov, iv), w=[bYOT, bp])
                        P.rec = None
                        psx[0] = psM
                        P.zip_emit([lst_M, lst_R, pend_out])
                        pend_out = lst_O
                    P.zip_emit([pend_out])
                    if with_out and dr == 1:
                        P.dma(yoT_d.rearrange("(k p) n -> p k n", p=128)[:, :, base:base + NT], YOT[:, :, 0:NT], r=[bYOT], w=[B_yoT], sb=bYOT)

            scan_pass(0)
            scan_pass(1)
        barrier()

        with ExitStack() as ph:
            gt1b, b_gt1b = sbt(ph, "gt1b", [128, D], F32)
            P.dma(gt1b[:], ada_d[0:1, 2 * D:3 * D].partition_broadcast(128), r=[B_ada], w=[b_gt1b], sb=b_gt1b)
            XTr = Ring(nc, ph, "cXT", [128, KC, 512], BF16, 1)
            YTr = Ring(nc, ph, "cYT", [128, KC, 512], BF16, 1)
            MTr = Ring(nc, ph, "cMT", [128, KC, 512], BF16, 1, multi=True)
            Wr = Ring(nc, ph, "cW", [128, KC, 512], BF16, 3)
            sgr = Ring(nc, ph, "sg", [128, 512], F32, 2)
            t1r = Ring(nc, ph, "t1", [128, 512], F32, 2)
            t2r = Ring(nc, ph, "t2", [128, 512], F32, 2)
            xr = Ring(nc, ph, "cx", [128, D], F32, 4, multi=True)
            for s in range(NST):
                base = s * 512
                XT, bXT = XTr.next()
                YT, bYT = YTr.next()
                MT, bMT = MTr.next()
                P.dma(XT[:], xmT_d.rearrange("(k p) n -> p k n", p=128)[:, :, 1 + base:1 + base + 512], r=[B_xmT], w=[bXT], sb=bXT)
                P.dma(YT[:], yoT_d.rearrange("(k p) n -> p k n", p=128)[:, :, base:base + 512], r=[B_yoT], w=[bYT], sb=bYT)
                xs = []
                for j in range(4):
                    xt, bx = xr.next()
                    P.dma(xt[:], x_d[base + j * 128:base + (j + 1) * 128, :], w=[bx], sb=bx)
                    xs.append((xt, bx))
                for cb in range(4):
                    Wgr, bWgr = Wr.next()
                    P.dma(Wgr[:], winb_d[:, O_GR + cb * 512:O_GR + (cb + 1) * 512].rearrange("(k p) n -> p k n", p=128), r=[B_winb], w=[bWgr], sb=bWgr)
                    Wgm, bWgm = Wr.next()
                    P.dma(Wgm[:], winb_d[:, O_GM + cb * 512:O_GM + (cb + 1) * 512].rearrange("(k p) n -> p k n", p=128), r=[B_winb], w=[bWgm], sb=bWgm)
                    Wo, bWo = Wr.next()
                    P.dma(Wo[:, 0:8, :], wrob_d[:, cb * 512:(cb + 1) * 512].rearrange("(k p) n -> p k n", p=128), r=[B_wrob], w=[bWo], sb=bWo)
                    P.dma(Wo[:, 8:16, :], wmob_d[:, cb * 512:(cb + 1) * 512].rearrange("(k p) n -> p k n", p=128), r=[B_wmob], w=[bWo], sb=bWo)
                    for m in range(4):
                        db = cb * 4 + m
                        res = []
                        for (Wg, bWg, k0, tr_) in ((Wgr, bWgr, 0, t1r), (Wgm, bWgm, 8, t2r)):
                            pg_, bpg_ = psr.next()
                            for k in range(KC):
                                P.op("pe", lambda e: e.matmul(pg_[:, 0:512], lhsT=Wg[:, k, m * 128:(m + 1) * 128], rhs=XT[:, k, :],
                                                              start=(k == 0), stop=(k == KC - 1)), r=[bWg, bXT], w=[bpg_])
                            sg, bsg = sgr.next()
                            P.op("act", lambda e: e.activation(sg[:], pg_[:, 0:512], AF.Sigmoid), w=[bsg, bpg_])
                            pp, bpp = psr.next()
                            for k in range(8):
                                P.op("pe", lambda e: e.matmul(pp[:, 0:512], lhsT=Wo[:, k0 + k, m * 128:(m + 1) * 128], rhs=YT[:, k0 + k, :],
                                                              start=(k == 0), stop=(k == 7)), r=[bWo, bYT], w=[bpp])
                            tt, btt = tr_.next()
                            P.op("dve", lambda e: e.tensor_tensor(tt[:], pp[:, 0:512], sg[:], ALU.mult), r=[bsg], w=[btt, bpp])
                            res.append((tt, btt))
                        P.op("pool", lambda e: e.tensor_tensor(MT[:, db, :], res[0][0][:], res[1][0][:], ALU.add), r=[res[0][1], res[1][1]], w=[bMT])
                for cb in range(4):
                    Wo, bWo = Wr.next()
                    P.dma(Wo[:], woutb_d[:, cb * 512:(cb + 1) * 512].rearrange("(k p) n -> p k n", p=128), r=[B_woutb], w=[bWo], sb=bWo)
                    for j in range(4):
                        po, bpo = psr.next()
                        for k in range(KC):
                            P.op("pe", lambda e: e.matmul(po[:, 0:512], lhsT=MT[:, k, j * 128:(j + 1) * 128], rhs=Wo[:, k, :],
                                                          start=(k == 0), stop=(k == KC - 1)), r=[bMT, bWo], w=[bpo])
                        tt, btt = t1r.next()
                        P.op("dve", lambda e: e.tensor_tensor(tt[:], po[:, 0:512], gt1b[:, cb * 512:(cb + 1) * 512], ALU.mult), r=[b_gt1b], w=[btt, bpo])
                        xt, bx = xs[j]
                        P.op("pool", lambda e: e.tensor_tensor(xt[:, cb * 512:(cb + 1) * 512], xt[:, cb * 512:(cb + 1) * 512], tt[:], ALU.add), r=[btt], w=[bx])
                for j in range(4):
                    xt, bx = xs[j]
                    P.dma(x1_d[base + j * 128:base + (j + 1) * 128, :], xt[:], r=[bx], w=[B_x1], sb=bx)
        barrier()

        with ExitStack() as ph:
            Wq, b_Wq = sbt(ph, "Wq", [128, KC, D], BF16)
            for cb in range(4):
                P.dma(Wq[:, :, cb * 512:(cb + 1) * 512], pqb_d[:, cb * 512:(cb + 1) * 512].rearrange("(k p) n -> p k n", p=128), r=[B_pqb], w=[b_Wq], sb=b_Wq)
            keysT, b_keysT = sbt(ph, "keysT", [128, 16, 128], BF16)
            with ExitStack() as ph2:
                kf, b_kf = sbt(ph2, "kf", [128, 16, 128], F32)
                P.dma(kf[:], pk_d.rearrange("(g k) d -> k g d", k=128), w=[b_kf], sb=b_kf)
                for g4 in range(4):
                    pt, bp = psr.next()
                    for gg in range(4):
                        g = g4 * 4 + gg
                        P.op("pe", lambda e: e.transpose(pt[:, gg * 128:(gg + 1) * 128], kf[:, g, :], identf), r=[b_kf, b_cst], w=[bp])
                    P.op("dve", lambda e: e.tensor_copy(keysT[:, g4 * 4:(g4 + 1) * 4, :], pt[:, 0:512].rearrange("p (g k) -> p g k", g=4)), w=[b_keysT, bp])
            barrier()
            gt2b, b_gt2b = sbt(ph, "gt2b", [128, D], F32)
            fgb, b_fgb = sbt(ph, "fgb", [128, D], F32)
            P.dma(gt2b[:], ada_d[0:1, 5 * D:6 * D].partition_broadcast(128), r=[B_ada], w=[b_gt2b], sb=b_gt2b)
            P.dma(fgb[:], fg_d.partition_broadcast(128), w=[b_fgb], sb=b_fgb)
            iota16, b_iota = sbt(ph, "iota16", [128, 16], F32)
            for i in range(16):
                P.op("dve", lambda e: e.memset(iota16[:, i:i + 1], float(i)), w=[b_iota])
            x1r = Ring(nc, ph, "px1", [128, D], F32, 1)
            hnbr = Ring(nc, ph, "phn", [128, D], BF16, 1)
            hbr = Ring(nc, ph, "phb", [128, D], BF16, 2, multi=True)
            hTr = Ring(nc, ph, "phT", [128, KC, 128], BF16, 1, multi=True)
            qTr = Ring(nc, ph, "pqT", [128, 16, 128], BF16, 1, multi=True)
            Scr = Ring(nc, ph, "pSc", [128, 16, 128], F32, 1, multi=True)
            wkr = Ring(nc, ph, "pwk", [128, 16, 128], F32, 1)
            mxr = Ring(nc, ph, "pmx", [128, 16, 16], F32, 1)
            mir = Ring(nc, ph, "pmi", [128, 16, 16], U32, 1)
            mifr = Ring(nc, ph, "pmif", [128, 16, 16], F32, 1)
            bsr = Ring(nc, ph, "pbs", [128, 8, 16], F32, 1)
            bjr = Ring(nc, ph, "pbj", [128, 8, 16], U32, 1)
            ijr = Ring(nc, ph, "pij", [128, 4, 8, 16], F32, 1)
            iju = Ring(nc, ph, "piju", [128, 2, 8, 16], U32, 1)
            eqr = Ring(nc, ph, "peq", [128, 4, 16, 16], F32, 1)
            eidr = Ring(nc, ph, "peid", [128, 128], I32, 2)
            gwr = Ring(nc, ph, "pgw", [128, 2, 128], F32, 2)
            avr = Ring(nc, ph, "pav", [128, 3, 128], F32, 2, multi=True)
            Gr_ = Ring(nc, ph, "pG", [128, 2 * D], BF16, 6)
            jkr = Ring(nc, ph, "pjk", [128, D], BF16, 1)
            dgr = Ring(nc, ph, "pdg", [128, 128], BF16, 4)
            str_ = Ring(nc, ph, "pst", [128, 4], F32, 4)
            pacc = [psr.items[4 + i] for i in range(4)]
            psr.items = psr.items[0:4]
            psr.i = 0

            def rms_rstd(src, bsrc):
                st, bs = str_.next()
                jk, bjk = jkr.next()
                P.op("dve", lambda e: e.scalar_tensor_tensor(out=jk[:], in0=src[:], scalar=1.0, in1=src[:], op0=ALU.mult, op1=ALU.mult, accum_out=st[:, 0:1]),
                     r=[bsrc], w=[bjk, bs])
                P.op("dve", lambda e: e.tensor_scalar(st[:, 1:2], st[:, 0:1], 1.0 / D, EPS, op0=ALU.mult, op1=ALU.add), w=[bs])
                P.op("act", lambda e: e.activation(st[:, 2:3], st[:, 1:2], AF.Sqrt), w=[bs])
                P.op("dve", lambda e: e.reciprocal(st[:, 3:4], st[:, 2:3]), w=[bs])
                return st, bs

            def stage1(blk, res):
                x1, bx1 = x1r.next()
                P.dma(x1[:], x1_d[blk * 128:(blk + 1) * 128, :], r=[B_x1], w=[bx1], sb=bx1)
                st, bs = rms_rstd(x1, bx1)
                yield
                hnb, bhnb = hnbr.next()
                P.op("act", lambda e: e.activation(hnb[:], x1[:], AF.Identity, scale=st[:, 3:4]), r=[bx1, bs], w=[bhnb])
                yield
                hT, bhT = hTr.next()
                for half in range(2):
                    pt, bp = psr.next()
                    ptb = pt[:].bitcast(BF16)
                    for kk in range(8):
                        k = half * 8 + kk
                        P.op("pe", lambda e: e.transpose(ptb[:, kk * 128:(kk + 1) * 128], hnb[:, k * 128:(k + 1) * 128], identb[:]), r=[bhnb, b_identb], w=[bp])
                    for kk in range(8):
                        k = half * 8 + kk
                        if half == 0:
                            P.op("act", lambda e: e.activation(hT[:, k, :], ptb[:, kk * 128:(kk + 1) * 128], AF.Identity, bias=modF[:, 5, k:k + 1], scale=modF[:, 4, k:k + 1]),
                                 r=[b_modF], w=[bhT, bp])
                        else:
                            P.op("dve", lambda e: e.tensor_scalar(hT[:, k, :], ptb[:, kk * 128:(kk + 1) * 128], modF[:, 4, k:k + 1], modF[:, 5, k:k + 1], op0=ALU.mult, op1=ALU.add),
                                 r=[b_modF], w=[bhT, bp])
                    yield
                hb, bhb = hbr.next()
                for half in range(2):
                    pt, bp = psr.next()
                    ptb = pt[:].bitcast(BF16)
                    for kk in range(8):
                        k = half * 8 + kk
                        P.op("pe", lambda e: e.transpose(ptb[:, kk * 128:(kk + 1) * 128], hT[:, k, :], identb[:]), r=[bhT, b_identb], w=[bp])
                    P.op("act", lambda e: e.copy(hb[:, half * 1024:(half + 1) * 1024], ptb[:, 0:1024]), w=[bhb, bp])
                    yield
                qT, bqT = qTr.next()
                for g4 in range(4):
                    pt, bp = psr.next()
                    for gg in range(4):
                        g = g4 * 4 + gg
                        for k in range(KC):
                            P.op("pe", lambda e: e.matmul(pt[:, gg * 128:(gg + 1) * 128], lhsT=Wq[:, k, g * 128:(g + 1) * 128], rhs=hT[:, k, :],
                                                          start=(k == 0), stop=(k == KC - 1)), r=[b_Wq, bhT], w=[bp])
                    P.op("act", lambda e: e.copy(qT[:, g4 * 4:(g4 + 1) * 4, :], pt[:, 0:512].rearrange("p (g t) -> p g t", g=4)), w=[bqT, bp])
                    yield
                Sc, bSc = Scr.next()
                for g4 in range(4):
                    pt, bp = psr.next()
                    for gg in range(4):
                        g = g4 * 4 + gg
                        P.op("pe", lambda e: e.matmul(pt[:, gg * 128:(gg + 1) * 128], lhsT=qT[:, g, :], rhs=keysT[:, g, :], start=True, stop=True),
                             r=[bqT, b_keysT], w=[bp])
                    P.op("act", lambda e: e.copy(Sc[:, g4 * 4:(g4 + 1) * 4, :], pt[:, 0:512].rearrange("p (g t) -> p g t", g=4)), w=[bSc, bp])
                    yield
                mx, bmx = mxr.next()
                mi, bmi = mir.next()
                wk, bwk = wkr.next()
                for g in range(16):
                    P.op("dve", lambda e: e.max(out=mx[:, g, 0:8], in_=Sc[:, g, :]), r=[bSc], w=[bmx])
                    yield
                    P.op("dve", lambda e: e.max_index(out=mi[:, g, 0:8], in_max=mx[:, g, 0:8], in_values=Sc[:, g, :]), r=[bSc, bmx], w=[bmi])
                    yield
                    P.op("dve", lambda e: e.match_replace(out=wk[:, g, :], in_to_replace=mx[:, g, 0:8], in_values=Sc[:, g, :], imm_value=NEG), r=[bSc, bmx], w=[bwk])
                    yield
                    P.op("dve", lambda e: e.max(out=mx[:, g, 8:16], in_=wk[:, g, :]), r=[bwk], w=[bmx])
                    yield
                    P.op("dve", lambda e: e.max_index(out=mi[:, g, 8:16], in_max=mx[:, g, 8:16], in_values=wk[:, g, :]), r=[bwk, bmx], w=[bmi])
                    yield
                    yield
                mif, bmif = mifr.next()
                P.op("dve", lambda e: e.tensor_copy(mif[:], mi[:]), r=[bmi], w=[bmif])
                cand_t, bcand = wkr.next()
                cand = cand_t[:].rearrange("p (h a) k -> p h (a k)", a=2)
                mxv = mx[:].rearrange("p (h t) i -> p h t i", t=2)
                P.op("dve", lambda e: e.tensor_tensor(cand.rearrange("p h (i j) -> p h i j", i=16),
                                                      mxv[:, :, 0, :].unsqueeze(3).to_broadcast([128, 8, 16, 16]),
                                                      mxv[:, :, 1, :].unsqueeze(2).to_broadcast([128, 8, 16, 16]), ALU.add), r=[bmx], w=[bcand])
                yield
                bs_, bbs = bsr.next()
                bj, bbj = bjr.next()
                cw2_t, bcw2 = Scr.next()
                cw2 = cw2_t[:].rearrange("p (h a) k -> p h (a k)", a=2)
                for h in range(8):
                    P.op("dve", lambda e: e.max(out=bs_[:, h, 0:8], in_=cand[:, h, :]), r=[bcand], w=[bbs])
                    yield
                    P.op("dve", lambda e: e.max_index(out=bj[:, h, 0:8], in_max=bs_[:, h, 0:8], in_values=cand[:, h, :]), r=[bcand, bbs], w=[bbj])
                    yield
                    P.op("dve", lambda e: e.match_replace(out=cw2[:, h, :], in_to_replace=bs_[:, h, 0:8], in_values=cand[:, h, :], imm_value=NEG), r=[bcand, bbs], w=[bcw2])
                    yield
                    P.op("dve", lambda e: e.max(out=bs_[:, h, 8:16], in_=cw2[:, h, :]), r=[bcw2], w=[bbs])
                    yield
                    P.op("dve", lambda e: e.max_index(out=bj[:, h, 8:16], in_max=bs_[:, h, 8:16], in_values=cw2[:, h, :]), r=[bcw2, bbs], w=[bbj])
                    yield
                    yield
                ju, bju = iju.next()
                ij, bij = ijr.next()
                P.op("dve", lambda e: e.tensor_single_scalar(ju[:, 0], bj[:], 4, op=ALU.logical_shift_right), r=[bbj], w=[bju])
                P.op("dve", lambda e: e.tensor_single_scalar(ju[:, 1], bj[:], 15, op=ALU.bitwise_and), r=[bbj], w=[bju])
                P.op("dve", lambda e: e.tensor_copy(ij[:, 0:2], ju[:]), r=[bju], w=[bij])
                mifv = mif[:].rearrange("p (h t) i -> p h t i", t=2)
                for t in range(2):
                    for hh in range(2):
                        hs = slice(hh * 4, (hh + 1) * 4)
                        eq, beq = eqr.next()
                        P.op("dve", lambda e: e.tensor_tensor(eq[:], ij[:, t, hs].unsqueeze(3).to_broadcast([128, 4, 16, 16]),
                                                               iota16[:].unsqueeze(1).unsqueeze(1).to_broadcast([128, 4, 16, 16]), ALU.is_equal), r=[bij, b_iota], w=[beq])
                        P.op("dve", lambda e: e.tensor_tensor(eq[:], eq[:], mifv[:, hs, t, :].unsqueeze(2).to_broadcast([128, 4, 16, 16]), ALU.mult), r=[bmif], w=[beq])
                        P.op("dve", lambda e: e.tensor_reduce(out=ij[:, 2 + t, hs], in_=eq[:], axis=AX.X, op=ALU.add), r=[beq], w=[bij])
                        yield
                P.op("dve", lambda e: e.scalar_tensor_tensor(out=ij[:, 0], in0=ij[:, 2], scalar=128.0, in1=ij[:, 3], op0=ALU.mult, op1=ALU.add), w=[bij])
                eid, beid = eidr.next()
                P.op("dve", lambda e: e.tensor_copy(eid[:], ij[:, 0].rearrange("p h k -> p (h k)")), r=[bij], w=[beid])
                yield
                gw, bgw = gwr.next()
                gwv = gw[:, 0, :].rearrange("p (h k) -> p h k", h=8)
                P.op("dve", lambda e: e.tensor_tensor(gwv, bs_[:], bs_[:, :, 0:1].to_broadcast([128, 8, 16]), ALU.subtract), r=[bbs], w=[bgw])
                P.op("act", lambda e: e.activation(gw[:, 0, :], gw[:, 0, :], AF.Exp), w=[bgw])
                P.op("dve", lambda e: e.tensor_reduce(out=gw[:, 1, 0:8], in_=gwv, axis=AX.X, op=ALU.add), w=[bgw])
                P.op("dve", lambda e: e.reciprocal(gw[:, 1, 0:8], gw[:, 1, 0:8]), w=[bgw])
                P.op("dve", lambda e: e.tensor_tensor(gwv, gwv, gw[:, 1, 0:8].unsqueeze(2).to_broadcast([128, 8, 16]), ALU.mult), w=[bgw])
                res.update(hb=hb, bhb=bhb, eid=eid, beid=beid, gw=gw, bgw=bgw)

            def stage2(blk, s_, bg=None):
                hb, bhb, eid, beid, gw, bgw = (s_[k_] for k_ in ("hb", "bhb", "eid", "beid", "gw", "bgw"))
                av, bav = avr.next()
                jk, bjk = jkr.next()
                pend = None
                bsl = [Buf("sl%d" % i_) for i_ in range(128)]
                for b__ in bsl:
                    b__.w = dict(bav.base)

                def tail(slot, Gt, bGt):
                    P.op("dve", lambda e: e.tensor_tensor(av[:, 2, slot:slot + 1], av[:, 1, slot:slot + 1], gw[:, 0, slot:slot + 1], ALU.mult), r=[bsl[slot], bgw], w=[bsl[slot]])
                    dg, bdg = dgr.next()
                    P.op("act", lambda e: e.activation(dg[:], identb[:], AF.Identity, scale=av[:, 2, slot:slot + 1]), r=[b_identb, bsl[slot]], w=[bdg])
                    for cb in range(4):
                        po, bpo = pacc[cb]
                        P.op("pe", lambda e: e.matmul(po[:, 0:512], lhsT=dg[:], rhs=Gt[:, D + cb * 512:D + (cb + 1) * 512], start=(slot == 0), stop=(slot == 127)),
                             r=[bdg, bGt], w=[bpo])

                for slot in range(128):
                    Gt, bGt = Gr_.next()
                    P.gather(Gt[:], pcomb_d, eid[:, slot:slot + 1].bitcast(U32), r=[beid, B_pdnb, B_pupb], w=[bGt], sb=bGt)
                    P.op("dve", lambda e: e.scalar_tensor_tensor(out=jk[:], in0=Gt[:, 0:D], scalar=1.0, in1=hb[:], op0=ALU.mult, op1=ALU.mult,
                                                                 accum_out=av[:, 0, slot:slot + 1]), r=[bGt, bhb], w=[bsl[slot]])
                    P.op("act", lambda e: e.activation(av[:, 1, slot:slot + 1], av[:, 0, slot:slot + 1], AF.Gelu), r=[bsl[slot]], w=[bsl[slot]])
                    if pend is not None:
                        tail(*pend)
                    pend = (slot, Gt, bGt)
                    if bg is not None and slot >= 4:
                        next(bg, None)
                        next(bg, None)
                tail(*pend)
                if bg is not None:
                    for _ in bg:
                        pass
                for b__ in bsl:
                    for k__, v__ in list(b__.w.items()) + list(b__.r.items()):
                        if bav.w.get(k__, 0) < v__:
                            bav.w[k__] = v__
                tmG, btm = Gr_.next()
                tm = tmG[:].bitcast(F32)
                x1G, bx1 = Gr_.next()
                x1 = x1G[:].bitcast(F32)
                P.dma(x1, x1_d[blk * 128:(blk + 1) * 128, :], r=[B_x1], w=[bx1], sb=bx1)
                for cb in range(4):
                    po, bpo = pacc[cb]
                    P.op("dve", lambda e: e.tensor_tensor(tm[:, cb * 512:(cb + 1) * 512], po[:, 0:512], gt2b[:, cb * 512:(cb + 1) * 512], ALU.mult), r=[b_gt2b], w=[btm, bpo])
                P.op("dve", lambda e: e.tensor_tensor(x1, x1, tm, ALU.add), r=[btm], w=[bx1])
                st2, bs2 = str_.next()
                jk, bjk = jkr.next()
                P.op("dve", lambda e: e.scalar_tensor_tensor(out=jk[:], in0=x1, scalar=1.0, in1=x1, op0=ALU.mult, op1=ALU.mult, accum_out=st2[:, 0:1]),
                     r=[bx1], w=[bjk, bs2])
                P.op("dve", lambda e: e.tensor_scalar(st2[:, 1:2], st2[:, 0:1], 1.0 / D, EPS, op0=ALU.mult, op1=ALU.add), w=[bs2])
                P.op("act", lambda e: e.activation(st2[:, 2:3], st2[:, 1:2], AF.Sqrt), w=[bs2])
                P.op("dve", lambda e: e.reciprocal(st2[:, 3:4], st2[:, 2:3]), w=[bs2])
                P.op("dve", lambda e: e.scalar_tensor_tensor(out=tm, in0=x1, scalar=st2[:, 3:4], in1=fgb[:], op0=ALU.mult, op1=ALU.mult), r=[bx1, bs2, b_fgb], w=[btm]) if False else \
                    P.op("dve", lambda e: e.scalar_tensor_tensor(out=tm, in0=x1, scalar=st2[:, 3:4], in1=fgb[:], op0=ALU.mult, op1=ALU.mult), r=[bx1, bs2, b_fgb], w=[btm])
                P.dma(out_d[blk * 128:(blk + 1) * 128, :], tm, r=[btm], w=[B_out], sb=btm)

            NBLK = H // 128
            cur = {}
            for _ in stage1(0, cur):
                pass
            for blk in range(NBLK):
                nxt = {}
                bg = stage1(blk + 1, nxt) if blk + 1 < NBLK else None
                stage2(blk, cur, bg)
                cur = nxt
        P.finish()
    return nc


_PROG_CACHE = {}


def _host_inputs(inp, L, cores):
    f32 = np.float32
    x = np.asarray(inp["x"], f32)
    ctx = np.asarray(inp["ctx"], f32)
    c = np.asarray(inp["c"], f32)
    c_ctx = np.asarray(inp["c_ctx"], f32)
    w_in = np.ascontiguousarray(np.asarray(inp["w_in"], f32)[0])
    shared = dict(
        w_ada=np.ascontiguousarray(np.asarray(inp["w_ada"], f32)[0]),
        b_ada=np.asarray(inp["b_ada"], f32).reshape(1, -1),
        norm1_g=np.asarray(inp["norm1_g"], f32).reshape(1, -1),
        w_in=w_in,
        w_ret_out=np.ascontiguousarray(np.asarray(inp["w_ret_out"], f32)[0]),
        w_mlstm_out=np.ascontiguousarray(np.asarray(inp["w_mlstm_out"], f32)[0]),
        w_out=np.ascontiguousarray(np.asarray(inp["w_out"], f32)[0]),
        norm2_g=np.asarray(inp["norm2_g"], f32).reshape(1, -1),
        peer_query=np.ascontiguousarray(np.asarray(inp["peer_query"], f32)[0]),
        peer_keys=np.ascontiguousarray(np.asarray(inp["peer_keys"], f32)[0].reshape(16 * 128, 128)),
        peer_down=np.ascontiguousarray(np.asarray(inp["peer_down"], f32)[0]),
        peer_up=np.ascontiguousarray(np.asarray(inp["peer_up"], f32)[0]),
        final_g=np.asarray(inp["final_g"], f32).reshape(1, -1),
    )
    p = np.arange(128)
    ident = (p[:, None] == p[None, :]).astype(f32)
    triA = (p[:, None] <= p[None, :]).astype(f32)
    triB = (p[:, None] >= p[None, :]).astype(f32)
    negA = (triB - 1.0) * 1.0e30
    negB = (triA - 1.0) * 1.0e30
    ones = np.ones((128, 128), f32)
    cst = np.ascontiguousarray(np.stack([ident, triA, triB, negA, negB, ones], axis=1).astype(f32))
    pos = np.zeros((128, 4), f32)
    pos[:, 0] = p + 1.0
    pos[:, 1] = 128.0 - p
    shared["cst"] = cst
    shared["pos"] = pos
    w_mg = w_in[:, O_MG:O_MG + 16]
    gbias = np.asarray(inp["m_gate_bias"], f32)[0].reshape(16)
    rdec = np.asarray(inp["ret_decay"], f32)[0]
    mconv = np.asarray(inp["m_conv"], f32)[0]
    n_ax = 16
    freq = (10000.0 ** (-np.arange(n_ax, dtype=np.float64) / n_ax))
    maps = []
    for (b, half) in cores:
        t = np.arange(L) if half == 0 else np.arange(L - 1, -1, -1)
        row = (t // 64).astype(np.float64)
        col = (t % 64).astype(np.float64)
        ang = np.concatenate([row[:, None] * freq, col[:, None] * freq], axis=-1)
        rot = np.concatenate([np.cos(ang), np.sin(ang)], axis=-1).astype(f32)
        m = dict(shared)
        if half == 0:
            m["x"] = np.ascontiguousarray(x[b])
            m["ctx"] = np.ascontiguousarray(ctx[b])
            m["w_mg"] = np.ascontiguousarray(w_mg)
            m["gbias"] = gbias.reshape(1, 16).copy()
            m["ret_decay"] = rdec.reshape(1, 16).copy()
            m["m_conv"] = np.ascontiguousarray(mconv)
        else:
            m["x"] = np.ascontiguousarray(x[b, ::-1])
            m["ctx"] = np.ascontiguousarray(ctx[b, ::-1])
            m["w_mg"] = np.ascontiguousarray(np.concatenate([w_mg[:, 8:16], w_mg[:, 0:8]], axis=1))
            m["gbias"] = np.concatenate([gbias[8:16], gbias[0:8]]).reshape(1, 16).copy()
            m["ret_decay"] = np.concatenate([rdec[1], rdec[0]]).reshape(1, 16).copy()
            m["m_conv"] = np.ascontiguousarray(mconv[::-1])
        m["cvec"] = np.ascontiguousarray(np.stack([c[b], c_ctx], axis=0))
        m["rot"] = rot
        maps.append(m)
    return maps


def kernel(**inputs):
    x = np.asarray(inputs["x"])
    B, L, _ = x.shape
    cores = [(b, h) for b in range(B) for h in range(2)]
    if L not in _PROG_CACHE:
        _PROG_CACHE[L] = build_program(L)
    nc = _PROG_CACHE[L]
    maps = _host_inputs(inputs, L, cores)
    res = run_bass_kernel_spmd(nc, maps, core_ids=list(range(len(cores))))
    out = np.empty((B, L, D), np.float32)
    Hh = L // 2
    for i, (b, h) in enumerate(cores):
        o = np.asarray(res.results[i]["out"], np.float32)
        if h == 0:
            out[b, 0:Hh] = o
        else:
            out[b, Hh:L] = o[::-1]
    return out
```
